# Optimizing a Trainium2 kernel written in Bass

```python
import math
import jax, jax.numpy as jnp
from jax import lax
import numpy as np

D_MODEL = 1024
BATCH = 16
SEQ = 2048
DEPTH = 1

CHUNK = 64
D_MIX = D_MODEL
D_HGRN = D_MIX // 2
HG_HEADS = 4
HG_DK = D_HGRN // HG_HEADS
D_SSM = D_MIX - D_HGRN
SSM_GROUP = 16
SSM_GROUPS = D_SSM // SSM_GROUP
SSM_STATE = 64
IN_COLS = 4 * D_HGRN + D_SSM
PEER_HEADS = 8
PEER_NKEYS = 128
PEER_EXPERTS = PEER_NKEYS * PEER_NKEYS
PEER_DKEY = 256
PEER_HALF = PEER_DKEY // 2
PEER_TOPK = 16
PEER_TOKEN_BLOCK = 128
DEEP_ALPHA = (2.0 * DEPTH) ** 0.25
DEEP_BETA = (8.0 * DEPTH) ** -0.25
LN_EPS = 1e-5
RMS_EPS = 1e-6

kernel_name = 'hymba_hgrn2_s5_peer_deepnorm_adaln'


def layer_norm(x, w, b):
    xf = x.astype(jnp.float32)
    mu = jnp.mean(xf, axis=-1, keepdims=True)
    var = jnp.mean(jnp.square(xf - mu), axis=-1, keepdims=True)
    y = (xf - mu) * lax.rsqrt(var + LN_EPS) * w.astype(jnp.float32) + b.astype(jnp.float32)
    return y.astype(x.dtype)


def rms_norm(x, w):
    xf = x.astype(jnp.float32)
    return xf * lax.rsqrt(jnp.mean(jnp.square(xf), axis=-1, keepdims=True) + RMS_EPS) * w.astype(jnp.float32)


def hgrn2_group(q, f_logit, i, g, lb, norm_w):
    B, S, _ = q.shape
    nc = S // CHUNK
    f32 = jnp.float32
    q = jax.nn.silu(q.astype(f32))
    f = lb + (1.0 - lb) * jax.nn.sigmoid(f_logit.astype(f32))
    log_f = jnp.log(f)
    k = 1.0 - f
    v = i.astype(f32)

    def to_chunks(t):
        return t.reshape(B, nc, CHUNK, HG_HEADS, HG_DK).transpose(1, 0, 3, 2, 4)

    qc, kc, vc, lc = to_chunks(q), to_chunks(k), to_chunks(v), to_chunks(log_f)
    causal = jnp.tril(jnp.ones((CHUNK, CHUNK), dtype=bool))[:, :, None]

    def step(state, inp):
        q_c, k_c, v_c, l_c = inp
        b = jnp.cumsum(l_c, axis=2)
        diff = b[:, :, :, None, :] - b[:, :, None, :, :]
        decay = jnp.exp(jnp.where(causal, diff, -jnp.inf))
        scores = jnp.einsum('bhtk,bhtsk,bhsk->bhts', q_c, decay, k_c)
        o = (jnp.einsum('bhts,bhsv->bhtv', scores, v_c)
             + jnp.einsum('bhtk,bhkv->bhtv', q_c * jnp.exp(b), state))
        b_last = b[:, :, -1:, :]
        state = (jnp.exp(b_last[:, :, 0, :])[..., None] * state
                 + jnp.einsum('bhsk,bhsv->bhkv', k_c * jnp.exp(b_last - b), v_c))
        return state, o

    s0 = jnp.zeros((B, HG_HEADS, HG_DK, HG_DK), f32)
    _, o = lax.scan(step, s0, (qc, kc, vc, lc))
    o = o.transpose(1, 0, 3, 2, 4).reshape(B, S, HG_HEADS, HG_DK)
    o = rms_norm(o, norm_w.reshape(HG_HEADS, HG_DK))
    o = o * jax.nn.silu(g.astype(f32).reshape(B, S, HG_HEADS, HG_DK))
    return o.reshape(B, S, D_HGRN)


def s5_group(u, a_re, a_im, log_dt, b_re, b_im, c_re, c_im, d_skip, glu_w, glu_b, norm_w):
    B, S, _ = u.shape
    f32 = jnp.float32
    uf = u.astype(f32).reshape(B, S, SSM_GROUPS, SSM_GROUP)
    ar, ai = a_re.astype(f32), a_im.astype(f32)
    dt = jnp.exp(log_dt.astype(f32))[:, None]
    mag = jnp.exp(ar * dt)
    lam_re = mag * jnp.cos(ai * dt)
    lam_im = mag * jnp.sin(ai * dt)
    den = ar * ar + ai * ai
    nr, ni = lam_re - 1.0, lam_im
    z_re = (nr * ar + ni * ai) / den
    z_im = (ni * ar - nr * ai) / den
    br, bi = b_re.astype(f32), b_im.astype(f32)
    bb_re = z_re[..., None] * br - z_im[..., None] * bi
    bb_im = z_re[..., None] * bi + z_im[..., None] * br
    bu_re = jnp.einsum('bsgi,gpi->bsgp', uf, bb_re)
    bu_im = jnp.einsum('bsgi,gpi->bsgp', uf, bb_im)
    lam_re_s = jnp.broadcast_to(lam_re, (S, SSM_GROUPS, SSM_STATE))
    lam_im_s = jnp.broadcast_to(lam_im, (S, SSM_GROUPS, SSM_STATE))

    def combine(e_i, e_j):
        ar_i, ai_i, br_i, bi_i = e_i
        ar_j, ai_j, br_j, bi_j = e_j
        return (ar_j * ar_i - ai_j * ai_i,
                ar_j * ai_i + ai_j * ar_i,
                ar_j * br_i - ai_j * bi_i + br_j,
                ar_j * bi_i + ai_j * br_i + bi_j)

    def scan_one(bre, bim):
        _, _, xr, xi = lax.associative_scan(combine, (lam_re_s, lam_im_s, bre, bim), axis=0)
        return xr, xi

    x_re, x_im = jax.vmap(scan_one)(bu_re, bu_im)
    y = (jnp.einsum('bsgp,gip->bsgi', x_re, c_re.astype(f32))
         - jnp.einsum('bsgp,gip->bsgi', x_im, c_im.astype(f32))
         + d_skip.astype(f32) * uf)
    y = jax.nn.gelu(y.reshape(B, S, D_SSM), approximate=False)
    y = y * jax.nn.sigmoid(y @ glu_w.astype(f32) + glu_b.astype(f32))
    return rms_norm(y, norm_w)


def peer_ffn(h, w_q, sub_keys, u_tab, v_tab):
    B, S, D = h.shape
    T = B * S
    f32 = jnp.float32
    ht = h.reshape(T, D)
    q = (ht @ w_q).astype(f32).reshape(T, PEER_HEADS, 2, PEER_HALF)
    scores = jnp.einsum('thpc,hpnc->thpn', q, sub_keys.astype(f32))
    top_s, top_i = lax.top_k(scores, PEER_TOPK)
    cand = (top_s[:, :, 0, :, None] + top_s[:, :, 1, None, :]).reshape(T, PEER_HEADS, PEER_TOPK * PEER_TOPK)
    best_s, best_c = lax.top_k(cand, PEER_TOPK)
    i1 = jnp.take_along_axis(top_i[:, :, 0], best_c // PEER_TOPK, axis=-1)
    i2 = jnp.take_along_axis(top_i[:, :, 1], best_c % PEER_TOPK, axis=-1)
    hk = PEER_HEADS * PEER_TOPK
    expert = (i1 * PEER_NKEYS + i2).reshape(T, hk)
    gate = jax.nn.softmax(best_s, axis=-1).reshape(T, hk)
    nb = T // PEER_TOKEN_BLOCK

    def block(args):
        hb, eb, gb = args
        u_sel = jnp.take(u_tab, eb, axis=0)
        z = jnp.einsum('ted,td->te', u_sel, hb)
        a = jax.nn.gelu(z.astype(f32), approximate=False) * gb
        v_sel = jnp.take(v_tab, eb, axis=0)
        return jnp.einsum('te,ted->td', a.astype(v_sel.dtype), v_sel)

    out = lax.map(block, (ht.reshape(nb, PEER_TOKEN_BLOCK, D),
                          expert.reshape(nb, PEER_TOKEN_BLOCK, hk),
                          gate.reshape(nb, PEER_TOKEN_BLOCK, hk)))
    return out.reshape(B, S, D).astype(h.dtype)


def setup_inputs(seed: int = 0) -> dict:
    key = jax.random.key(seed)
    ks = jax.random.split(key, 32)
    f32 = jnp.float32
    L, D = DEPTH, D_MODEL

    def nrm(k, shape, s):
        return jax.random.normal(k, shape, f32) * s

    n_idx = jnp.arange(SSM_STATE, dtype=f32)
    return {
        'x': nrm(ks[0], (BATCH, SEQ, D), 1.0),
        'c': nrm(ks[1], (BATCH, D), 1.0),
        'ada_w': nrm(ks[2], (L, D, 6 * D), 0.5 * D ** -0.5),
        'ada_b': nrm(ks[3], (L, 6 * D), 0.02),
        'w_in': nrm(ks[4], (L, D, IN_COLS), D ** -0.5),
        'hg_lower_bound': nrm(ks[5], (DEPTH + 1, D_HGRN), 1.0),
        'hg_norm_w': 1.0 + nrm(ks[6], (L, D_HGRN), 0.02),
        'ssm_a_re': -0.5 + nrm(ks[7], (L, SSM_GROUPS, SSM_STATE), 0.01),
        'ssm_a_im': jnp.pi * n_idx + nrm(ks[8], (L, SSM_GROUPS, SSM_STATE), 0.01),
        'ssm_log_dt': jax.random.uniform(ks[9], (L, SSM_GROUPS), f32, math.log(1e-3), math.log(1e-1)),
        'ssm_b_re': nrm(ks[10], (L, SSM_GROUPS, SSM_STATE, SSM_GROUP), (2.0 * SSM_GROUP) ** -0.5),
        'ssm_b_im': nrm(ks[11], (L, SSM_GROUPS, SSM_STATE, SSM_GROUP), (2.0 * SSM_GROUP) ** -0.5),
        'ssm_c_re': nrm(ks[12], (L, SSM_GROUPS, SSM_GROUP, SSM_STATE), SSM_STATE ** -0.5),
        'ssm_c_im': nrm(ks[13], (L, SSM_GROUPS, SSM_GROUP, SSM_STATE), SSM_STATE ** -0.5),
        'ssm_d': nrm(ks[14], (L, SSM_GROUPS, SSM_GROUP), 1.0),
        'ssm_glu_w': nrm(ks[15], (L, D_SSM, D_SSM), D_SSM ** -0.5),
        'ssm_glu_b': nrm(ks[16], (L, D_SSM), 0.02),
        'ssm_norm_w': 1.0 + nrm(ks[17], (L, D_SSM), 0.02),
        'w_out': nrm(ks[18], (L, D_MIX, D), DEEP_BETA * D_MIX ** -0.5),
        'ln1_w': 1.0 + nrm(ks[19], (L, D), 0.02),
        'ln1_b': nrm(ks[20], (L, D), 0.02),
        'peer_w_q': nrm(ks[21], (L, D, PEER_HEADS * PEER_DKEY), D ** -0.5),
        'peer_sub_keys': nrm(ks[22], (L, PEER_HEADS, 2, PEER_NKEYS, PEER_HALF), PEER_HALF ** -0.5),
        'peer_u': nrm(ks[23], (L, PEER_EXPERTS, D), D ** -0.5),
        'peer_v': nrm(ks[24], (L, PEER_EXPERTS, D), DEEP_BETA * PEER_HEADS ** -0.5),
        'ln2_w': 1.0 + nrm(ks[25], (L, D), 0.02),
        'ln2_b': nrm(ks[26], (L, D), 0.02),
    }


def reference(x, c, ada_w, ada_b, w_in, hg_lower_bound, hg_norm_w, ssm_a_re, ssm_a_im, ssm_log_dt,
              ssm_b_re, ssm_b_im, ssm_c_re, ssm_c_im, ssm_d, ssm_glu_w, ssm_glu_b, ssm_norm_w,
              w_out, ln1_w, ln1_b, peer_w_q, peer_sub_keys, peer_u, peer_v, ln2_w, ln2_b):
    lb_all = jnp.cumsum(jax.nn.softmax(hg_lower_bound.astype(jnp.float32), axis=0), axis=0)
    cond = jax.nn.silu(c)
    for l in range(DEPTH):
        mod = cond @ ada_w[l] + ada_b[l]
        sh1, sc1, g1, sh2, sc2, g2 = jnp.split(mod[:, None, :], 6, axis=-1)

        h = x * (1.0 + sc1) + sh1
        proj = h @ w_in[l]
        q, f, i, g, u = jnp.split(proj, [D_HGRN, 2 * D_HGRN, 3 * D_HGRN, 4 * D_HGRN], axis=-1)
        o_hg = hgrn2_group(q, f, i, g, lb_all[l], hg_norm_w[l])
        o_ssm = s5_group(u, ssm_a_re[l], ssm_a_im[l], ssm_log_dt[l], ssm_b_re[l], ssm_b_im[l],
                         ssm_c_re[l], ssm_c_im[l], ssm_d[l], ssm_glu_w[l], ssm_glu_b[l], ssm_norm_w[l])
        mixed = jnp.concatenate([o_hg, o_ssm], axis=-1).astype(x.dtype) @ w_out[l]
        x = layer_norm(DEEP_ALPHA * x + (1.0 + g1) * mixed, ln1_w[l], ln1_b[l])

        h2 = x * (1.0 + sc2) + sh2
        ffn = peer_ffn(h2, peer_w_q[l], peer_sub_keys[l], peer_u[l], peer_v[l])
        x = layer_norm(DEEP_ALPHA * x + (1.0 + g2) * ffn, ln2_w[l], ln2_b[l])
    return x
```

```python
import math
from contextlib import ExitStack, contextmanager
import numpy as np
import concourse.bass as bass
import concourse.mybir as mybir
from concourse.bass_utils import run_bass_kernel_spmd

F32 = mybir.dt.float32
I32 = mybir.dt.int32
U32 = mybir.dt.uint32
AF = mybir.ActivationFunctionType
ALU = mybir.AluOpType
AX = mybir.AxisListType

NCORES = 8
D = 1024
SEQ = 2048
NB = 2
TOK = NB * SEQ
ALPHA = 2.0 ** 0.25
LN_EPS = 1e-5
RMS_EPS = 1e-6
MID = 31
NEG = -1.0e30


class V:
    __slots__ = ("t", "ap")

    def __init__(self, t, ap):
        self.t = t
        self.ap = ap


class T:
    def __init__(self, h, name):
        self.h = h
        self.name = name
        self.w = None
        self.wd = {}
        self.r = {}
        self.dsem = None
        self.dcnt = 0

    def __getitem__(self, idx):
        return V(self, self.h[idx])

    def cust(self, offset, dims):
        return V(self, bass.AP(self.h, offset, [list(d) for d in dims]))


class Prog:
    def __init__(self, nc):
        self.nc = nc
        self.root = ExitStack()
        self.stacks = [self.root]
        self.E = {"pe": nc.tensor, "act": nc.scalar, "dve": nc.vector, "pool": nc.gpsimd, "sp": nc.sync}
        self.sem = {k: self.root.enter_context(nc.semaphore("s_" + k)) for k in ("pe", "act", "dve", "pool")}
        self.cnt = {k: 0 for k in self.sem}
        self.waited = {k: {} for k in self.E}
        self.dma_tiles = []
        self.nname = 0

    def sb(self, name, shape, dt=F32):
        self.nname += 1
        h = self.stacks[-1].enter_context(self.nc.sbuf_tensor("%s_%d" % (name, self.nname), list(shape), dt))
        return T(h, name)

    def ps(self, name):
        self.nname += 1
        h = self.stacks[-1].enter_context(self.nc.psum_tensor("%s_%d" % (name, self.nname), [128, 512], F32))
        return T(h, name)

    def dram(self, name, shape, dt=F32, kind="Internal"):
        h = self.nc.dram_tensor(name, list(shape), dt, kind=kind)
        return T(h, name)

    @contextmanager
    def scope(self):
        st = ExitStack()
        self.stacks.append(st)
        try:
            yield
        finally:
            self.barrier()
            self.stacks.pop()
            st.close()

    def _wait(self, eng, evs):
        need = {}
        for ev in evs:
            if ev is None:
                continue
            s, v = ev
            k = id(s)
            if k not in need or need[k][1] < v:
                need[k] = (s, v)
        for k, (s, v) in need.items():
            if eng == "pe" and s is self.sem["pe"]:
                continue
            if self.waited[eng].get(k, 0) >= v:
                continue
            self.E[eng].wait_ge(s, v)
            self.waited[eng][k] = v

    def barrier(self):
        evs = [(self.sem[k], self.cnt[k]) for k in self.sem if self.cnt[k] > 0]
        evs += [(t.dsem, t.dcnt) for t in self.dma_tiles if t.dcnt > 0]
        for eng in self.E:
            self._wait(eng, evs)

    def op(self, eng, fn, reads=(), writes=()):
        evs = []
        for t in reads:
            evs.append(t.w)
            evs.extend(t.wd.values())
        for t in writes:
            evs.append(t.w)
            evs.extend(t.wd.values())
            evs.extend(t.r.values())
        self._wait(eng, evs)
        inst = fn(self.E[eng])
        self.cnt[eng] += 1
        inst.then_inc(self.sem[eng], 1)
        ev = (self.sem[eng], self.cnt[eng])
        for t in writes:
            t.w = ev
            t.wd = {}
            t.r = {}
        for t in reads:
            if t not in writes:
                t.r[eng] = ev
        return inst

    def dma(self, q, o, i, fn=None, extra=(), src_sem=False):
        ot, it = o.t, i.t
        st = it if src_sem else ot
        evs = [it.w] + list(it.wd.values()) + [t.w for t in extra]
        if ot.w is not None and not (st.dsem is not None and ot.w[0] is st.dsem):
            evs.append(ot.w)
        for k, ev in ot.wd.items():
            if not (st.dsem is not None and ev[0] is st.dsem):
                evs.append(ev)
        evs.extend(ot.r.values())
        self._wait(q, evs)
        if st.dsem is None:
            self.nname += 1
            st.dsem = self.root.enter_context(self.nc.semaphore("d%d" % self.nname))
            self.dma_tiles.append(st)
        if fn is None:
            inst = self.E[q].dma_start(out=o.ap, in_=i.ap)
        else:
            inst = fn(self.E[q])
        st.dcnt += 16
        inst.then_inc(st.dsem, 16)
        ev = (st.dsem, st.dcnt)
        if src_sem:
            ot.wd[id(st.dsem)] = ev
        else:
            ot.w = ev
            ot.wd = {}
        ot.r = {}
        it.r["d%d" % id(st)] = ev
        for t in extra:
            t.r["d%d" % id(st)] = ev
        return inst

    @staticmethod
    def _sv(x, reads):
        if isinstance(x, V):
            reads.append(x.t)
            return x.ap
        return x

    def tt(self, eng, o, a, b, op):
        return self.op(eng, lambda e: e.tensor_tensor(out=o.ap, in0=a.ap, in1=b.ap, op=op), [a.t, b.t], [o.t])

    def ts(self, eng, o, a, s1, op0, s2=None, op1=None):
        reads = [a.t]
        s1a = self._sv(s1, reads)
        s2a = self._sv(s2, reads)
        if op1 is None:
            return self.op(eng, lambda e: e.tensor_scalar(out=o.ap, in0=a.ap, scalar1=s1a, scalar2=None, op0=op0), reads, [o.t])
        return self.op(eng, lambda e: e.tensor_scalar(out=o.ap, in0=a.ap, scalar1=s1a, scalar2=s2a, op0=op0, op1=op1), reads, [o.t])

    def stt(self, eng, o, a, s, b, op0, op1):
        reads = [a.t, b.t]
        sa = self._sv(s, reads)
        return self.op(eng, lambda e: e.scalar_tensor_tensor(out=o.ap, in0=a.ap, scalar=sa, in1=b.ap, op0=op0, op1=op1), reads, [o.t])

    def cp(self, eng, o, a):
        if eng == "act":
            return self.op(eng, lambda e: e.copy(out=o.ap, in_=a.ap), [a.t], [o.t])
        return self.op(eng, lambda e: e.tensor_copy(out=o.ap, in_=a.ap), [a.t], [o.t])

    def act(self, o, a, func, bias=None, scale=None, accum=None):
        reads = [a.t]
        kw = {}
        if bias is not None:
            kw["bias"] = self._sv(bias, reads)
        if scale is not None:
            kw["scale"] = self._sv(scale, reads)
        writes = [o.t]
        if accum is not None:
            kw["accum_out"] = accum.ap
            writes.append(accum.t)
        return self.op("act", lambda e: e.activation(out=o.ap, in_=a.ap, func=func, **kw), reads, writes)

    def mm(self, o, l, r, start=True, stop=True, tp=None):
        if tp is None:
            return self.op("pe", lambda e: e.matmul(o.ap, l.ap, r.ap, start=start, stop=stop), [l.t, r.t], [o.t])
        return self.op("pe", lambda e: e.matmul(o.ap, l.ap, r.ap, start=start, stop=stop, tile_position=tp), [l.t, r.t], [o.t])

    def tr(self, o, a, ident):
        return self.op("pe", lambda e: e.transpose(o.ap, a.ap, ident.ap), [a.t, ident.t], [o.t])

    def recip(self, o, a):
        return self.op("dve", lambda e: e.reciprocal(out=o.ap, in_=a.ap), [a.t], [o.t])


def _consts():
    s = np.arange(64)[:, None]
    t = np.arange(64)[None, :]
    le = (s <= t).astype(np.float32)
    lm = (s <= MID).astype(np.float32) * np.ones((1, 64), np.float32)
    A1 = le - lm
    A2 = lm - le
    A3 = (s > t).astype(np.float32)
    z = np.zeros((64, 64), np.float32)
    bd = lambda a: np.block([[a, z], [z, a]]).astype(np.float32)
    RB = np.zeros((128, 132), np.float32)
    RB[0:64, 0:64] = A1
    RB[64:128, 64:128] = A1
    RB[0:64, 128] = 1.0
    RB[0:64, 129] = lm[:, 0]
    RB[64:128, 130] = 1.0
    RB[64:128, 131] = lm[:, 0]
    sel = np.zeros((2, 2, 128), np.float32)
    sel[0, 0, :] = 1.0
    sel[1, 1, :] = 1.0
    return {
        "k_ident": np.eye(128, dtype=np.float32),
        "k_a2": bd(A2),
        "k_a3": bd(A3),
        "k_rb": RB,
        "k_mask": bd(le),
        "k_iota": np.broadcast_to(np.arange(16, dtype=np.float32), (128, 16)).copy(),
        "k_ones": np.ones((128, 128), np.float32),
        "k_sel": sel,
        "k_selj": _selj(),
        "k_cmask": np.kron((np.arange(4)[:, None] <= np.arange(4)[None, :]).astype(np.float32), np.ones((32, 32), np.float32)),
    }


def _selj():
    a = np.zeros((128, 16, 128), np.float32)
    for j in range(4):
        for sl in range(4):
            for c in range(32):
                a[32 * j + c, j * 4 + sl, 32 * sl + c] = 1.0
    return a


def _prep_shared(inp):
    f = lambda a: np.ascontiguousarray(a, dtype=np.float32)
    o = {}
    o["ada_w"] = f(inp["ada_w"][0])
    o["ada_b2"] = f(np.broadcast_to(inp["ada_b"][0][None, :], (2, 6 * D)))
    o["w_in"] = f(inp["w_in"][0])
    o["hb"] = f(inp["hg_lower_bound"])
    col = lambda v, n: f(np.asarray(v).reshape(n, 128).T)
    o["hg_nw"] = col(inp["hg_norm_w"][0], 4)
    o["s_ar"] = col(inp["ssm_a_re"][0], 16)
    o["s_ai"] = col(inp["ssm_a_im"][0], 16)
    o["s_ldt"] = col(np.repeat(inp["ssm_log_dt"][0], 64), 16)
    bre, bim = inp["ssm_b_re"][0], inp["ssm_b_im"][0]
    cre, cim = inp["ssm_c_re"][0], inp["ssm_c_im"][0]
    BTr = np.zeros((512, 128), np.float32)
    BTi = np.zeros((512, 128), np.float32)
    CTr = np.zeros((16, 128, 128), np.float32)
    CTi = np.zeros((16, 128, 128), np.float32)
    for g in range(32):
        gl = g % 2
        st = g // 2
        j = st % 4
        BTr[g * 16:(g + 1) * 16, gl * 64:gl * 64 + 64] = bre[g].T
        BTi[g * 16:(g + 1) * 16, gl * 64:gl * 64 + 64] = bim[g].T
        CTr[st, gl * 64:gl * 64 + 64, 32 * j + gl * 16:32 * j + gl * 16 + 16] = cre[g].T
        CTi[st, gl * 64:gl * 64 + 64, 32 * j + gl * 16:32 * j + gl * 16 + 16] = cim[g].T
    o["s_btr"] = f(BTr.reshape(4, 128, 128).transpose(1, 0, 2))
    o["s_bti"] = f(BTi.reshape(4, 128, 128).transpose(1, 0, 2))
    o["s_ctr"] = f(CTr.transpose(1, 0, 2))
    o["s_cti"] = f(CTi.transpose(1, 0, 2))
    o["s_d"] = col(inp["ssm_d"][0].reshape(512), 4)
    BM_r = np.zeros((16, 128, 32), np.float32); BM_i = np.zeros((16, 128, 32), np.float32)
    CM_r = np.zeros((16, 128, 32), np.float32); CM_i = np.zeros((16, 128, 32), np.float32)
    for g in range(32):
        gl = g % 2
        st = g // 2
        BM_r[st, gl * 64:gl * 64 + 64, gl * 16:gl * 16 + 16] = bre[g]
        BM_i[st, gl * 64:gl * 64 + 64, gl * 16:gl * 16 + 16] = bim[g]
        CM_r[st, gl * 64:gl * 64 + 64, gl * 16:gl * 16 + 16] = cre[g].T
        CM_i[st, gl * 64:gl * 64 + 64, gl * 16:gl * 16 + 16] = cim[g].T
    o["s_bmr"] = f(BM_r.transpose(1, 0, 2)); o["s_bmi"] = f(BM_i.transpose(1, 0, 2))
    o["s_cmr"] = f(CM_r.transpose(1, 0, 2)); o["s_cmi"] = f(CM_i.transpose(1, 0, 2))
    dd = inp["ssm_d"][0].reshape(16, 32)
    o["s_drep"] = f(np.tile(dd, (1, 4)).T)
    o["glu_w"] = f(inp["ssm_glu_w"][0])
    o["glu_b"] = col(inp["ssm_glu_b"][0], 4)
    o["s_nw"] = col(inp["ssm_norm_w"][0], 4)
    o["w_out"] = f(inp["w_out"][0])
    o["ln1w"] = f(inp["ln1_w"])
    o["ln1b"] = f(inp["ln1_b"])
    o["ln2w"] = f(inp["ln2_w"])
    o["ln2b"] = f(inp["ln2_b"])
    o["w_q"] = f(inp["peer_w_q"][0])
    o["keysT"] = f(inp["peer_sub_keys"][0].transpose(3, 0, 1, 2).reshape(128, 16, 128))
    o["pu"] = f(inp["peer_u"][0])
    o["pv"] = f(inp["peer_v"][0])
    o.update(_consts())
    return o


SHAPES = {
    "x": [TOK, D], "cT": [128, 8, 2], "ada_w": [D, 6 * D], "ada_b2": [2, 6 * D], "w_in": [D, 2560], "hb": [2, 512],
    "hg_nw": [128, 4], "s_ar": [128, 16], "s_ai": [128, 16], "s_ldt": [128, 16], "s_btr": [128, 4, 128],
    "s_bti": [128, 4, 128], "s_ctr": [128, 16, 128], "s_cti": [128, 16, 128], "s_d": [128, 4], "glu_w": [512, 512],
    "glu_b": [128, 4], "s_nw": [128, 4], "w_out": [D, D], "ln1w": [1, D], "ln1b": [1, D], "ln2w": [1, D],
    "ln2b": [1, D], "w_q": [D, 2048], "keysT": [128, 16, 128], "pu": [16384, D], "pv": [16384, D],
    "k_ident": [128, 128], "k_a2": [128, 128], "k_a3": [128, 128], "k_rb": [128, 132], "k_mask": [128, 128],
    "k_iota": [128, 16], "k_ones": [128, 128], "k_sel": [2, 2, 128], "k_selj": [128, 16, 128], "k_cmask": [128, 128],
    "s_bmr": [128, 16, 32], "s_bmi": [128, 16, 32], "s_cmr": [128, 16, 32], "s_cmi": [128, 16, 32], "s_drep": [128, 16],
}


def build(upto=99, taps=()):
    nc = bass.Bass("TRN2", target_bir_lowering=False)
    P = Prog(nc)
    I = {k: P.dram(k, v, F32, kind="ExternalInput") for k, v in SHAPES.items()}
    out_d = P.dram("out", [TOK, D], F32, kind="ExternalOutput")
    mixed_d = P.dram("mixed_scr", [TOK, D], F32, kind="Internal")
    tapd = {}

    def tap(name, src_t, shape):
        if name in taps:
            td = P.dram("tap_" + name, shape, F32, kind="ExternalOutput")
            tapd[name] = td
            P.dma("sp", td[tuple(slice(None) for _ in shape)], src_t[tuple(slice(None) for _ in shape)])

    def ld(dst, src, q="sp"):
        P.dma(q, dst, src)

    def bc_row(tname, rows=128):
        t = I[tname]
        n = SHAPES[tname][1]
        return V(t, bass.AP(t.h, 0, [[0, rows], [1, n]]))

    ident = P.sb("ident", [128, 128]); ld(ident[:], I["k_ident"][:, :])
    ones = P.sb("ones", [128, 128]); ld(ones[:], I["k_ones"][:, :])
    PS = [P.ps("ps%d" % i) for i in range(8)]
    sh1T = P.sb("sh1T", [128, 8, 2])
    sc1T = P.sb("sc1T", [128, 8, 2])
    mod_d = P.dram("mod_scr", [2, 6 * D], F32, kind="Internal")

    with P.scope():
        cT = P.sb("cT", [128, 8, 2]); ld(cT[:], I["cT"][:, :, :])
        condT = P.sb("condT", [128, 8, 2])
        P.act(condT[:], cT[:], AF.Silu)
        adab = P.sb("adab", [2, 6 * D]); ld(adab[:], I["ada_b2"][:, :])
        mod = P.sb("mod", [2, 6 * D])
        sel = P.sb("sel", [2, 2, 128]); ld(sel[:], I["k_sel"][:, :, :])
        wab = [P.sb("wab%d" % i, [128, 8, 512]) for i in range(2)]
        aw = I["ada_w"]
        for cb in range(12):
            wb = wab[cb % 2]
            src = V(aw, aw.h[:, cb * 512:(cb + 1) * 512].rearrange("(kt p) n -> p kt n", p=128))
            ld(wb[:], src)
            for kt in range(8):
                P.mm(PS[cb % 2][0:2, :], condT[:, kt, :], wb[:, kt, :], start=(kt == 0), stop=(kt == 7))
            P.tt("dve", mod[:, cb * 512:(cb + 1) * 512], PS[cb % 2][0:2, :], adab[:, cb * 512:(cb + 1) * 512], ALU.add)
        tap("mod", mod, [2, 6 * D])
        for j in range(16):
            P.tr(PS[2][:, 2 * j:2 * j + 2], mod[:, j * 128:(j + 1) * 128], ident[0:2, 0:2])
        P.cp("dve", V(sh1T, sh1T.h[:].rearrange("p a b -> p (a b)")), PS[2][:, 0:16])
        P.ts("dve", V(sc1T, sc1T.h[:].rearrange("p a b -> p (a b)")), PS[2][:, 16:32], 1.0, ALU.add)
        P.dma("sp", mod_d[:, :], mod[:])
    if upto <= 0:
        return finish(nc, P, out_d)

    with P.scope():
        a2 = P.sb("a2", [128, 128]); ld(a2[:], I["k_a2"][:, :])
        a3 = P.sb("a3", [128, 128]); ld(a3[:], I["k_a3"][:, :])
        rb = P.sb("rbm", [128, 132]); ld(rb[:], I["k_rb"][:, :])
        mask = P.sb("mask", [128, 128]); ld(mask[:], I["k_mask"][:, :])
        hgnw = P.sb("hgnw", [128, 4]); ld(hgnw[:], I["hg_nw"][:, :])
        lbb = P.sb("lbb", [128, 512])
        omlb = P.sb("omlb", [128, 512])
        with P.scope():
            h0 = P.sb("h0", [128, 512]); h1 = P.sb("h1", [128, 512])
            hbt = I["hb"]
            ld(h0[:], V(hbt, bass.AP(hbt.h, 0, [[0, 128], [1, 512]])))
            ld(h1[:], V(hbt, bass.AP(hbt.h, 512, [[0, 128], [1, 512]])))
            P.tt("dve", h0[:], h0[:], h1[:], ALU.subtract)
            P.act(lbb[:], h0[:], AF.Sigmoid)
            P.ts("dve", omlb[:], lbb[:], -1.0, ALU.mult, 1.0, ALU.add)
        s_zre = P.sb("s_zre", [128, 16]); s_zim = P.sb("s_zim", [128, 16]); s_zimn = P.sb("s_zimn", [128, 16])
        Lre = P.sb("Lre", [128, 11, 16]); Lim = P.sb("Lim", [128, 11, 16]); Limn = P.sb("Limn", [128, 11, 16])
        PWr = P.sb("PWr", [128, 9, 16]); PWi = P.sb("PWi", [128, 9, 16])
        PWDr = P.sb("PWDr", [128, 8, 16]); PWDi = P.sb("PWDi", [128, 8, 16])
        IPr = P.sb("IPr", [128, 8, 16]); IPi = P.sb("IPi", [128, 8, 16])
        LCr = P.sb("LCr", [128, 8, 16]); LCi = P.sb("LCi", [128, 8, 16]); LCin = P.sb("LCin", [128, 8, 16])
        S5T = dict(PWr=PWr, PWi=PWi, PWDr=PWDr, PWDi=PWDi, IPr=IPr, IPi=IPi, LCr=LCr, LCi=LCi, LCin=LCin)
        sdk = P.sb("sdk", [128, 4]); ld(sdk[:], I["s_d"][:, :])
        glub = P.sb("glub", [128, 4]); ld(glub[:], I["glu_b"][:, :])
        snw = P.sb("snw", [128, 4]); ld(snw[:], I["s_nw"][:, :])
        with P.scope():
            ar = P.sb("ar", [128, 16]); ld(ar[:], I["s_ar"][:, :])
            ai = P.sb("ai", [128, 16]); ld(ai[:], I["s_ai"][:, :])
            dt = P.sb("dt", [128, 16]); ld(dt[:], I["s_ldt"][:, :])
            t1 = P.sb("t1", [128, 16]); t2 = P.sb("t2", [128, 16]); t3 = P.sb("t3", [128, 16])
            cc = P.sb("cc", [128, 16]); ss = P.sb("ss", [128, 16]); mag = P.sb("mag", [128, 16])
            P.act(dt[:], dt[:], AF.Exp)
            P.tt("dve", t1[:], ar[:], dt[:], ALU.mult)
            P.act(mag[:], t1[:], AF.Exp)
            P.tt("dve", t2[:], ai[:], dt[:], ALU.mult)
            P.ts("dve", t2[:], t2[:], 1.0 / 16.0, ALU.mult)
            P.act(ss[:], t2[:], AF.Sin)
            P.ts("dve", t3[:], t2[:], -1.0, ALU.mult, math.pi / 2.0, ALU.add)
            P.act(cc[:], t3[:], AF.Sin)
            for _ in range(4):
                P.tt("dve", t1[:], cc[:], cc[:], ALU.mult)
                P.tt("dve", t3[:], ss[:], ss[:], ALU.mult)
                P.tt("dve", t2[:], ss[:], cc[:], ALU.mult)
                P.tt("dve", cc[:], t1[:], t3[:], ALU.subtract)
                P.ts("dve", ss[:], t2[:], 2.0, ALU.mult)
            P.tt("dve", Lre[:, 0, :], mag[:], cc[:], ALU.mult)
            P.tt("dve", Lim[:, 0, :], mag[:], ss[:], ALU.mult)
            den = P.sb("den", [128, 16]); nr = P.sb("nr", [128, 16])
            P.tt("dve", t1[:], ar[:], ar[:], ALU.mult)
            P.tt("dve", t2[:], ai[:], ai[:], ALU.mult)
            P.tt("dve", den[:], t1[:], t2[:], ALU.add)
            P.recip(den[:], den[:])
            P.ts("dve", nr[:], Lre[:, 0, :], -1.0, ALU.add)
            P.tt("dve", t1[:], nr[:], ar[:], ALU.mult)
            P.tt("dve", t2[:], Lim[:, 0, :], ai[:], ALU.mult)
            P.tt("dve", t1[:], t1[:], t2[:], ALU.add)
            P.tt("dve", s_zre[:], t1[:], den[:], ALU.mult)
            P.tt("dve", t1[:], Lim[:, 0, :], ar[:], ALU.mult)
            P.tt("dve", t2[:], nr[:], ai[:], ALU.mult)
            P.tt("dve", t1[:], t1[:], t2[:], ALU.subtract)
            P.tt("dve", s_zim[:], t1[:], den[:], ALU.mult)
            P.ts("dve", s_zimn[:], s_zim[:], -1.0, ALU.mult)
            for k in range(1, 11):
                P.tt("dve", t1[:], Lre[:, k - 1, :], Lre[:, k - 1, :], ALU.mult)
                P.tt("dve", t2[:], Lim[:, k - 1, :], Lim[:, k - 1, :], ALU.mult)
                P.tt("dve", t3[:], Lre[:, k - 1, :], Lim[:, k - 1, :], ALU.mult)
                P.tt("dve", Lre[:, k, :], t1[:], t2[:], ALU.subtract)
                P.ts("dve", Lim[:, k, :], t3[:], 2.0, ALU.mult)
            P.ts("dve", Limn[:], Lim[:], -1.0, ALU.mult)
            P.op("dve", lambda e: e.memset(PWr.h[:, 0, :], 1.0), [], [PWr])
            P.op("dve", lambda e: e.memset(PWi.h[:, 0, :], 0.0), [], [PWi])
            for k in range(1, 9):
                cmul_s(P, PWr[:, k, :], PWi[:, k, :], PWr[:, k - 1, :], PWi[:, k - 1, :], Lre[:, 0, :], Lim[:, 0, :], t1, t2)
            for sx in range(8):
                P.cp("dve", PWDr[:, sx, :], PWr[:, 7 - sx, :])
                P.cp("dve", PWDi[:, sx, :], PWi[:, 7 - sx, :])
                P.tt("dve", t1[:], PWr[:, sx, :], PWr[:, sx, :], ALU.mult)
                P.tt("dve", t2[:], PWi[:, sx, :], PWi[:, sx, :], ALU.mult)
                P.tt("dve", t1[:], t1[:], t2[:], ALU.add)
                P.recip(t1[:], t1[:])
                P.tt("dve", IPr[:, sx, :], PWr[:, sx, :], t1[:], ALU.mult)
                P.tt("dve", t2[:], PWi[:, sx, :], t1[:], ALU.mult)
                P.ts("dve", IPi[:, sx, :], t2[:], -1.0, ALU.mult)
            P.cp("dve", LCr[:, 0, :], PWr[:, 8, :])
            P.cp("dve", LCi[:, 0, :], PWi[:, 8, :])
            for k in range(1, 8):
                cmul_s(P, LCr[:, k, :], LCi[:, k, :], LCr[:, k - 1, :], LCi[:, k - 1, :], LCr[:, k - 1, :], LCi[:, k - 1, :], t1, t2)
            P.ts("dve", LCin[:], LCi[:], -1.0, ALU.mult)
        tap("Lre", Lre, [128, 11, 16]); tap("Lim", Lim, [128, 11, 16]); tap("zre", s_zre, [128, 16]); tap("zim", s_zim, [128, 16])

        for b in range(NB):
            with P.scope():
                ohg = P.sb("ohg", [128, 4, SEQ])
                uT = P.sb("uT", [128, 4, SEQ])
                with P.scope():
                    stage1(P, I, PS, b, ident, ones, sh1T, sc1T, a2, a3, rb, mask, hgnw, lbb, omlb, ohg, uT, tap)
                if upto <= 1:
                    continue
                yT = P.sb("yT", [128, 4, SEQ])
                if True:
                    with P.scope():
                        stage2c(P, I, PS, ident, uT, yT, s_zre, s_zim, s_zimn, S5T, tap, b)
                if upto <= 2:
                    continue
                with P.scope():
                    stage3a(P, I, PS, b, ones, ohg, yT, glub, snw, mixed_d, tap)
    if upto <= 3:
        if "mixed" in taps:
            td = P.dram("tap_mixed", [TOK, D], F32, kind="ExternalOutput")
            with P.scope():
                tmp = P.sb("tmpm", [128, D])
                for i in range(TOK // 128):
                    P.dma("sp", tmp[:], mixed_d[i * 128:(i + 1) * 128, :])
                    P.dma("sp", td[i * 128:(i + 1) * 128, :], tmp[:])
        return finish(nc, P, out_d)

    with P.scope():
        stage3b(P, I, PS, ident, mod_d, mixed_d, out_d, bc_row, tap, upto)
    return finish(nc, P, out_d)


def finish(nc, P, out_d):
    P.barrier()
    P.root.close()
    return nc


def stage1(P, I, PS, b, ident, ones, sh1T, sc1T, a2, a3, rb, mask, hgnw, lbb, omlb, ohg, uT, tap):
    xin = I["x"]
    win = I["w_in"]
    hT = P.sb("hT", [128, 8, 512])
    xt = [P.sb("xt%d" % i, [128, D]) for i in range(2)]
    wbuf = [P.sb("wbuf%d" % i, [128, 8, 512]) for i in range(2)]
    qsT = P.sb("qsT", [128, 4, 512])
    gsT = P.sb("gsT", [128, 4, 512])
    lf = P.sb("lf", [128, 4, 512])
    omf = P.sb("omf", [128, 4, 512])
    vv = P.sb("vv", [128, 4, 512])
    tmpa = P.sb("tmpa", [128, 512])
    tmpb = P.sb("tmpb", [128, 512])
    S = P.sb("S", [128, 4, 128])
    Smid = P.sb("Smid", [128, 4, 128])
    Stmp = P.sb("Stmp", [128, 4, 128])
    Esb = P.sb("Esb", [128, 4, 132])
    EK = P.sb("EK", [128, 512])
    EH = P.sb("EH", [128, 512])
    Kt = P.sb("Kt", [128, 512])
    Kh = P.sb("Kh", [128, 512])
    QT = P.sb("QT", [128, 4, 128])
    KTT = P.sb("KTT", [128, 4, 128])
    ST = P.sb("ST", [128, 4, 128])
    P.op("dve", lambda e: e.memset(S.h[:], 0.0), [], [S])
    wi = 0
    for tb in range(4):
        t0 = b * SEQ + tb * 512
        for tt_ in range(4):
            xb = xt[tt_ % 2]
            P.dma("sp", xb[:], xin[t0 + tt_ * 128: t0 + (tt_ + 1) * 128, :])
            for half in range(2):
                pt = PS[half]
                for j in range(4):
                    kt = half * 4 + j
                    P.tr(pt[:, j * 128:(j + 1) * 128], xb[:, kt * 128:(kt + 1) * 128], ident[:, :])
                for j in range(4):
                    kt = half * 4 + j
                    P.act(hT[:, kt, tt_ * 128:(tt_ + 1) * 128], pt[:, j * 128:(j + 1) * 128], AF.Identity,
                          bias=sh1T[:, kt, b:b + 1], scale=sc1T[:, kt, b:b + 1])
        if tb == 0 and b == 0:
            tap("hT", hT, [128, 8, 512])
        for grp in (0, 3, 4, 1, 2):
            wb = wbuf[wi % 2]
            wi += 1
            src = V(win, win.h[:, grp * 512:(grp + 1) * 512].rearrange("(kt p) n -> p kt n", p=128))
            P.dma("sp", wb[:], src)
            if grp in (0, 3, 4):
                for ct in range(4):
                    pt = PS[2 + ct % 2]
                    for kt in range(8):
                        P.mm(pt[:, :], wb[:, kt, ct * 128:(ct + 1) * 128], hT[:, kt, :], start=(kt == 0), stop=(kt == 7))
                    if grp == 0:
                        P.act(qsT[:, ct, :], pt[:, :], AF.Silu)
                    elif grp == 3:
                        P.act(gsT[:, ct, :], pt[:, :], AF.Silu)
                    else:
                        P.cp("dve", uT[:, ct, tb * 512:(tb + 1) * 512], pt[:, :])
            else:
                for tt_ in range(4):
                    pt = PS[2 + tt_ % 2]
                    for kt in range(8):
                        P.mm(pt[:, :], hT[:, kt, tt_ * 128:(tt_ + 1) * 128], wb[:, kt, :], start=(kt == 0), stop=(kt == 7))
                    if grp == 1:
                        P.act(tmpa[:], pt[:, :], AF.Sigmoid)
                        P.tt("dve", tmpa[:], tmpa[:], omlb[:], ALU.mult)
                        P.tt("dve", tmpa[:], tmpa[:], lbb[:], ALU.add)
                        P.act(lf[:, tt_, :], tmpa[:], AF.Ln)
                        P.ts("dve", omf[:, tt_, :], tmpa[:], -1.0, ALU.mult, 1.0, ALU.add)
                    else:
                        P.cp("dve", vv[:, tt_, :], pt[:, :])
        if tb == 0 and b == 0:
            tap("qsT", qsT, [128, 4, 512]); tap("lf", lf, [128, 4, 512]); tap("vv", vv, [128, 4, 512])
        pK, pH, pB0, pB1, pT, pS, pO, pD = PS[0], PS[1], PS[2], PS[3], PS[4], PS[5], PS[6], PS[7]
        for tt_ in range(4):
            P.mm(pK[:, :], a2[:, :], lf[:, tt_, :])
            P.mm(pH[:, :], a3[:, :], lf[:, tt_, :])
            P.act(EK[:], pK[:, :], AF.Exp)
            P.act(EH[:], pH[:, :], AF.Exp)
            P.tt("dve", Kt[:], omf[:, tt_, :], EK[:], ALU.mult)
            P.tt("dve", Kh[:], omf[:, tt_, :], EH[:], ALU.mult)
            for h in range(4):
                pb = pB0 if h < 2 else pB1
                P.mm(pb[:, (h % 2) * 132:(h % 2) * 132 + 132], lf[:, tt_, h * 128:(h + 1) * 128], rb[:, :])
            P.act(Esb[:, 0:2, :], V(pB0, pB0.h[:, 0:264].rearrange("p (a b) -> p a b", a=2)), AF.Exp)
            P.act(Esb[:, 2:4, :], V(pB1, pB1.h[:, 0:264].rearrange("p (a b) -> p a b", a=2)), AF.Exp)
            P.tt("dve", QT[:], qsT[:, :, tt_ * 128:(tt_ + 1) * 128], Esb[:, :, 0:128], ALU.mult)
            for h in range(4):
                P.tr(pT[:, h * 128:(h + 1) * 128], Kt[:, h * 128:(h + 1) * 128], ident[:, :])
            P.cp("act", V(KTT, KTT.h[:].rearrange("p a b -> p (a b)")), pT[:, :])
            for h in range(4):
                P.mm(pS[:, h * 128:(h + 1) * 128], KTT[:, h, :], QT[:, h, :])
            mk = V(mask, bass.AP(mask.h, 0, [[128, 128], [0, 4], [1, 128]]))
            P.tt("dve", ST[:], V(pS, pS.h[:, :].rearrange("p (a b) -> p a b", a=4)), mk, ALU.mult)
            if tb == 0 and b == 0 and tt_ == 0:
                tap("Esb", Esb, [128, 4, 132]); tap("ST", ST, [128, 4, 128]); tap("QT", QT, [128, 4, 128]); tap("KTT", KTT, [128, 4, 128])
            for j in range(2):
                em = V(Esb, bass.AP(Esb.h, 129 + 2 * j, [[528, 128], [132, 4], [0, 128]]))
                P.tt("dve", Smid[:], S[:], em, ALU.mult)
                for h in range(4):
                    oc = pO[:, h * 128 + j * 64: h * 128 + j * 64 + 64]
                    P.mm(oc, vv[:, tt_, h * 128:(h + 1) * 128], ST[:, h, j * 64:(j + 1) * 64], start=True, stop=False)
                    P.mm(oc, Smid[:, h, :], QT[:, h, j * 64:(j + 1) * 64], start=False, stop=True)
                for h in range(4):
                    P.mm(pD[:, h * 128:(h + 1) * 128], Kh[j * 64:(j + 1) * 64, h * 128:(h + 1) * 128],
                         vv[j * 64:(j + 1) * 64, tt_, h * 128:(h + 1) * 128])
                dc = V(Esb, bass.AP(Esb.h, 128 + 2 * j, [[528, 128], [132, 4], [0, 128]]))
                P.tt("dve", Stmp[:], S[:], dc, ALU.mult)
                P.tt("dve", S[:], Stmp[:], V(pD, pD.h[:, :].rearrange("p (a b) -> p a b", a=4)), ALU.add)
            tc0 = tb * 512 + tt_ * 128
            P.cp("act", ohg[:, :, tc0:tc0 + 128], V(pO, pO.h[:, :].rearrange("p (a b) -> p a b", a=4)))
        if tb == 0 and b == 0:
            tap("oraw", ohg, [128, 4, SEQ])
        cs = slice(tb * 512, (tb + 1) * 512)
        for h in range(4):
            pn = PS[h % 2]
            P.act(tmpa[:], ohg[:, h, cs], AF.Square)
            P.mm(pn[:, :], ones[:, :], tmpa[:])
            P.act(tmpb[:], pn[:, :], AF.Sqrt, bias=RMS_EPS, scale=1.0 / 128.0)
            P.recip(tmpb[:], tmpb[:])
            P.stt("dve", tmpa[:], ohg[:, h, cs], hgnw[:, h:h + 1], tmpb[:], ALU.mult, ALU.mult)
            P.tt("dve", ohg[:, h, cs], tmpa[:], gsT[:, h, :], ALU.mult)
    if b == 0:
        tap("ohg", ohg, [128, 4, SEQ]); tap("uT", uT, [128, 4, SEQ])


def cmul_s(P, outr, outi, ar, ai, br, bi, t1, t2):
    P.tt("dve", t1[:], ar, br, ALU.mult)
    P.tt("dve", t2[:], ai, bi, ALU.mult)
    P.tt("dve", outr, t1[:], t2[:], ALU.subtract)
    P.tt("dve", t1[:], ar, bi, ALU.mult)
    P.tt("dve", t2[:], ai, br, ALU.mult)
    P.tt("dve", outi, t1[:], t2[:], ALU.add)


def stage2c(P, I, PS, ident, uT, yT, zre, zim, zimn, TB, tap, b):
    NCH = SEQ // 8
    bmr = P.sb("bmr", [128, 16, 32]); P.dma("sp", bmr[:], I["s_bmr"][:, :, :])
    bmi = P.sb("bmi", [128, 16, 32]); P.dma("sp", bmi[:], I["s_bmi"][:, :, :])
    cmr = P.sb("cmr", [128, 16, 32]); P.dma("sp", cmr[:], I["s_cmr"][:, :, :])
    cmi = P.sb("cmi", [128, 16, 32]); P.dma("sp", cmi[:], I["s_cmi"][:, :, :])
    drep = P.sb("drep", [128, 16]); P.dma("sp", drep[:], I["s_drep"][:, :])
    selj = P.sb("selj", [128, 16, 128]); P.dma("sp", selj[:], I["k_selj"][:, :, :])
    cmask = P.sb("cmask", [128, 128]); P.dma("sp", cmask[:], I["k_cmask"][:, :])
    sets = []
    for i in range(2):
        d = {}
        d["Bbr"] = P.sb("Bbr", [128, 32]); d["Bbi"] = P.sb("Bbi", [128, 32])
        for nm in ("ETr", "ETi", "G1r", "G1in", "G2r", "G2i", "Fr", "Fin", "ta", "tb"):
            d[nm] = P.sb(nm, [128, 8, 32])
        d["Esb"] = P.sb("Esb2", [128, 2, 2, 128])
        d["Msb"] = P.sb("Msb", [128, 3, 128])
        d["Usb"] = P.sb("Usb", [128, 2, NCH])
        d["XA"] = P.sb("XA", [128, 2, NCH + 1]); d["XB"] = P.sb("XB", [128, 2, NCH + 1])
        P.op("dve", lambda e, t=d["XA"]: e.memset(t.h[:], 0.0), [], [d["XA"]])
        P.op("dve", lambda e, t=d["XB"]: e.memset(t.h[:], 0.0), [], [d["XB"]])
        sets.append(d)
    pY = PS[0:4]
    pU, pW, pE, pM = PS[4], PS[5], PS[6], PS[7]

    def bc8(t, off, pstride, kstride):
        return V(t, bass.AP(t.h, off, [[pstride, 128], [kstride, 8], [0, 32]]))

    def bcx(t, off, pstride):
        return V(t, bass.AP(t.h, off, [[pstride, 128], [0, 8], [1, 32]]))

    def cm(d, outr, outi, ar, ai, br, bi, neg_im=False):
        ta, tb_ = d["ta"], d["tb"]
        P.tt("dve", ta[:], ar, br, ALU.mult)
        P.tt("dve", tb_[:], ai, bi, ALU.mult)
        P.tt("dve", outr[:], ta[:], tb_[:], ALU.subtract)
        P.tt("dve", ta[:], ar, bi, ALU.mult)
        P.tt("dve", tb_[:], ai, br, ALU.mult)
        if neg_im:
            P.stt("dve", outi[:], ta[:], -1.0, tb_[:], ALU.mult, ALU.subtract)
        else:
            P.tt("dve", outi[:], ta[:], tb_[:], ALU.add)

    for ct in range(4):
        for j in range(4):
            st = ct * 4 + j
            d = sets[st % 2]
            Bbr, Bbi = d["Bbr"], d["Bbi"]
            P.ts("dve", Bbr[:], bmi[:, st, :], zimn[:, st:st + 1], ALU.mult)
            P.stt("dve", Bbr[:], bmr[:, st, :], zre[:, st:st + 1], Bbr[:], ALU.mult, ALU.add)
            P.ts("dve", Bbi[:], bmr[:, st, :], zim[:, st:st + 1], ALU.mult)
            P.stt("dve", Bbi[:], bmi[:, st, :], zre[:, st:st + 1], Bbi[:], ALU.mult, ALU.add)
            bbr_b, bbi_b = bcx(Bbr, 0, 32), bcx(Bbi, 0, 32)
            cr_b, ci_b = bcx(cmr, st * 32, 512), bcx(cmi, st * 32, 512)
            cm(d, d["ETr"], d["ETi"], bbr_b, bbi_b, bc8(TB["PWDr"], st, 128, 16), bc8(TB["PWDi"], st, 128, 16))
            cm(d, d["G1r"], d["G1in"], bbr_b, bbi_b, bc8(TB["IPr"], st, 128, 16), bc8(TB["IPi"], st, 128, 16), neg_im=True)
            cm(d, d["G2r"], d["G2i"], cr_b, ci_b, bc8(TB["PWr"], st, 144, 16), bc8(TB["PWi"], st, 144, 16))
            cm(d, d["Fr"], d["Fin"], cr_b, ci_b, bc8(TB["PWr"], st + 16, 144, 16), bc8(TB["PWi"], st + 16, 144, 16), neg_im=True)
            fl = lambda t: t.h[:].rearrange("p a b -> p (a b)")
            for ri, nm in enumerate(("ETr", "ETi")):
                for k in range(2):
                    P.tr(pE[:, (ri * 2 + k) * 128:(ri * 2 + k + 1) * 128], V(d[nm], fl(d[nm])[:, k * 128:(k + 1) * 128]), ident[:, :])
            P.cp("act", V(d["Esb"], d["Esb"].h[:].rearrange("p a b c -> p (a b c)")), pE[:, :])
            for bi_, (k, mt) in enumerate(((0, 0), (0, 1), (1, 1))):
                oc = pM[:, bi_ * 128:(bi_ + 1) * 128]
                P.mm(oc, V(d["G1r"], fl(d["G1r"])[:, k * 128:(k + 1) * 128]), V(d["G2r"], fl(d["G2r"])[:, mt * 128:(mt + 1) * 128]), start=True, stop=False)
                P.mm(oc, V(d["G1in"], fl(d["G1in"])[:, k * 128:(k + 1) * 128]), V(d["G2i"], fl(d["G2i"])[:, mt * 128:(mt + 1) * 128]), start=False, stop=True)
            Msb = d["Msb"]
            P.tt("dve", Msb[:, 0, :], pM[:, 0:128], cmask[:, :], ALU.mult)
            P.cp("dve", Msb[:, 1, :], pM[:, 128:256])
            P.tt("dve", Msb[:, 2, :], pM[:, 256:384], cmask[:, :], ALU.mult)
            P.stt("dve", Msb[:, 0, :], ident[:, :], drep[:, st:st + 1], Msb[:, 0, :], ALU.mult, ALU.add)
            P.stt("dve", Msb[:, 2, :], ident[:, :], drep[:, st:st + 1], Msb[:, 2, :], ALU.mult, ALU.add)
            Usb = d["Usb"]
            for k in range(2):
                for sl in range(4):
                    sx = 4 * k + sl
                    rhs = V(uT, bass.AP(uT.h, ct * SEQ + sx, [[4 * SEQ, 128], [8, NCH]]))
                    P.mm(pU[:, k * NCH:(k + 1) * NCH], selj[:, j * 4 + sl, :], rhs, start=(sl == 0), stop=(sl == 3))
            P.cp("act", V(Usb, Usb.h[:].rearrange("p a b -> p (a b)")), pU[:, :])
            Esb = d["Esb"]
            for ri in range(2):
                for k in range(2):
                    P.mm(pW[:, ri * NCH:(ri + 1) * NCH], Esb[:, ri, k, :], Usb[:, k, :], start=(k == 0), stop=(k == 1))
            XA, XB = d["XA"], d["XB"]
            P.cp("dve", XA[:, :, 1:NCH + 1], V(pW, pW.h[:, :].rearrange("p (a b) -> p a b", a=2)))
            src, dst = XA, XB
            for k in range(8):
                dd = 1 << k
                lr = TB["LCr"][:, k, st:st + 1]; li = TB["LCi"][:, k, st:st + 1]; lin = TB["LCin"][:, k, st:st + 1]
                P.cp("act", dst[:, :, 1:1 + dd], src[:, :, 1:1 + dd])
                lo = slice(1, NCH + 1 - dd)
                hi = slice(1 + dd, NCH + 1)
                P.stt("dve", dst[:, 0, hi], src[:, 0, lo], lr, src[:, 0, hi], ALU.mult, ALU.add)
                P.stt("dve", dst[:, 0, hi], src[:, 1, lo], lin, dst[:, 0, hi], ALU.mult, ALU.add)
                P.stt("dve", dst[:, 1, hi], src[:, 1, lo], lr, src[:, 1, hi], ALU.mult, ALU.add)
                P.stt("dve", dst[:, 1, hi], src[:, 0, lo], li, dst[:, 1, hi], ALU.mult, ALU.add)
                src, dst = dst, src
            X = src
            pr = slice(32 * j, 32 * j + 32)
            for t in range(8):
                mt, tl = t // 4, t % 4
                oc = pY[t // 2][pr, (t % 2) * NCH:(t % 2 + 1) * NCH]
                cs_ = slice(tl * 32, (tl + 1) * 32)
                tp = (0, 32 * j)
                P.mm(oc, Msb[:, mt, cs_], Usb[:, 0, :], start=True, stop=False, tp=tp)
                if mt == 1:
                    P.mm(oc, Msb[:, 2, cs_], Usb[:, 1, :], start=False, stop=False, tp=tp)
                P.mm(oc, d["Fr"][:, t, :], X[:, 0, 0:NCH], start=False, stop=False, tp=tp)
                P.mm(oc, d["Fin"][:, t, :], X[:, 1, 0:NCH], start=False, stop=True, tp=tp)
        for t in range(8):
            dst_ = V(yT, bass.AP(yT.h, ct * SEQ + t, [[4 * SEQ, 128], [8, NCH]]))
            P.act(dst_, pY[t // 2][:, (t % 2) * NCH:(t % 2 + 1) * NCH], AF.Gelu)
    if b == 0:
        tap("ygelu", yT, [128, 4, SEQ])


def stage2_old(P, I, PS, uT, yT, s_zre, s_zim, s_zimn, Lre, Lim, Limn, sdk, tap, b):
    N = SEQ
    btr = P.sb("btr", [128, 4, 128]); P.dma("sp", btr[:], I["s_btr"][:, :, :])
    bti = P.sb("bti", [128, 4, 128]); P.dma("sp", bti[:], I["s_bti"][:, :, :])
    ctr = P.sb("ctr", [128, 16, 128]); P.dma("sp", ctr[:], I["s_ctr"][:, :, :])
    ctin = P.sb("ctin", [128, 16, 128]); P.dma("sp", ctin[:], I["s_cti"][:, :, :])
    P.ts("dve", ctin[:], ctin[:], -1.0, ALU.mult)
    X = [[P.sb("x%d%d" % (i, j), [128, N]) for j in range(2)] for i in range(2)]
    t1 = P.sb("s2t1", [128, 512])
    t2 = P.sb("s2t2", [128, 512])
    pY = PS[0:4]
    pR, pI_ = PS[4], PS[5]
    for ct in range(4):
        for j in range(4):
            st = ct * 4 + j
            pr = slice(32 * j, 32 * j + 32)
            for tb in range(4):
                cs = slice(tb * 512, (tb + 1) * 512)
                P.mm(pR[:, :], btr[pr, ct, :], uT[pr, ct, cs], tp=(32 * j, 0))
                P.mm(pI_[:, :], bti[pr, ct, :], uT[pr, ct, cs], tp=(32 * j, 0))
                P.ts("dve", t1[:], pI_[:, :], s_zimn[:, st:st + 1], ALU.mult)
                P.stt("dve", X[0][0][:, cs], pR[:, :], s_zre[:, st:st + 1], t1[:], ALU.mult, ALU.add)
                P.ts("dve", t2[:], pR[:, :], s_zim[:, st:st + 1], ALU.mult)
                P.stt("dve", X[0][1][:, cs], pI_[:, :], s_zre[:, st:st + 1], t2[:], ALU.mult, ALU.add)
            if st == 0 and b == 0:
                tap("bure", X[0][0], [128, N])
            cur = 0
            for k in range(11):
                d = 1 << k
                sr, si = X[cur]
                dr, di = X[1 - cur]
                lr = Lre[:, k, st:st + 1]
                li = Lim[:, k, st:st + 1]
                lin = Limn[:, k, st:st + 1]
                P.cp("act", dr[:, 0:d], sr[:, 0:d])
                P.cp("act", di[:, 0:d], si[:, 0:d])
                P.stt("dve", dr[:, d:N], sr[:, 0:N - d], lr, sr[:, d:N], ALU.mult, ALU.add)
                P.stt("dve", dr[:, d:N], si[:, 0:N - d], lin, dr[:, d:N], ALU.mult, ALU.add)
                P.stt("dve", di[:, d:N], si[:, 0:N - d], lr, si[:, d:N], ALU.mult, ALU.add)
                P.stt("dve", di[:, d:N], sr[:, 0:N - d], li, di[:, d:N], ALU.mult, ALU.add)
                cur = 1 - cur
            xr, xi = X[cur]
            if st == 0 and b == 0:
                tap("xre", xr, [128, N])
            for tb in range(4):
                cs = slice(tb * 512, (tb + 1) * 512)
                P.mm(pY[tb][:, :], ctr[:, st, :], xr[:, cs], start=(j == 0), stop=False)
                P.mm(pY[tb][:, :], ctin[:, st, :], xi[:, cs], start=False, stop=(j == 3))
        for tb in range(4):
            cs = slice(tb * 512, (tb + 1) * 512)
            P.stt("dve", t1[:], uT[:, ct, cs], sdk[:, ct:ct + 1], pY[tb][:, :], ALU.mult, ALU.add)
            P.act(yT[:, ct, cs], t1[:], AF.Gelu)
    if b == 0:
        tap("ygelu", yT, [128, 4, SEQ])


def stage3a(P, I, PS, b, ones, ohg, yT, glub, snw, mixed_d, tap):
    gluw = P.sb("gluw", [128, 4, 512])
    gw = I["glu_w"]
    P.dma("sp", gluw[:], V(gw, gw.h[:, :].rearrange("(kt p) n -> p kt n", p=128)))
    wout = P.sb("wout", [128, 8, D])
    wo = I["w_out"]
    P.dma("sp", wout[:], V(wo, wo.h[:, :].rearrange("(kt p) n -> p kt n", p=128)))
    y2 = P.sb("y2", [128, 4, 512])
    sq = P.sb("sq", [128, 512])
    rstd = P.sb("rstd", [128, 512])
    sg = P.sb("sg", [128, 512])
    mt = [P.sb("mt%d" % i, [128, D]) for i in range(2)]
    for tb in range(4):
        cs = slice(tb * 512, (tb + 1) * 512)
        pn = PS[2]
        for c2 in range(4):
            pg = PS[c2 % 2]
            for kt in range(4):
                P.mm(pg[:, :], gluw[:, kt, c2 * 128:(c2 + 1) * 128], yT[:, kt, cs], start=(kt == 0), stop=(kt == 3))
            P.act(sg[:], pg[:, :], AF.Sigmoid, bias=glub[:, c2:c2 + 1])
            P.tt("dve", y2[:, c2, :], yT[:, c2, cs], sg[:], ALU.mult)
            P.act(sq[:], y2[:, c2, :], AF.Square)
            P.mm(pn[:, :], ones[:, :], sq[:], start=(c2 == 0), stop=(c2 == 3))
        P.act(rstd[:], pn[:, :], AF.Sqrt, bias=RMS_EPS, scale=1.0 / 512.0)
        P.recip(rstd[:], rstd[:])
        for c2 in range(4):
            P.stt("dve", yT[:, c2, cs], y2[:, c2, :], snw[:, c2:c2 + 1], rstd[:], ALU.mult, ALU.mult)
        for tt_ in range(4):
            tcs = slice(tb * 512 + tt_ * 128, tb * 512 + (tt_ + 1) * 128)
            m = mt[tt_ % 2]
            for nb in range(2):
                pm = PS[3 + nb]
                for ft in range(8):
                    l = ohg[:, ft, tcs] if ft < 4 else yT[:, ft - 4, tcs]
                    P.mm(pm[:, :], l, wout[:, ft, nb * 512:(nb + 1) * 512], start=(ft == 0), stop=(ft == 7))
                P.cp("act" if nb == 0 else "dve", m[:, nb * 512:(nb + 1) * 512], pm[:, :])
            r0 = b * SEQ + tb * 512 + tt_ * 128
            P.dma("sp", mixed_d[r0:r0 + 128, :], m[:], src_sem=True)
    if b == 0:
        tap("ossm", yT, [128, 4, SEQ])


def layer_norm_tile(P, v, outt, stats, mv, sc, w_bc, b_bc):
    for c in range(2):
        P.op("dve", lambda e, c=c: e.bn_stats(out=stats.h[:, c, :], in_=v.h[:, c * 512:(c + 1) * 512]), [v], [stats])
    P.op("dve", lambda e: e.bn_aggr(out=mv.h[:, :], in_=stats.h[:].rearrange("p a b -> p (a b)")), [stats], [mv])
    P.ts("dve", sc[:, 0:1], mv[:, 1:2], LN_EPS, ALU.add)
    P.act(sc[:, 0:1], sc[:, 0:1], AF.Sqrt)
    P.recip(sc[:, 0:1], sc[:, 0:1])
    P.stt("dve", sc[:, 1:2], mv[:, 0:1], -1.0, sc[:, 0:1], ALU.mult, ALU.mult)
    yield
    P.act(v[:], v[:], AF.Identity, bias=sc[:, 1:2], scale=sc[:, 0:1])
    P.tt("dve", v[:], v[:], w_bc[:], ALU.mult)
    yield
    P.tt("dve", outt[:], v[:], b_bc[:], ALU.add)
    yield


def stage3b(P, I, PS, ident, mod_d, mixed_d, out_d, bc_row, tap, upto):
    xin = I["x"]
    wq_d = I["w_q"]
    pu, pv = I["pu"], I["pv"]
    NT = TOK // 128
    RB1 = [P.sb("rb_%d" % q, [128, D]) for q in range(4)]
    sel = P.sb("sel3", [2, 2, 128]); P.dma("sp", sel[:], I["k_sel"][:, :, :])
    modq = P.sb("modq", [2, 1024])
    lnw = []
    for nm in ("ln1w", "ln1b", "ln2w", "ln2b"):
        t = P.sb(nm, [128, D])
        P.dma("sp", t[:], bc_row(nm))
        lnw.append(t)
    keysT = P.sb("keysT", [128, 16, 128]); P.dma("sp", keysT[:], I["keysT"][:, :, :])
    iota = P.sb("iota", [128, 16]); P.dma("sp", iota[:], I["k_iota"][:, :])
    xt = P.sb("xt3", [128, D]); mt = P.sb("mt3", [128, D]); vtf = P.sb("vtf", [128, D])
    statsf = P.sb("statsf", [128, 2, 6]); mvf = P.sb("mvf", [128, 2]); scf = P.sb("scf", [128, 2])
    h2T = P.sb("h2T", [128, 8, 128])
    wqb = [P.sb("wqb%d" % i, [128, 8, 128]) for i in range(2)]
    qT = P.sb("qT", [128, 16, 128])
    scs = P.sb("scs", [128, 16, 128]); scs2 = P.sb("scs2", [128, 16, 128])
    top = P.sb("top", [128, 16, 16])
    idx = P.sb("idx", [128, 16, 16], U32)
    idxf = P.sb("idxf", [128, 16, 16])
    cand = P.sb("cand", [128, 8, 256])
    best = P.sb("best", [128, 8, 16])
    pos = P.sb("pos", [128, 8, 16], U32)
    pi_ = P.sb("pi", [128, 8, 16], U32); pj_ = P.sb("pj", [128, 8, 16], U32)
    pif = P.sb("pif", [128, 8, 16]); pjf = P.sb("pjf", [128, 8, 16])
    oh = P.sb("oh", [128, 8, 16, 16])
    i1 = P.sb("i1", [128, 8, 16]); i2 = P.sb("i2", [128, 8, 16])
    eidf = P.sb("eidf", [128, 128]); gsum = P.sb("gsum", [128, 8])
    x1s = [P.sb("x1_%d" % i, [128, D]) for i in range(2)]
    h2s = [P.sb("h2_%d" % i, [128, D]) for i in range(2)]
    eids = [P.sb("eid_%d" % i, [128, 128], I32) for i in range(2)]
    gates = [P.sb("gate_%d" % i, [128, 8, 16]) for i in range(2)]
    vtg = P.sb("vtg", [128, D]); acc = P.sb("acc", [128, D]); ot = P.sb("ot", [128, D])
    acc2 = P.sb("acc2", [128, D])
    statsg = P.sb("statsg", [128, 2, 6]); mvg = P.sb("mvg", [128, 2]); scg = P.sb("scg", [128, 2])
    z = P.sb("z", [128, 128]); a_ = P.sb("a", [128, 128])
    NBUF = 6
    dgs = [P.sb("dg%d" % i, [128, 128]) for i in range(4)]
    gb = [P.sb("gb%d" % i, [128, D]) for i in range(2 * NBUF)]
    gi = [0]

    def nextbuf():
        t = gb[gi[0] % (2 * NBUF)]
        gi[0] += 1
        return t
    wi = [0]

    def front(ti):
        S = ti % 2
        x1, h2, eid, gate = x1s[S], h2s[S], eids[S], gates[S]
        b = ti // (SEQ // 128)
        r0 = ti * 128
        if ti % (SEQ // 128) == 0:
            for q, (off, plus1) in enumerate([(2048, True), (3072, False), (4096, True), (5120, True)]):
                P.dma("sp", modq[:], mod_d[:, off:off + 1024])
                for hb_ in range(2):
                    pt = PS[hb_]
                    P.mm(pt[:, :], sel[:, b, :], modq[:, hb_ * 512:(hb_ + 1) * 512])
                    dst = RB1[q][:, hb_ * 512:(hb_ + 1) * 512]
                    if plus1:
                        P.ts("dve", dst, pt[:, :], 1.0, ALU.add)
                    else:
                        P.cp("dve", dst, pt[:, :])
                    yield
        g1p, sh2, sc2p, g2p = RB1
        P.dma("sp", xt[:], xin[r0:r0 + 128, :])
        P.dma("sp", mt[:], mixed_d[r0:r0 + 128, :])
        P.tt("dve", vtf[:], mt[:], g1p[:], ALU.mult)
        yield
        P.stt("dve", vtf[:], xt[:], ALPHA, vtf[:], ALU.mult, ALU.add)
        yield
        for _ in layer_norm_tile(P, vtf, x1, statsf, mvf, scf, lnw[0], lnw[1]):
            yield
        P.tt("dve", h2[:], x1[:], sc2p[:], ALU.mult)
        yield
        P.tt("dve", h2[:], h2[:], sh2[:], ALU.add)
        yield
        if ti == 0:
            tap("x1", x1, [128, D]); tap("h2", h2, [128, D])
        for half in range(2):
            pt = PS[half]
            for j in range(4):
                kt = half * 4 + j
                P.tr(pt[:, j * 128:(j + 1) * 128], h2[:, kt * 128:(kt + 1) * 128], ident[:, :])
            P.cp("act", V(h2T, h2T.h[:, half * 4:(half + 1) * 4, :].rearrange("p a b -> p (a b)")), pt[:, :])
            yield
        for cj in range(16):
            wb = wqb[wi[0] % 2]
            wi[0] += 1
            P.dma("sp", wb[:], V(wq_d, wq_d.h[:, cj * 128:(cj + 1) * 128].rearrange("(kt p) n -> p kt n", p=128)))
            pq = PS[2 + (cj // 4) % 2]
            for kt in range(8):
                P.mm(pq[:, (cj % 4) * 128:(cj % 4 + 1) * 128], wb[:, kt, :], h2T[:, kt, :], start=(kt == 0), stop=(kt == 7))
                if kt % 2 == 1:
                    yield
            if cj % 4 == 3:
                g4 = cj // 4
                P.cp("act", V(qT, qT.h[:, g4 * 4:(g4 + 1) * 4, :].rearrange("p a b -> p (a b)")), pq[:, :])
        for g4 in range(4):
            psc = PS[4 + g4 % 2]
            for jj in range(4):
                cj = g4 * 4 + jj
                P.mm(psc[:, jj * 128:(jj + 1) * 128], qT[:, cj, :], keysT[:, cj, :])
            P.cp("act" if g4 % 2 else "dve", V(scs, scs.h[:, g4 * 4:(g4 + 1) * 4, :].rearrange("p a b -> p (a b)")), psc[:, :])
            yield
        if ti == 0:
            tap("scs", scs, [128, 16, 128])
        for cj in range(16):
            P.op("dve", lambda e, cj=cj: e.max(out=top.h[:, cj, 0:8], in_=scs.h[:, cj, :]), [scs], [top])
            P.op("dve", lambda e, cj=cj: e.match_replace(out=scs2.h[:, cj, :], in_to_replace=top.h[:, cj, 0:8],
                                                         in_values=scs.h[:, cj, :], imm_value=NEG), [scs, top], [scs2])
            yield
            P.op("dve", lambda e, cj=cj: e.max(out=top.h[:, cj, 8:16], in_=scs2.h[:, cj, :]), [scs2], [top])
            P.op("dve", lambda e, cj=cj: e.max_index(out=idx.h[:, cj, 0:8], in_max=top.h[:, cj, 0:8], in_values=scs.h[:, cj, :]), [scs, top], [idx])
            yield
            P.op("dve", lambda e, cj=cj: e.max_index(out=idx.h[:, cj, 8:16], in_max=top.h[:, cj, 8:16], in_values=scs.h[:, cj, :]), [scs, top], [idx])
            yield
        P.cp("dve", idxf[:], idx[:])
        in0 = V(top, bass.AP(top.h, 0, [[256, 128], [32, 8], [1, 16], [0, 16]]))
        in1 = V(top, bass.AP(top.h, 16, [[256, 128], [32, 8], [0, 16], [1, 16]]))
        P.tt("dve", V(cand, cand.h[:].rearrange("p h (i j) -> p h i j", i=16)), in0, in1, ALU.add)
        yield
        for h in range(8):
            c2v = lambda h=h: scs2.h[:, 2 * h:2 * h + 2, :].rearrange("p a b -> p (a b)")
            P.op("dve", lambda e, h=h: e.max(out=best.h[:, h, 0:8], in_=cand.h[:, h, :]), [cand], [best])
            P.op("dve", lambda e, h=h: e.match_replace(out=c2v(h), in_to_replace=best.h[:, h, 0:8],
                                                       in_values=cand.h[:, h, :], imm_value=NEG), [cand, best], [scs2])
            yield
            P.op("dve", lambda e, h=h: e.max(out=best.h[:, h, 8:16], in_=c2v(h)), [scs2], [best])
            P.op("dve", lambda e, h=h: e.max_index(out=pos.h[:, h, 0:8], in_max=best.h[:, h, 0:8], in_values=cand.h[:, h, :]), [cand, best], [pos])
            yield
            P.op("dve", lambda e, h=h: e.max_index(out=pos.h[:, h, 8:16], in_max=best.h[:, h, 8:16], in_values=cand.h[:, h, :]), [cand, best], [pos])
            yield
        P.op("dve", lambda e: e.tensor_single_scalar(out=pi_.h[:], in_=pos.h[:], scalar=4, op=ALU.logical_shift_right), [pos], [pi_])
        P.op("dve", lambda e: e.tensor_single_scalar(out=pj_.h[:], in_=pos.h[:], scalar=15, op=ALU.bitwise_and), [pos], [pj_])
        yield
        P.cp("dve", pif[:], pi_[:])
        P.cp("dve", pjf[:], pj_[:])
        yield
        io = V(iota, bass.AP(iota.h, 0, [[16, 128], [0, 8], [0, 16], [1, 16]]))
        for (pf, which, dst) in ((pif, 0, i1), (pjf, 1, i2)):
            pfb = V(pf, bass.AP(pf.h, 0, [[128, 128], [16, 8], [1, 16], [0, 16]]))
            P.tt("dve", oh[:], pfb, io, ALU.is_equal)
            yield
            ixb = V(idxf, bass.AP(idxf.h, 16 * which, [[256, 128], [32, 8], [0, 16], [1, 16]]))
            P.tt("dve", oh[:], oh[:], ixb, ALU.mult)
            yield
            P.op("dve", lambda e, dst=dst: e.tensor_reduce(out=dst.h[:], in_=oh.h[:], axis=AX.X, op=ALU.add), [oh], [dst])
            yield
        P.stt("dve", eidf[:], V(i1, i1.h[:].rearrange("p a b -> p (a b)")), 128.0, V(i2, i2.h[:].rearrange("p a b -> p (a b)")), ALU.mult, ALU.add)
        P.cp("dve", eid[:], eidf[:])
        yield
        b0 = V(best, bass.AP(best.h, 0, [[128, 128], [16, 8], [0, 16]]))
        P.tt("dve", gate[:], best[:], b0, ALU.subtract)
        P.act(gate[:], gate[:], AF.Exp)
        yield
        P.op("dve", lambda e: e.tensor_reduce(out=gsum.h[:], in_=gate.h[:], axis=AX.X, op=ALU.add), [gate], [gsum])
        P.recip(gsum[:], gsum[:])
        gs = V(gsum, bass.AP(gsum.h, 0, [[8, 128], [1, 8], [0, 16]]))
        P.tt("dve", gate[:], gate[:], gs, ALU.mult)
        yield
        if ti == 0:
            tap("eidf", eidf, [128, 128]); tap("gate", gate, [128, 8, 16])

    def adv(g, n):
        if g is None:
            return None
        for _ in range(n):
            try:
                next(g)
            except StopIteration:
                return None
        return g

    def gather(ti, fg):
        S = ti % 2
        x1, h2, eid, gate = x1s[S], h2s[S], eids[S], gates[S]
        r0 = ti * 128
        g2p = RB1[3]
        for s in range(128):
            u = nextbuf()
            P.dma("pool", u[:], V(pu, pu.h[:, :]),
                  fn=lambda e, u=u, s=s: e.indirect_dma_start(out=u.h[:], out_offset=None, in_=pu.h[:, :],
                                                             in_offset=bass.IndirectOffsetOnAxis(ap=eid.h[:, s:s + 1], axis=0)),
                  extra=[eid])
            P.op("dve", lambda e, u=u, s=s: e.scalar_tensor_tensor(out=ot.h[:], in0=u.h[:], scalar=1.0, in1=h2.h[:],
                                                                 op0=ALU.mult, op1=ALU.mult, accum_out=z.h[:, s:s + 1]), [u, h2], [ot, z])
            fg = adv(fg, 1)
        P.act(a_[:], z[:], AF.Gelu)
        P.tt("dve", a_[:], a_[:], V(gate, gate.h[:].rearrange("p a b -> p (a b)")), ALU.mult)
        for s in range(128):
            v = nextbuf()
            P.dma("pool", v[:], V(pv, pv.h[:, :]),
                  fn=lambda e, v=v, s=s: e.indirect_dma_start(out=v.h[:], out_offset=None, in_=pv.h[:, :],
                                                             in_offset=bass.IndirectOffsetOnAxis(ap=eid.h[:, s:s + 1], axis=0)),
                  extra=[eid])
            if s % 2 == 1:
                if s == 1:
                    P.ts("dve", acc2[:], v[:], a_[:, s:s + 1], ALU.mult)
                else:
                    P.stt("dve", acc2[:], v[:], a_[:, s:s + 1], acc2[:], ALU.mult, ALU.add)
            else:
                dg = dgs[s % 4]
                P.act(dg[:], ident[:, :], AF.Copy, scale=a_[:, s:s + 1])
                for hf in range(2):
                    P.mm(PS[6 + hf][:, :], dg[:], v[:, hf * 512:(hf + 1) * 512], start=(s == 0), stop=(s == 126))
            fg = adv(fg, 2)
        fg = adv(fg, 100000)
        P.tt("dve", acc[:, 0:512], PS[6][:, :], acc2[:, 0:512], ALU.add)
        P.tt("dve", acc[:, 512:1024], PS[7][:, :], acc2[:, 512:1024], ALU.add)
        if ti == 0:
            tap("ffn", acc, [128, D]); tap("z", z, [128, 128])
        P.tt("dve", vtg[:], acc[:], g2p[:], ALU.mult)
        P.stt("dve", vtg[:], x1[:], ALPHA, vtg[:], ALU.mult, ALU.add)
        for _ in layer_norm_tile(P, vtg, ot, statsg, mvg, scg, lnw[2], lnw[3]):
            pass
        P.dma("sp", out_d[r0:r0 + 128, :], ot[:], src_sem=True)

    adv(front(0), 100000)
    for ti in range(NT):
        nxt = ti + 1
        if nxt < NT and nxt % (SEQ // 128) != 0:
            gather(ti, front(nxt))
        else:
            gather(ti, None)
            if nxt < NT:
                adv(front(nxt), 100000)


_CACHE = {}


def kernel(**inputs):
    shared = _prep_shared(inputs)
    x = np.ascontiguousarray(inputs["x"], dtype=np.float32)
    c = np.ascontiguousarray(inputs["c"], dtype=np.float32)
    in_maps = []
    for core in range(NCORES):
        m = dict(shared)
        m["x"] = x[core * NB:(core + 1) * NB].reshape(TOK, D)
        m["cT"] = np.ascontiguousarray(c[core * NB:(core + 1) * NB].T.reshape(8, 128, NB).transpose(1, 0, 2))
        in_maps.append(m)
    nc = build()
    res = run_bass_kernel_spmd(nc, in_maps, core_ids=list(range(NCORES)))
    out = np.concatenate([r["out"].reshape(NB, SEQ, D) for r in res.results], axis=0)
    return out.astype(np.float32)
```

```python
import math
from contextlib import ExitStack, contextmanager
import numpy as np
import concourse.bass as bass
import concourse.mybir as mybir
from concourse.bass_utils import run_bass_kernel_spmd

F32 = mybir.dt.float32
I32 = mybir.dt.int32
U32 = mybir.dt.uint32
AF = mybir.ActivationFunctionType
ALU = mybir.AluOpType
AX = mybir.AxisListType

NCORES = 8
D = 1024
SEQ = 2048
NB = 2
TOK = NB * SEQ
ALPHA = 2.0 ** 0.25
LN_EPS = 1e-5
RMS_EPS = 1e-6
MID = 31
NEG = -1.0e30


class V:
    __slots__ = ("t", "ap")

    def __init__(self, t, ap):
        self.t = t
        self.ap = ap


class T:
    def __init__(self, h, name):
        self.h = h
        self.name = name
        self.w = None
        self.wd = {}
        self.r = {}
        self.dsem = None
        self.dcnt = 0

    def __getitem__(self, idx):
        return V(self, self.h[idx])

    def cust(self, offset, dims):
        return V(self, bass.AP(self.h, offset, [list(d) for d in dims]))


class Prog:
    def __init__(self, nc):
        self.nc = nc
        self.root = ExitStack()
        self.stacks = [self.root]
        self.E = {"pe": nc.tensor, "act": nc.scalar, "dve": nc.vector, "pool": nc.gpsimd, "sp": nc.sync}
        self.sem = {k: self.root.enter_context(nc.semaphore("s_" + k)) for k in ("pe", "act", "dve", "pool")}
        self.cnt = {k: 0 for k in self.sem}
        self.waited = {k: {} for k in self.E}
        self.dma_tiles = []
        self.nname = 0

    def sb(self, name, shape, dt=F32):
        self.nname += 1
        h = self.stacks[-1].enter_context(self.nc.sbuf_tensor("%s_%d" % (name, self.nname), list(shape), dt))
        return T(h, name)

    def ps(self, name):
        self.nname += 1
        h = self.stacks[-1].enter_context(self.nc.psum_tensor("%s_%d" % (name, self.nname), [128, 512], F32))
        return T(h, name)

    def dram(self, name, shape, dt=F32, kind="Internal"):
        h = self.nc.dram_tensor(name, list(shape), dt, kind=kind)
        return T(h, name)

    @contextmanager
    def scope(self):
        st = ExitStack()
        self.stacks.append(st)
        try:
            yield
        finally:
            self.barrier()
            self.stacks.pop()
            st.close()

    def _wait(self, eng, evs):
        need = {}
        for ev in evs:
            if ev is None:
                continue
            s, v = ev
            k = id(s)
            if k not in need or need[k][1] < v:
                need[k] = (s, v)
        for k, (s, v) in need.items():
            if eng == "pe" and s is self.sem["pe"]:
                continue
            if self.waited[eng].get(k, 0) >= v:
                continue
            self.E[eng].wait_ge(s, v)
            self.waited[eng][k] = v

    def barrier(self):
        evs = [(self.sem[k], self.cnt[k]) for k in self.sem if self.cnt[k] > 0]
        evs += [(t.dsem, t.dcnt) for t in self.dma_tiles if t.dcnt > 0]
        for eng in self.E:
            self._wait(eng, evs)

    def op(self, eng, fn, reads=(), writes=()):
        evs = []
        for t in reads:
            evs.append(t.w)
            evs.extend(t.wd.values())
        for t in writes:
            evs.append(t.w)
            evs.extend(t.wd.values())
            evs.extend(t.r.values())
        self._wait(eng, evs)
        inst = fn(self.E[eng])
        self.cnt[eng] += 1
        inst.then_inc(self.sem[eng], 1)
        ev = (self.sem[eng], self.cnt[eng])
        for t in writes:
            t.w = ev
            t.wd = {}
            t.r = {}
        for t in reads:
            if t not in writes:
                t.r[eng] = ev
        return inst

    def dma(self, q, o, i, fn=None, extra=(), src_sem=False):
        ot, it = o.t, i.t
        st = it if src_sem else ot
        evs = [it.w] + list(it.wd.values()) + [t.w for t in extra]
        if ot.w is not None and not (st.dsem is not None and ot.w[0] is st.dsem):
            evs.append(ot.w)
        for k, ev in ot.wd.items():
            if not (st.dsem is not None and ev[0] is st.dsem):
                evs.append(ev)
        evs.extend(ot.r.values())
        self._wait(q, evs)
        if st.dsem is None:
            self.nname += 1
            st.dsem = self.root.enter_context(self.nc.semaphore("d%d" % self.nname))
            self.dma_tiles.append(st)
        if fn is None:
            inst = self.E[q].dma_start(out=o.ap, in_=i.ap)
        else:
            inst = fn(self.E[q])
        st.dcnt += 16
        inst.then_inc(st.dsem, 16)
        ev = (st.dsem, st.dcnt)
        if src_sem:
            ot.wd[id(st.dsem)] = ev
        else:
            ot.w = ev
            ot.wd = {}
        ot.r = {}
        it.r["d%d" % id(st)] = ev
        for t in extra:
            t.r["d%d" % id(st)] = ev
        return inst

    @staticmethod
    def _sv(x, reads):
        if isinstance(x, V):
            reads.append(x.t)
            return x.ap
        return x

    def tt(self, eng, o, a, b, op):
        return self.op(eng, lambda e: e.tensor_tensor(out=o.ap, in0=a.ap, in1=b.ap, op=op), [a.t, b.t], [o.t])

    def ts(self, eng, o, a, s1, op0, s2=None, op1=None):
        reads = [a.t]
        s1a = self._sv(s1, reads)
        s2a = self._sv(s2, reads)
        if op1 is None:
            return self.op(eng, lambda e: e.tensor_scalar(out=o.ap, in0=a.ap, scalar1=s1a, scalar2=None, op0=op0), reads, [o.t])
        return self.op(eng, lambda e: e.tensor_scalar(out=o.ap, in0=a.ap, scalar1=s1a, scalar2=s2a, op0=op0, op1=op1), reads, [o.t])

    def stt(self, eng, o, a, s, b, op0, op1):
        reads = [a.t, b.t]
        sa = self._sv(s, reads)
        return self.op(eng, lambda e: e.scalar_tensor_tensor(out=o.ap, in0=a.ap, scalar=sa, in1=b.ap, op0=op0, op1=op1), reads, [o.t])

    def cp(self, eng, o, a):
        if eng == "act":
            return self.op(eng, lambda e: e.copy(out=o.ap, in_=a.ap), [a.t], [o.t])
        return self.op(eng, lambda e: e.tensor_copy(out=o.ap, in_=a.ap), [a.t], [o.t])

    def act(self, o, a, func, bias=None, scale=None, accum=None):
        reads = [a.t]
        kw = {}
        if bias is not None:
            kw["bias"] = self._sv(bias, reads)
        if scale is not None:
            kw["scale"] = self._sv(scale, reads)
        writes = [o.t]
        if accum is not None:
            kw["accum_out"] = accum.ap
            writes.append(accum.t)
        return self.op("act", lambda e: e.activation(out=o.ap, in_=a.ap, func=func, **kw), reads, writes)

    def mm(self, o, l, r, start=True, stop=True, tp=None):
        if tp is None:
            return self.op("pe", lambda e: e.matmul(o.ap, l.ap, r.ap, start=start, stop=stop), [l.t, r.t], [o.t])
        return self.op("pe", lambda e: e.matmul(o.ap, l.ap, r.ap, start=start, stop=stop, tile_position=tp), [l.t, r.t], [o.t])

    def tr(self, o, a, ident):
        return self.op("pe", lambda e: e.transpose(o.ap, a.ap, ident.ap), [a.t, ident.t], [o.t])

    def recip(self, o, a):
        return self.op("dve", lambda e: e.reciprocal(out=o.ap, in_=a.ap), [a.t], [o.t])


def _consts():
    s = np.arange(64)[:, None]
    t = np.arange(64)[None, :]
    le = (s <= t).astype(np.float32)
    lm = (s <= MID).astype(np.float32) * np.ones((1, 64), np.float32)
    A1 = le - lm
    A2 = lm - le
    A3 = (s > t).astype(np.float32)
    z = np.zeros((64, 64), np.float32)
    bd = lambda a: np.block([[a, z], [z, a]]).astype(np.float32)
    RB = np.zeros((128, 132), np.float32)
    RB[0:64, 0:64] = A1
    RB[64:128, 64:128] = A1
    RB[0:64, 128] = 1.0
    RB[0:64, 129] = lm[:, 0]
    RB[64:128, 130] = 1.0
    RB[64:128, 131] = lm[:, 0]
    sel = np.zeros((2, 2, 128), np.float32)
    sel[0, 0, :] = 1.0
    sel[1, 1, :] = 1.0
    return {
        "k_ident": np.eye(128, dtype=np.float32),
        "k_a2": bd(A2),
        "k_a3": bd(A3),
        "k_rb": RB,
        "k_mask": bd(le),
        "k_iota": np.broadcast_to(np.arange(16, dtype=np.float32), (128, 16)).copy(),
        "k_ones": np.ones((128, 128), np.float32),
        "k_sel": sel,
        "k_selj": _selj(),
        "k_cmask": np.kron((np.arange(4)[:, None] <= np.arange(4)[None, :]).astype(np.float32), np.ones((32, 32), np.float32)),
    }


def _selj():
    a = np.zeros((128, 16, 128), np.float32)
    for j in range(4):
        for sl in range(4):
            for c in range(32):
                a[32 * j + c, j * 4 + sl, 32 * sl + c] = 1.0
    return a


def _prep_shared(inp):
    f = lambda a: np.ascontiguousarray(a, dtype=np.float32)
    o = {}
    o["ada_w"] = f(inp["ada_w"][0])
    o["ada_b2"] = f(np.broadcast_to(inp["ada_b"][0][None, :], (2, 6 * D)))
    o["w_in"] = f(inp["w_in"][0])
    o["hb"] = f(inp["hg_lower_bound"])
    col = lambda v, n: f(np.asarray(v).reshape(n, 128).T)
    o["hg_nw"] = col(inp["hg_norm_w"][0], 4)
    o["s_ar"] = col(inp["ssm_a_re"][0], 16)
    o["s_ai"] = col(inp["ssm_a_im"][0], 16)
    o["s_ldt"] = col(np.repeat(inp["ssm_log_dt"][0], 64), 16)
    bre, bim = inp["ssm_b_re"][0], inp["ssm_b_im"][0]
    cre, cim = inp["ssm_c_re"][0], inp["ssm_c_im"][0]
    BTr = np.zeros((512, 128), np.float32)
    BTi = np.zeros((512, 128), np.float32)
    CTr = np.zeros((16, 128, 128), np.float32)
    CTi = np.zeros((16, 128, 128), np.float32)
    for g in range(32):
        gl = g % 2
        st = g // 2
        j = st % 4
        BTr[g * 16:(g + 1) * 16, gl * 64:gl * 64 + 64] = bre[g].T
        BTi[g * 16:(g + 1) * 16, gl * 64:gl * 64 + 64] = bim[g].T
        CTr[st, gl * 64:gl * 64 + 64, 32 * j + gl * 16:32 * j + gl * 16 + 16] = cre[g].T
        CTi[st, gl * 64:gl * 64 + 64, 32 * j + gl * 16:32 * j + gl * 16 + 16] = cim[g].T
    o["s_btr"] = f(BTr.reshape(4, 128, 128).transpose(1, 0, 2))
    o["s_bti"] = f(BTi.reshape(4, 128, 128).transpose(1, 0, 2))
    o["s_ctr"] = f(CTr.transpose(1, 0, 2))
    o["s_cti"] = f(CTi.transpose(1, 0, 2))
    o["s_d"] = col(inp["ssm_d"][0].reshape(512), 4)
    BM_r = np.zeros((16, 128, 32), np.float32); BM_i = np.zeros((16, 128, 32), np.float32)
    CM_r = np.zeros((16, 128, 32), np.float32); CM_i = np.zeros((16, 128, 32), np.float32)
    for g in range(32):
        gl = g % 2
        st = g // 2
        BM_r[st, gl * 64:gl * 64 + 64, gl * 16:gl * 16 + 16] = bre[g]
        BM_i[st, gl * 64:gl * 64 + 64, gl * 16:gl * 16 + 16] = bim[g]
        CM_r[st, gl * 64:gl * 64 + 64, gl * 16:gl * 16 + 16] = cre[g].T
        CM_i[st, gl * 64:gl * 64 + 64, gl * 16:gl * 16 + 16] = cim[g].T
    o["s_bmr"] = f(BM_r.transpose(1, 0, 2)); o["s_bmi"] = f(BM_i.transpose(1, 0, 2))
    o["s_cmr"] = f(CM_r.transpose(1, 0, 2)); o["s_cmi"] = f(CM_i.transpose(1, 0, 2))
    dd = inp["ssm_d"][0].reshape(16, 32)
    o["s_drep"] = f(np.tile(dd, (1, 4)).T)
    o["glu_w"] = f(inp["ssm_glu_w"][0])
    o["glu_b"] = col(inp["ssm_glu_b"][0], 4)
    o["s_nw"] = col(inp["ssm_norm_w"][0], 4)
    o["w_out"] = f(inp["w_out"][0])
    o["ln1w"] = f(inp["ln1_w"])
    o["ln1b"] = f(inp["ln1_b"])
    o["ln2w"] = f(inp["ln2_w"])
    o["ln2b"] = f(inp["ln2_b"])
    o["w_q"] = f(inp["peer_w_q"][0])
    o["keysT"] = f(inp["peer_sub_keys"][0].transpose(3, 0, 1, 2).reshape(128, 16, 128))
    o["pu"] = f(inp["peer_u"][0])
    o["pv"] = f(inp["peer_v"][0])
    o.update(_consts())
    return o


SHAPES = {
    "x": [TOK, D], "cT": [128, 8, 2], "ada_w": [D, 6 * D], "ada_b2": [2, 6 * D], "w_in": [D, 2560], "hb": [2, 512],
    "hg_nw": [128, 4], "s_ar": [128, 16], "s_ai": [128, 16], "s_ldt": [128, 16], "s_btr": [128, 4, 128],
    "s_bti": [128, 4, 128], "s_ctr": [128, 16, 128], "s_cti": [128, 16, 128], "s_d": [128, 4], "glu_w": [512, 512],
    "glu_b": [128, 4], "s_nw": [128, 4], "w_out": [D, D], "ln1w": [1, D], "ln1b": [1, D], "ln2w": [1, D],
    "ln2b": [1, D], "w_q": [D, 2048], "keysT": [128, 16, 128], "pu": [16384, D], "pv": [16384, D],
    "k_ident": [128, 128], "k_a2": [128, 128], "k_a3": [128, 128], "k_rb": [128, 132], "k_mask": [128, 128],
    "k_iota": [128, 16], "k_ones": [128, 128], "k_sel": [2, 2, 128], "k_selj": [128, 16, 128], "k_cmask": [128, 128],
    "s_bmr": [128, 16, 32], "s_bmi": [128, 16, 32], "s_cmr": [128, 16, 32], "s_cmi": [128, 16, 32], "s_drep": [128, 16],
}


def build(upto=99, taps=()):
    nc = bass.Bass("TRN2", target_bir_lowering=False)
    P = Prog(nc)
    I = {k: P.dram(k, v, F32, kind="ExternalInput") for k, v in SHAPES.items()}
    out_d = P.dram("out", [TOK, D], F32, kind="ExternalOutput")
    mixed_d = P.dram("mixed_scr", [TOK, D], F32, kind="Internal")
    tapd = {}

    def tap(name, src_t, shape):
        if name in taps:
            td = P.dram("tap_" + name, shape, F32, kind="ExternalOutput")
            tapd[name] = td
            P.dma("sp", td[tuple(slice(None) for _ in shape)], src_t[tuple(slice(None) for _ in shape)])

    def ld(dst, src, q="sp"):
        P.dma(q, dst, src)

    def bc_row(tname, rows=128):
        t = I[tname]
        n = SHAPES[tname][1]
        return V(t, bass.AP(t.h, 0, [[0, rows], [1, n]]))

    ident = P.sb("ident", [128, 128]); ld(ident[:], I["k_ident"][:, :])
    ones = P.sb("ones", [128, 128]); ld(ones[:], I["k_ones"][:, :])
    PS = [P.ps("ps%d" % i) for i in range(8)]
    sh1T = P.sb("sh1T", [128, 8, 2])
    sc1T = P.sb("sc1T", [128, 8, 2])
    mod_d = P.dram("mod_scr", [2, 6 * D], F32, kind="Internal")

    with P.scope():
        cT = P.sb("cT", [128, 8, 2]); ld(cT[:], I["cT"][:, :, :])
        condT = P.sb("condT", [128, 8, 2])
        P.act(condT[:], cT[:], AF.Silu)
        adab = P.sb("adab", [2, 6 * D]); ld(adab[:], I["ada_b2"][:, :])
        mod = P.sb("mod", [2, 6 * D])
        sel = P.sb("sel", [2, 2, 128]); ld(sel[:], I["k_sel"][:, :, :])
        wab = [P.sb("wab%d" % i, [128, 8, 512]) for i in range(2)]
        aw = I["ada_w"]
        for cb in range(12):
            wb = wab[cb % 2]
            src = V(aw, aw.h[:, cb * 512:(cb + 1) * 512].rearrange("(kt p) n -> p kt n", p=128))
            ld(wb[:], src)
            for kt in range(8):
                P.mm(PS[cb % 2][0:2, :], condT[:, kt, :], wb[:, kt, :], start=(kt == 0), stop=(kt == 7))
            P.tt("dve", mod[:, cb * 512:(cb + 1) * 512], PS[cb % 2][0:2, :], adab[:, cb * 512:(cb + 1) * 512], ALU.add)
        tap("mod", mod, [2, 6 * D])
        for j in range(16):
            P.tr(PS[2][:, 2 * j:2 * j + 2], mod[:, j * 128:(j + 1) * 128], ident[0:2, 0:2])
        P.cp("dve", V(sh1T, sh1T.h[:].rearrange("p a b -> p (a b)")), PS[2][:, 0:16])
        P.ts("dve", V(sc1T, sc1T.h[:].rearrange("p a b -> p (a b)")), PS[2][:, 16:32], 1.0, ALU.add)
        P.dma("sp", mod_d[:, :], mod[:])
    if upto <= 0:
        return finish(nc, P, out_d)

    with P.scope():
        a2 = P.sb("a2", [128, 128]); ld(a2[:], I["k_a2"][:, :])
        a3 = P.sb("a3", [128, 128]); ld(a3[:], I["k_a3"][:, :])
        rb = P.sb("rbm", [128, 132]); ld(rb[:], I["k_rb"][:, :])
        mask = P.sb("mask", [128, 128]); ld(mask[:], I["k_mask"][:, :])
        hgnw = P.sb("hgnw", [128, 4]); ld(hgnw[:], I["hg_nw"][:, :])
        lbb = P.sb("lbb", [128, 512])
        omlb = P.sb("omlb", [128, 512])
        with P.scope():
            h0 = P.sb("h0", [128, 512]); h1 = P.sb("h1", [128, 512])
            hbt = I["hb"]
            ld(h0[:], V(hbt, bass.AP(hbt.h, 0, [[0, 128], [1, 512]])))
            ld(h1[:], V(hbt, bass.AP(hbt.h, 512, [[0, 128], [1, 512]])))
            P.tt("dve", h0[:], h0[:], h1[:], ALU.subtract)
            P.act(lbb[:], h0[:], AF.Sigmoid)
            P.ts("dve", omlb[:], lbb[:], -1.0, ALU.mult, 1.0, ALU.add)
        s_zre = P.sb("s_zre", [128, 16]); s_zim = P.sb("s_zim", [128, 16]); s_zimn = P.sb("s_zimn", [128, 16])
        Lre = P.sb("Lre", [128, 11, 16]); Lim = P.sb("Lim", [128, 11, 16]); Limn = P.sb("Limn", [128, 11, 16])
        PWr = P.sb("PWr", [128, 9, 16]); PWi = P.sb("PWi", [128, 9, 16])
        PWDr = P.sb("PWDr", [128, 8, 16]); PWDi = P.sb("PWDi", [128, 8, 16])
        IPr = P.sb("IPr", [128, 8, 16]); IPi = P.sb("IPi", [128, 8, 16])
        LCr = P.sb("LCr", [128, 8, 16]); LCi = P.sb("LCi", [128, 8, 16]); LCin = P.sb("LCin", [128, 8, 16])
        S5T = dict(PWr=PWr, PWi=PWi, PWDr=PWDr, PWDi=PWDi, IPr=IPr, IPi=IPi, LCr=LCr, LCi=LCi, LCin=LCin)
        sdk = P.sb("sdk", [128, 4]); ld(sdk[:], I["s_d"][:, :])
        glub = P.sb("glub", [128, 4]); ld(glub[:], I["glu_b"][:, :])
        snw = P.sb("snw", [128, 4]); ld(snw[:], I["s_nw"][:, :])
        with P.scope():
            ar = P.sb("ar", [128, 16]); ld(ar[:], I["s_ar"][:, :])
            ai = P.sb("ai", [128, 16]); ld(ai[:], I["s_ai"][:, :])
            dt = P.sb("dt", [128, 16]); ld(dt[:], I["s_ldt"][:, :])
            t1 = P.sb("t1", [128, 16]); t2 = P.sb("t2", [128, 16]); t3 = P.sb("t3", [128, 16])
            cc = P.sb("cc", [128, 16]); ss = P.sb("ss", [128, 16]); mag = P.sb("mag", [128, 16])
            P.act(dt[:], dt[:], AF.Exp)
            P.tt("dve", t1[:], ar[:], dt[:], ALU.mult)
            P.act(mag[:], t1[:], AF.Exp)
            P.tt("dve", t2[:], ai[:], dt[:], ALU.mult)
            P.ts("dve", t2[:], t2[:], 1.0 / 16.0, ALU.mult)
            P.act(ss[:], t2[:], AF.Sin)
            P.ts("dve", t3[:], t2[:], -1.0, ALU.mult, math.pi / 2.0, ALU.add)
            P.act(cc[:], t3[:], AF.Sin)
            for _ in range(4):
                P.tt("dve", t1[:], cc[:], cc[:], ALU.mult)
                P.tt("dve", t3[:], ss[:], ss[:], ALU.mult)
                P.tt("dve", t2[:], ss[:], cc[:], ALU.mult)
                P.tt("dve", cc[:], t1[:], t3[:], ALU.subtract)
                P.ts("dve", ss[:], t2[:], 2.0, ALU.mult)
            P.tt("dve", Lre[:, 0, :], mag[:], cc[:], ALU.mult)
            P.tt("dve", Lim[:, 0, :], mag[:], ss[:], ALU.mult)
            den = P.sb("den", [128, 16]); nr = P.sb("nr", [128, 16])
            P.tt("dve", t1[:], ar[:], ar[:], ALU.mult)
            P.tt("dve", t2[:], ai[:], ai[:], ALU.mult)
            P.tt("dve", den[:], t1[:], t2[:], ALU.add)
            P.recip(den[:], den[:])
            P.ts("dve", nr[:], Lre[:, 0, :], -1.0, ALU.add)
            P.tt("dve", t1[:], nr[:], ar[:], ALU.mult)
            P.tt("dve", t2[:], Lim[:, 0, :], ai[:], ALU.mult)
            P.tt("dve", t1[:], t1[:], t2[:], ALU.add)
            P.tt("dve", s_zre[:], t1[:], den[:], ALU.mult)
            P.tt("dve", t1[:], Lim[:, 0, :], ar[:], ALU.mult)
            P.tt("dve", t2[:], nr[:], ai[:], ALU.mult)
            P.tt("dve", t1[:], t1[:], t2[:], ALU.subtract)
            P.tt("dve", s_zim[:], t1[:], den[:], ALU.mult)
            P.ts("dve", s_zimn[:], s_zim[:], -1.0, ALU.mult)
            for k in range(1, 11):
                P.tt("dve", t1[:], Lre[:, k - 1, :], Lre[:, k - 1, :], ALU.mult)
                P.tt("dve", t2[:], Lim[:, k - 1, :], Lim[:, k - 1, :], ALU.mult)
                P.tt("dve", t3[:], Lre[:, k - 1, :], Lim[:, k - 1, :], ALU.mult)
                P.tt("dve", Lre[:, k, :], t1[:], t2[:], ALU.subtract)
                P.ts("dve", Lim[:, k, :], t3[:], 2.0, ALU.mult)
            P.ts("dve", Limn[:], Lim[:], -1.0, ALU.mult)
            P.op("dve", lambda e: e.memset(PWr.h[:, 0, :], 1.0), [], [PWr])
            P.op("dve", lambda e: e.memset(PWi.h[:, 0, :], 0.0), [], [PWi])
            for k in range(1, 9):
                cmul_s(P, PWr[:, k, :], PWi[:, k, :], PWr[:, k - 1, :], PWi[:, k - 1, :], Lre[:, 0, :], Lim[:, 0, :], t1, t2)
            for sx in range(8):
                P.cp("dve", PWDr[:, sx, :], PWr[:, 7 - sx, :])
                P.cp("dve", PWDi[:, sx, :], PWi[:, 7 - sx, :])
                P.tt("dve", t1[:], PWr[:, sx, :], PWr[:, sx, :], ALU.mult)
                P.tt("dve", t2[:], PWi[:, sx, :], PWi[:, sx, :], ALU.mult)
                P.tt("dve", t1[:], t1[:], t2[:], ALU.add)
                P.recip(t1[:], t1[:])
                P.tt("dve", IPr[:, sx, :], PWr[:, sx, :], t1[:], ALU.mult)
                P.tt("dve", t2[:], PWi[:, sx, :], t1[:], ALU.mult)
                P.ts("dve", IPi[:, sx, :], t2[:], -1.0, ALU.mult)
            P.cp("dve", LCr[:, 0, :], PWr[:, 8, :])
            P.cp("dve", LCi[:, 0, :], PWi[:, 8, :])
            for k in range(1, 8):
                cmul_s(P, LCr[:, k, :], LCi[:, k, :], LCr[:, k - 1, :], LCi[:, k - 1, :], LCr[:, k - 1, :], LCi[:, k - 1, :], t1, t2)
            P.ts("dve", LCin[:], LCi[:], -1.0, ALU.mult)
        tap("Lre", Lre, [128, 11, 16]); tap("Lim", Lim, [128, 11, 16]); tap("zre", s_zre, [128, 16]); tap("zim", s_zim, [128, 16])

        for b in range(NB):
            with P.scope():
                ohg = P.sb("ohg", [128, 4, SEQ])
                uT = P.sb("uT", [128, 4, SEQ])
                with P.scope():
                    stage1(P, I, PS, b, ident, ones, sh1T, sc1T, a2, a3, rb, mask, hgnw, lbb, omlb, ohg, uT, tap)
                if upto <= 1:
                    continue
                yT = P.sb("yT", [128, 4, SEQ])
                if True:
                    with P.scope():
                        stage2c(P, I, PS, ident, uT, yT, s_zre, s_zim, s_zimn, S5T, tap, b)
                if upto <= 2:
                    continue
                with P.scope():
                    stage3a(P, I, PS, b, ones, ohg, yT, glub, snw, mixed_d, tap)
    if upto <= 3:
        if "mixed" in taps:
            td = P.dram("tap_mixed", [TOK, D], F32, kind="ExternalOutput")
            with P.scope():
                tmp = P.sb("tmpm", [128, D])
                for i in range(TOK // 128):
                    P.dma("sp", tmp[:], mixed_d[i * 128:(i + 1) * 128, :])
                    P.dma("sp", td[i * 128:(i + 1) * 128, :], tmp[:])
        return finish(nc, P, out_d)

    with P.scope():
        stage3b(P, I, PS, ident, mod_d, mixed_d, out_d, bc_row, tap, upto)
    return finish(nc, P, out_d)


def finish(nc, P, out_d):
    P.barrier()
    P.root.close()
    return nc


def stage1(P, I, PS, b, ident, ones, sh1T, sc1T, a2, a3, rb, mask, hgnw, lbb, omlb, ohg, uT, tap):
    xin = I["x"]
    win = I["w_in"]
    hT = P.sb("hT", [128, 8, 512])
    xt = [P.sb("xt%d" % i, [128, D]) for i in range(2)]
    wbuf = [P.sb("wbuf%d" % i, [128, 8, 512]) for i in range(2)]
    qsT = P.sb("qsT", [128, 4, 512])
    gsT = P.sb("gsT", [128, 4, 512])
    lf = P.sb("lf", [128, 4, 512])
    omf = P.sb("omf", [128, 4, 512])
    vv = P.sb("vv", [128, 4, 512])
    tmpa = P.sb("tmpa", [128, 512])
    tmpb = P.sb("tmpb", [128, 512])
    S = P.sb("S", [128, 4, 128])
    Smid = P.sb("Smid", [128, 4, 128])
    Stmp = P.sb("Stmp", [128, 4, 128])
    Esb = P.sb("Esb", [128, 4, 132])
    EK = P.sb("EK", [128, 512])
    EH = P.sb("EH", [128, 512])
    Kt = P.sb("Kt", [128, 512])
    Kh = P.sb("Kh", [128, 512])
    QT = P.sb("QT", [128, 4, 128])
    KTT = P.sb("KTT", [128, 4, 128])
    ST = P.sb("ST", [128, 4, 128])
    P.op("dve", lambda e: e.memset(S.h[:], 0.0), [], [S])
    wi = 0
    for tb in range(4):
        t0 = b * SEQ + tb * 512
        for tt_ in range(4):
            xb = xt[tt_ % 2]
            P.dma("sp", xb[:], xin[t0 + tt_ * 128: t0 + (tt_ + 1) * 128, :])
            for half in range(2):
                pt = PS[half]
                for j in range(4):
                    kt = half * 4 + j
                    P.tr(pt[:, j * 128:(j + 1) * 128], xb[:, kt * 128:(kt + 1) * 128], ident[:, :])
                for j in range(4):
                    kt = half * 4 + j
                    P.act(hT[:, kt, tt_ * 128:(tt_ + 1) * 128], pt[:, j * 128:(j + 1) * 128], AF.Identity,
                          bias=sh1T[:, kt, b:b + 1], scale=sc1T[:, kt, b:b + 1])
        if tb == 0 and b == 0:
            tap("hT", hT, [128, 8, 512])
        for grp in (0, 3, 4, 1, 2):
            wb = wbuf[wi % 2]
            wi += 1
            src = V(win, win.h[:, grp * 512:(grp + 1) * 512].rearrange("(kt p) n -> p kt n", p=128))
            P.dma("sp", wb[:], src)
            if grp in (0, 3, 4):
                for ct in range(4):
                    pt = PS[2 + ct % 2]
                    for kt in range(8):
                        P.mm(pt[:, :], wb[:, kt, ct * 128:(ct + 1) * 128], hT[:, kt, :], start=(kt == 0), stop=(kt == 7))
                    if grp == 0:
                        P.act(qsT[:, ct, :], pt[:, :], AF.Silu)
                    elif grp == 3:
                        P.act(gsT[:, ct, :], pt[:, :], AF.Silu)
                    else:
                        P.cp("dve", uT[:, ct, tb * 512:(tb + 1) * 512], pt[:, :])
            else:
                for tt_ in range(4):
                    pt = PS[2 + tt_ % 2]
                    for kt in range(8):
                        P.mm(pt[:, :], hT[:, kt, tt_ * 128:(tt_ + 1) * 128], wb[:, kt, :], start=(kt == 0), stop=(kt == 7))
                    if grp == 1:
                        P.act(tmpa[:], pt[:, :], AF.Sigmoid)
                        P.tt("dve", tmpa[:], tmpa[:], omlb[:], ALU.mult)
                        P.tt("dve", tmpa[:], tmpa[:], lbb[:], ALU.add)
                        P.act(lf[:, tt_, :], tmpa[:], AF.Ln)
                        P.ts("dve", omf[:, tt_, :], tmpa[:], -1.0, ALU.mult, 1.0, ALU.add)
                    else:
                        P.cp("dve", vv[:, tt_, :], pt[:, :])
        if tb == 0 and b == 0:
            tap("qsT", qsT, [128, 4, 512]); tap("lf", lf, [128, 4, 512]); tap("vv", vv, [128, 4, 512])
        pK, pH, pB0, pB1, pT, pS, pO, pD = PS[0], PS[1], PS[2], PS[3], PS[4], PS[5], PS[6], PS[7]
        for tt_ in range(4):
            P.mm(pK[:, :], a2[:, :], lf[:, tt_, :])
            P.mm(pH[:, :], a3[:, :], lf[:, tt_, :])
            P.act(EK[:], pK[:, :], AF.Exp)
            P.act(EH[:], pH[:, :], AF.Exp)
            P.tt("dve", Kt[:], omf[:, tt_, :], EK[:], ALU.mult)
            P.tt("dve", Kh[:], omf[:, tt_, :], EH[:], ALU.mult)
            for h in range(4):
                pb = pB0 if h < 2 else pB1
                P.mm(pb[:, (h % 2) * 132:(h % 2) * 132 + 132], lf[:, tt_, h * 128:(h + 1) * 128], rb[:, :])
            P.act(Esb[:, 0:2, :], V(pB0, pB0.h[:, 0:264].rearrange("p (a b) -> p a b", a=2)), AF.Exp)
            P.act(Esb[:, 2:4, :], V(pB1, pB1.h[:, 0:264].rearrange("p (a b) -> p a b", a=2)), AF.Exp)
            P.tt("dve", QT[:], qsT[:, :, tt_ * 128:(tt_ + 1) * 128], Esb[:, :, 0:128], ALU.mult)
            for h in range(4):
                P.tr(pT[:, h * 128:(h + 1) * 128], Kt[:, h * 128:(h + 1) * 128], ident[:, :])
            P.cp("act", V(KTT, KTT.h[:].rearrange("p a b -> p (a b)")), pT[:, :])
            for h in range(4):
                P.mm(pS[:, h * 128:(h + 1) * 128], KTT[:, h, :], QT[:, h, :])
            mk = V(mask, bass.AP(mask.h, 0, [[128, 128], [0, 4], [1, 128]]))
            P.tt("dve", ST[:], V(pS, pS.h[:, :].rearrange("p (a b) -> p a b", a=4)), mk, ALU.mult)
            if tb == 0 and b == 0 and tt_ == 0:
                tap("Esb", Esb, [128, 4, 132]); tap("ST", ST, [128, 4, 128]); tap("QT", QT, [128, 4, 128]); tap("KTT", KTT, [128, 4, 128])
            for j in range(2):
                em = V(Esb, bass.AP(Esb.h, 129 + 2 * j, [[528, 128], [132, 4], [0, 128]]))
                P.tt("dve", Smid[:], S[:], em, ALU.mult)
                for h in range(4):
                    oc = pO[:, h * 128 + j * 64: h * 128 + j * 64 + 64]
                    P.mm(oc, vv[:, tt_, h * 128:(h + 1) * 128], ST[:, h, j * 64:(j + 1) * 64], start=True, stop=False)
                    P.mm(oc, Smid[:, h, :], QT[:, h, j * 64:(j + 1) * 64], start=False, stop=True)
                for h in range(4):
                    P.mm(pD[:, h * 128:(h + 1) * 128], Kh[j * 64:(j + 1) * 64, h * 128:(h + 1) * 128],
                         vv[j * 64:(j + 1) * 64, tt_, h * 128:(h + 1) * 128])
                dc = V(Esb, bass.AP(Esb.h, 128 + 2 * j, [[528, 128], [132, 4], [0, 128]]))
                P.tt("dve", Stmp[:], S[:], dc, ALU.mult)
                P.tt("dve", S[:], Stmp[:], V(pD, pD.h[:, :].rearrange("p (a b) -> p a b", a=4)), ALU.add)
            tc0 = tb * 512 + tt_ * 128
            P.cp("act", ohg[:, :, tc0:tc0 + 128], V(pO, pO.h[:, :].rearrange("p (a b) -> p a b", a=4)))
        if tb == 0 and b == 0:
            tap("oraw", ohg, [128, 4, SEQ])
        cs = slice(tb * 512, (tb + 1) * 512)
        for h in range(4):
            pn = PS[h % 2]
            P.act(tmpa[:], ohg[:, h, cs], AF.Square)
            P.mm(pn[:, :], ones[:, :], tmpa[:])
            P.act(tmpb[:], pn[:, :], AF.Sqrt, bias=RMS_EPS, scale=1.0 / 128.0)
            P.recip(tmpb[:], tmpb[:])
            P.stt("dve", tmpa[:], ohg[:, h, cs], hgnw[:, h:h + 1], tmpb[:], ALU.mult, ALU.mult)
            P.tt("dve", ohg[:, h, cs], tmpa[:], gsT[:, h, :], ALU.mult)
    if b == 0:
        tap("ohg", ohg, [128, 4, SEQ]); tap("uT", uT, [128, 4, SEQ])


def cmul_s(P, outr, outi, ar, ai, br, bi, t1, t2):
    P.tt("dve", t1[:], ar, br, ALU.mult)
    P.tt("dve", t2[:], ai, bi, ALU.mult)
    P.tt("dve", outr, t1[:], t2[:], ALU.subtract)
    P.tt("dve", t1[:], ar, bi, ALU.mult)
    P.tt("dve", t2[:], ai, br, ALU.mult)
    P.tt("dve", outi, t1[:], t2[:], ALU.add)


def stage2c(P, I, PS, ident, uT, yT, zre, zim, zimn, TB, tap, b):
    NCH = SEQ // 8
    bmr = P.sb("bmr", [128, 16, 32]); P.dma("sp", bmr[:], I["s_bmr"][:, :, :])
    bmi = P.sb("bmi", [128, 16, 32]); P.dma("sp", bmi[:], I["s_bmi"][:, :, :])
    cmr = P.sb("cmr", [128, 16, 32]); P.dma("sp", cmr[:], I["s_cmr"][:, :, :])
    cmi = P.sb("cmi", [128, 16, 32]); P.dma("sp", cmi[:], I["s_cmi"][:, :, :])
    drep = P.sb("drep", [128, 16]); P.dma("sp", drep[:], I["s_drep"][:, :])
    selj = P.sb("selj", [128, 16, 128]); P.dma("sp", selj[:], I["k_selj"][:, :, :])
    cmask = P.sb("cmask", [128, 128]); P.dma("sp", cmask[:], I["k_cmask"][:, :])
    sets = []
    for i in range(2):
        d = {}
        d["Bbr"] = P.sb("Bbr", [128, 32]); d["Bbi"] = P.sb("Bbi", [128, 32])
        for nm in ("ETr", "ETi", "G1r", "G1in", "G2r", "G2i", "Fr", "Fin", "ta", "tb"):
            d[nm] = P.sb(nm, [128, 8, 32])
        d["Esb"] = P.sb("Esb2", [128, 2, 2, 128])
        d["Msb"] = P.sb("Msb", [128, 3, 128])
        d["Usb"] = P.sb("Usb", [128, 2, NCH])
        d["XA"] = P.sb("XA", [128, 2, NCH + 1]); d["XB"] = P.sb("XB", [128, 2, NCH + 1])
        P.op("dve", lambda e, t=d["XA"]: e.memset(t.h[:], 0.0), [], [d["XA"]])
        P.op("dve", lambda e, t=d["XB"]: e.memset(t.h[:], 0.0), [], [d["XB"]])
        sets.append(d)
    pY = PS[0:4]
    pU, pW, pE, pM = PS[4], PS[5], PS[6], PS[7]

    def bc8(t, off, pstride, kstride):
        return V(t, bass.AP(t.h, off, [[pstride, 128], [kstride, 8], [0, 32]]))

    def bcx(t, off, pstride):
        return V(t, bass.AP(t.h, off, [[pstride, 128], [0, 8], [1, 32]]))

    def cm(d, outr, outi, ar, ai, br, bi, neg_im=False):
        ta, tb_ = d["ta"], d["tb"]
        P.tt("dve", ta[:], ar, br, ALU.mult)
        P.tt("dve", tb_[:], ai, bi, ALU.mult)
        P.tt("dve", outr[:], ta[:], tb_[:], ALU.subtract)
        P.tt("dve", ta[:], ar, bi, ALU.mult)
        P.tt("dve", tb_[:], ai, br, ALU.mult)
        if neg_im:
            P.stt("dve", outi[:], ta[:], -1.0, tb_[:], ALU.mult, ALU.subtract)
        else:
            P.tt("dve", outi[:], ta[:], tb_[:], ALU.add)

    for ct in range(4):
        for j in range(4):
            st = ct * 4 + j
            d = sets[st % 2]
            Bbr, Bbi = d["Bbr"], d["Bbi"]
            P.ts("dve", Bbr[:], bmi[:, st, :], zimn[:, st:st + 1], ALU.mult)
            P.stt("dve", Bbr[:], bmr[:, st, :], zre[:, st:st + 1], Bbr[:], ALU.mult, ALU.add)
            P.ts("dve", Bbi[:], bmr[:, st, :], zim[:, st:st + 1], ALU.mult)
            P.stt("dve", Bbi[:], bmi[:, st, :], zre[:, st:st + 1], Bbi[:], ALU.mult, ALU.add)
            bbr_b, bbi_b = bcx(Bbr, 0, 32), bcx(Bbi, 0, 32)
            cr_b, ci_b = bcx(cmr, st * 32, 512), bcx(cmi, st * 32, 512)
            cm(d, d["ETr"], d["ETi"], bbr_b, bbi_b, bc8(TB["PWDr"], st, 128, 16), bc8(TB["PWDi"], st, 128, 16))
            cm(d, d["G1r"], d["G1in"], bbr_b, bbi_b, bc8(TB["IPr"], st, 128, 16), bc8(TB["IPi"], st, 128, 16), neg_im=True)
            cm(d, d["G2r"], d["G2i"], cr_b, ci_b, bc8(TB["PWr"], st, 144, 16), bc8(TB["PWi"], st, 144, 16))
            cm(d, d["Fr"], d["Fin"], cr_b, ci_b, bc8(TB["PWr"], st + 16, 144, 16), bc8(TB["PWi"], st + 16, 144, 16), neg_im=True)
            fl = lambda t: t.h[:].rearrange("p a b -> p (a b)")
            for ri, nm in enumerate(("ETr", "ETi")):
                for k in range(2):
                    P.tr(pE[:, (ri * 2 + k) * 128:(ri * 2 + k + 1) * 128], V(d[nm], fl(d[nm])[:, k * 128:(k + 1) * 128]), ident[:, :])
            P.cp("act", V(d["Esb"], d["Esb"].h[:].rearrange("p a b c -> p (a b c)")), pE[:, :])
            for bi_, (k, mt) in enumerate(((0, 0), (0, 1), (1, 1))):
                oc = pM[:, bi_ * 128:(bi_ + 1) * 128]
                P.mm(oc, V(d["G1r"], fl(d["G1r"])[:, k * 128:(k + 1) * 128]), V(d["G2r"], fl(d["G2r"])[:, mt * 128:(mt + 1) * 128]), start=True, stop=False)
                P.mm(oc, V(d["G1in"], fl(d["G1in"])[:, k * 128:(k + 1) * 128]), V(d["G2i"], fl(d["G2i"])[:, mt * 128:(mt + 1) * 128]), start=False, stop=True)
            Msb = d["Msb"]
            P.tt("dve", Msb[:, 0, :], pM[:, 0:128], cmask[:, :], ALU.mult)
            P.cp("dve", Msb[:, 1, :], pM[:, 128:256])
            P.tt("dve", Msb[:, 2, :], pM[:, 256:384], cmask[:, :], ALU.mult)
            P.stt("dve", Msb[:, 0, :], ident[:, :], drep[:, st:st + 1], Msb[:, 0, :], ALU.mult, ALU.add)
            P.stt("dve", Msb[:, 2, :], ident[:, :], drep[:, st:st + 1], Msb[:, 2, :], ALU.mult, ALU.add)
            Usb = d["Usb"]
            for k in range(2):
                for sl in range(4):
                    sx = 4 * k + sl
                    rhs = V(uT, bass.AP(uT.h, ct * SEQ + sx, [[4 * SEQ, 128], [8, NCH]]))
                    P.mm(pU[:, k * NCH:(k + 1) * NCH], selj[:, j * 4 + sl, :], rhs, start=(sl == 0), stop=(sl == 3))
            P.cp("act", V(Usb, Usb.h[:].rearrange("p a b -> p (a b)")), pU[:, :])
            Esb = d["Esb"]
            for ri in range(2):
                for k in range(2):
                    P.mm(pW[:, ri * NCH:(ri + 1) * NCH], Esb[:, ri, k, :], Usb[:, k, :], start=(k == 0), stop=(k == 1))
            XA, XB = d["XA"], d["XB"]
            P.cp("dve", XA[:, :, 1:NCH + 1], V(pW, pW.h[:, :].rearrange("p (a b) -> p a b", a=2)))
            src, dst = XA, XB
            for k in range(8):
                dd = 1 << k
                lr = TB["LCr"][:, k, st:st + 1]; li = TB["LCi"][:, k, st:st + 1]; lin = TB["LCin"][:, k, st:st + 1]
                P.cp("act", dst[:, :, 1:1 + dd], src[:, :, 1:1 + dd])
                lo = slice(1, NCH + 1 - dd)
                hi = slice(1 + dd, NCH + 1)
                P.stt("dve", dst[:, 0, hi], src[:, 0, lo], lr, src[:, 0, hi], ALU.mult, ALU.add)
                P.stt("dve", dst[:, 0, hi], src[:, 1, lo], lin, dst[:, 0, hi], ALU.mult, ALU.add)
                P.stt("dve", dst[:, 1, hi], src[:, 1, lo], lr, src[:, 1, hi], ALU.mult, ALU.add)
                P.stt("dve", dst[:, 1, hi], src[:, 0, lo], li, dst[:, 1, hi], ALU.mult, ALU.add)
                src, dst = dst, src
            X = src
            pr = slice(32 * j, 32 * j + 32)
            for t in range(8):
                mt, tl = t // 4, t % 4
                oc = pY[t // 2][pr, (t % 2) * NCH:(t % 2 + 1) * NCH]
                cs_ = slice(tl * 32, (tl + 1) * 32)
                tp = (0, 32 * j)
                P.mm(oc, Msb[:, mt, cs_], Usb[:, 0, :], start=True, stop=False, tp=tp)
                if mt == 1:
                    P.mm(oc, Msb[:, 2, cs_], Usb[:, 1, :], start=False, stop=False, tp=tp)
                P.mm(oc, d["Fr"][:, t, :], X[:, 0, 0:NCH], start=False, stop=False, tp=tp)
                P.mm(oc, d["Fin"][:, t, :], X[:, 1, 0:NCH], start=False, stop=True, tp=tp)
        for t in range(8):
            dst_ = V(yT, bass.AP(yT.h, ct * SEQ + t, [[4 * SEQ, 128], [8, NCH]]))
            P.act(dst_, pY[t // 2][:, (t % 2) * NCH:(t % 2 + 1) * NCH], AF.Gelu)
    if b == 0:
        tap("ygelu", yT, [128, 4, SEQ])


def stage2_old(P, I, PS, uT, yT, s_zre, s_zim, s_zimn, Lre, Lim, Limn, sdk, tap, b):
    N = SEQ
    btr = P.sb("btr", [128, 4, 128]); P.dma("sp", btr[:], I["s_btr"][:, :, :])
    bti = P.sb("bti", [128, 4, 128]); P.dma("sp", bti[:], I["s_bti"][:, :, :])
    ctr = P.sb("ctr", [128, 16, 128]); P.dma("sp", ctr[:], I["s_ctr"][:, :, :])
    ctin = P.sb("ctin", [128, 16, 128]); P.dma("sp", ctin[:], I["s_cti"][:, :, :])
    P.ts("dve", ctin[:], ctin[:], -1.0, ALU.mult)
    X = [[P.sb("x%d%d" % (i, j), [128, N]) for j in range(2)] for i in range(2)]
    t1 = P.sb("s2t1", [128, 512])
    t2 = P.sb("s2t2", [128, 512])
    pY = PS[0:4]
    pR, pI_ = PS[4], PS[5]
    for ct in range(4):
        for j in range(4):
            st = ct * 4 + j
            pr = slice(32 * j, 32 * j + 32)
            for tb in range(4):
                cs = slice(tb * 512, (tb + 1) * 512)
                P.mm(pR[:, :], btr[pr, ct, :], uT[pr, ct, cs], tp=(32 * j, 0))
                P.mm(pI_[:, :], bti[pr, ct, :], uT[pr, ct, cs], tp=(32 * j, 0))
                P.ts("dve", t1[:], pI_[:, :], s_zimn[:, st:st + 1], ALU.mult)
                P.stt("dve", X[0][0][:, cs], pR[:, :], s_zre[:, st:st + 1], t1[:], ALU.mult, ALU.add)
                P.ts("dve", t2[:], pR[:, :], s_zim[:, st:st + 1], ALU.mult)
                P.stt("dve", X[0][1][:, cs], pI_[:, :], s_zre[:, st:st + 1], t2[:], ALU.mult, ALU.add)
            if st == 0 and b == 0:
                tap("bure", X[0][0], [128, N])
            cur = 0
            for k in range(11):
                d = 1 << k
                sr, si = X[cur]
                dr, di = X[1 - cur]
                lr = Lre[:, k, st:st + 1]
                li = Lim[:, k, st:st + 1]
                lin = Limn[:, k, st:st + 1]
                P.cp("act", dr[:, 0:d], sr[:, 0:d])
                P.cp("act", di[:, 0:d], si[:, 0:d])
                P.stt("dve", dr[:, d:N], sr[:, 0:N - d], lr, sr[:, d:N], ALU.mult, ALU.add)
                P.stt("dve", dr[:, d:N], si[:, 0:N - d], lin, dr[:, d:N], ALU.mult, ALU.add)
                P.stt("dve", di[:, d:N], si[:, 0:N - d], lr, si[:, d:N], ALU.mult, ALU.add)
                P.stt("dve", di[:, d:N], sr[:, 0:N - d], li, di[:, d:N], ALU.mult, ALU.add)
                cur = 1 - cur
            xr, xi = X[cur]
            if st == 0 and b == 0:
                tap("xre", xr, [128, N])
            for tb in range(4):
                cs = slice(tb * 512, (tb + 1) * 512)
                P.mm(pY[tb][:, :], ctr[:, st, :], xr[:, cs], start=(j == 0), stop=False)
                P.mm(pY[tb][:, :], ctin[:, st, :], xi[:, cs], start=False, stop=(j == 3))
        for tb in range(4):
            cs = slice(tb * 512, (tb + 1) * 512)
            P.stt("dve", t1[:], uT[:, ct, cs], sdk[:, ct:ct + 1], pY[tb][:, :], ALU.mult, ALU.add)
            P.act(yT[:, ct, cs], t1[:], AF.Gelu)
    if b == 0:
        tap("ygelu", yT, [128, 4, SEQ])


def stage3a(P, I, PS, b, ones, ohg, yT, glub, snw, mixed_d, tap):
    gluw = P.sb("gluw", [128, 4, 512])
    gw = I["glu_w"]
    P.dma("sp", gluw[:], V(gw, gw.h[:, :].rearrange("(kt p) n -> p kt n", p=128)))
    wout = P.sb("wout", [128, 8, D])
    wo = I["w_out"]
    P.dma("sp", wout[:], V(wo, wo.h[:, :].rearrange("(kt p) n -> p kt n", p=128)))
    y2 = P.sb("y2", [128, 4, 512])
    sq = P.sb("sq", [128, 512])
    rstd = P.sb("rstd", [128, 512])
    sg = P.sb("sg", [128, 512])
    mt = [P.sb("mt%d" % i, [128, D]) for i in range(2)]
    for tb in range(4):
        cs = slice(tb * 512, (tb + 1) * 512)
        pn = PS[2]
        for c2 in range(4):
            pg = PS[c2 % 2]
            for kt in range(4):
                P.mm(pg[:, :], gluw[:, kt, c2 * 128:(c2 + 1) * 128], yT[:, kt, cs], start=(kt == 0), stop=(kt == 3))
            P.act(sg[:], pg[:, :], AF.Sigmoid, bias=glub[:, c2:c2 + 1])
            P.tt("dve", y2[:, c2, :], yT[:, c2, cs], sg[:], ALU.mult)
            P.act(sq[:], y2[:, c2, :], AF.Square)
            P.mm(pn[:, :], ones[:, :], sq[:], start=(c2 == 0), stop=(c2 == 3))
        P.act(rstd[:], pn[:, :], AF.Sqrt, bias=RMS_EPS, scale=1.0 / 512.0)
        P.recip(rstd[:], rstd[:])
        for c2 in range(4):
            P.stt("dve", yT[:, c2, cs], y2[:, c2, :], snw[:, c2:c2 + 1], rstd[:], ALU.mult, ALU.mult)
        for tt_ in range(4):
            tcs = slice(tb * 512 + tt_ * 128, tb * 512 + (tt_ + 1) * 128)
            m = mt[tt_ % 2]
            for nb in range(2):
                pm = PS[3 + nb]
                for ft in range(8):
                    l = ohg[:, ft, tcs] if ft < 4 else yT[:, ft - 4, tcs]
                    P.mm(pm[:, :], l, wout[:, ft, nb * 512:(nb + 1) * 512], start=(ft == 0), stop=(ft == 7))
                P.cp("act" if nb == 0 else "dve", m[:, nb * 512:(nb + 1) * 512], pm[:, :])
            r0 = b * SEQ + tb * 512 + tt_ * 128
            P.dma("sp", mixed_d[r0:r0 + 128, :], m[:], src_sem=True)
    if b == 0:
        tap("ossm", yT, [128, 4, SEQ])


def layer_norm_tile(P, v, outt, stats, mv, sc, w_bc, b_bc):
    for c in range(2):
        P.op("dve", lambda e, c=c: e.bn_stats(out=stats.h[:, c, :], in_=v.h[:, c * 512:(c + 1) * 512]), [v], [stats])
    P.op("dve", lambda e: e.bn_aggr(out=mv.h[:, :], in_=stats.h[:].rearrange("p a b -> p (a b)")), [stats], [mv])
    P.ts("dve", sc[:, 0:1], mv[:, 1:2], LN_EPS, ALU.add)
    P.act(sc[:, 0:1], sc[:, 0:1], AF.Sqrt)
    P.recip(sc[:, 0:1], sc[:, 0:1])
    P.stt("dve", sc[:, 1:2], mv[:, 0:1], -1.0, sc[:, 0:1], ALU.mult, ALU.mult)
    yield
    P.act(v[:], v[:], AF.Identity, bias=sc[:, 1:2], scale=sc[:, 0:1])
    P.tt("dve", v[:], v[:], w_bc[:], ALU.mult)
    yield
    P.tt("dve", outt[:], v[:], b_bc[:], ALU.add)
    yield


def stage3b(P, I, PS, ident, mod_d, mixed_d, out_d, bc_row, tap, upto):
    xin = I["x"]
    wq_d = I["w_q"]
    pu, pv = I["pu"], I["pv"]
    NT = TOK // 128
    RB1 = [P.sb("rb_%d" % q, [128, D]) for q in range(4)]
    sel = P.sb("sel3", [2, 2, 128]); P.dma("sp", sel[:], I["k_sel"][:, :, :])
    modq = P.sb("modq", [2, 1024])
    lnw = []
    for nm in ("ln1w", "ln1b", "ln2w", "ln2b"):
        t = P.sb(nm, [128, D])
        P.dma("sp", t[:], bc_row(nm))
        lnw.append(t)
    keysT = P.sb("keysT", [128, 16, 128]); P.dma("sp", keysT[:], I["keysT"][:, :, :])
    iota = P.sb("iota", [128, 16]); P.dma("sp", iota[:], I["k_iota"][:, :])
    xt = P.sb("xt3", [128, D]); mt = P.sb("mt3", [128, D]); vtf = P.sb("vtf", [128, D])
    statsf = P.sb("statsf", [128, 2, 6]); mvf = P.sb("mvf", [128, 2]); scf = P.sb("scf", [128, 2])
    h2T = P.sb("h2T", [128, 8, 128])
    wqb = [P.sb("wqb%d" % i, [128, 8, 128]) for i in range(2)]
    qT = P.sb("qT", [128, 16, 128])
    scs = P.sb("scs", [128, 16, 128]); scs2 = P.sb("scs2", [128, 16, 128])
    top = P.sb("top", [128, 16, 16])
    idx = P.sb("idx", [128, 16, 16], U32)
    idxf = P.sb("idxf", [128, 16, 16])
    cand = P.sb("cand", [128, 8, 256])
    best = P.sb("best", [128, 8, 16])
    pos = P.sb("pos", [128, 8, 16], U32)
    pi_ = P.sb("pi", [128, 8, 16], U32); pj_ = P.sb("pj", [128, 8, 16], U32)
    pif = P.sb("pif", [128, 8, 16]); pjf = P.sb("pjf", [128, 8, 16])
    oh = P.sb("oh", [128, 8, 16, 16])
    i1 = P.sb("i1", [128, 8, 16]); i2 = P.sb("i2", [128, 8, 16])
    eidf = P.sb("eidf", [128, 128]); gsum = P.sb("gsum", [128, 8])
    x1s = [P.sb("x1_%d" % i, [128, D]) for i in range(2)]
    h2s = [P.sb("h2_%d" % i, [128, D]) for i in range(2)]
    eids = [P.sb("eid_%d" % i, [128, 128], I32) for i in range(2)]
    gates = [P.sb("gate_%d" % i, [128, 8, 16]) for i in range(2)]
    vtg = P.sb("vtg", [128, D]); acc = P.sb("acc", [128, D]); ot = P.sb("ot", [128, D])
    acc2 = P.sb("acc2", [128, D])
    statsg = P.sb("statsg", [128, 2, 6]); mvg = P.sb("mvg", [128, 2]); scg = P.sb("scg", [128, 2])
    z = P.sb("z", [128, 128]); a_ = P.sb("a", [128, 128])
    NBUF = 6
    dgs = [P.sb("dg%d" % i, [128, 128]) for i in range(4)]
    gb = [P.sb("gb%d" % i, [128, D]) for i in range(2 * NBUF)]
    gi = [0]

    def nextbuf():
        t = gb[gi[0] % (2 * NBUF)]
        gi[0] += 1
        return t
    wi = [0]

    def front(ti):
        S = ti % 2
        x1, h2, eid, gate = x1s[S], h2s[S], eids[S], gates[S]
        b = ti // (SEQ // 128)
        r0 = ti * 128
        if ti % (SEQ // 128) == 0:
            for q, (off, plus1) in enumerate([(2048, True), (3072, False), (4096, True), (5120, True)]):
                P.dma("sp", modq[:], mod_d[:, off:off + 1024])
                for hb_ in range(2):
                    pt = PS[hb_]
                    P.mm(pt[:, :], sel[:, b, :], modq[:, hb_ * 512:(hb_ + 1) * 512])
                    dst = RB1[q][:, hb_ * 512:(hb_ + 1) * 512]
                    if plus1:
                        P.ts("dve", dst, pt[:, :], 1.0, ALU.add)
                    else:
                        P.cp("dve", dst, pt[:, :])
                    yield
        g1p, sh2, sc2p, g2p = RB1
        P.dma("sp", xt[:], xin[r0:r0 + 128, :])
        P.dma("sp", mt[:], mixed_d[r0:r0 + 128, :])
        P.tt("dve", vtf[:], mt[:], g1p[:], ALU.mult)
        yield
        P.stt("dve", vtf[:], xt[:], ALPHA, vtf[:], ALU.mult, ALU.add)
        yield
        for _ in layer_norm_tile(P, vtf, x1, statsf, mvf, scf, lnw[0], lnw[1]):
            yield
        P.tt("dve", h2[:], x1[:], sc2p[:], ALU.mult)
        yield
        P.tt("dve", h2[:], h2[:], sh2[:], ALU.add)
        yield
        if ti == 0:
            tap("x1", x1, [128, D]); tap("h2", h2, [128, D])
        for half in range(2):
            pt = PS[half]
            for j in range(4):
                kt = half * 4 + j
                P.tr(pt[:, j * 128:(j + 1) * 128], h2[:, kt * 128:(kt + 1) * 128], ident[:, :])
            P.cp("act", V(h2T, h2T.h[:, half * 4:(half + 1) * 4, :].rearrange("p a b -> p (a b)")), pt[:, :])
            yield
        for cj in range(16):
            wb = wqb[wi[0] % 2]
            wi[0] += 1
            P.dma("sp", wb[:], V(wq_d, wq_d.h[:, cj * 128:(cj + 1) * 128].rearrange("(kt p) n -> p kt n", p=128)))
            pq = PS[2 + (cj // 4) % 2]
            for kt in range(8):
                P.mm(pq[:, (cj % 4) * 128:(cj % 4 + 1) * 128], wb[:, kt, :], h2T[:, kt, :], start=(kt == 0), stop=(kt == 7))
                if kt % 2 == 1:
                    yield
            if cj % 4 == 3:
                g4 = cj // 4
                P.cp("act", V(qT, qT.h[:, g4 * 4:(g4 + 1) * 4, :].rearrange("p a b -> p (a b)")), pq[:, :])
        for g4 in range(4):
            psc = PS[4 + g4 % 2]
            for jj in range(4):
                cj = g4 * 4 + jj
                P.mm(psc[:, jj * 128:(jj + 1) * 128], qT[:, cj, :], keysT[:, cj, :])
            P.cp("act" if g4 % 2 else "dve", V(scs, scs.h[:, g4 * 4:(g4 + 1) * 4, :].rearrange("p a b -> p (a b)")), psc[:, :])
            yield
        if ti == 0:
            tap("scs", scs, [128, 16, 128])
        for cj in range(16):
            P.op("dve", lambda e, cj=cj: e.max(out=top.h[:, cj, 0:8], in_=scs.h[:, cj, :]), [scs], [top])
            P.op("dve", lambda e, cj=cj: e.match_replace(out=scs2.h[:, cj, :], in_to_replace=top.h[:, cj, 0:8],
                                                         in_values=scs.h[:, cj, :], imm_value=NEG), [scs, top], [scs2])
            yield
            P.op("dve", lambda e, cj=cj: e.max(out=top.h[:, cj, 8:16], in_=scs2.h[:, cj, :]), [scs2], [top])
            P.op("dve", lambda e, cj=cj: e.max_index(out=idx.h[:, cj, 0:8], in_max=top.h[:, cj, 0:8], in_values=scs.h[:, cj, :]), [scs, top], [idx])
            yield
            P.op("dve", lambda e, cj=cj: e.max_index(out=idx.h[:, cj, 8:16], in_max=top.h[:, cj, 8:16], in_values=scs.h[:, cj, :]), [scs, top], [idx])
            yield
        P.cp("dve", idxf[:], idx[:])
        in0 = V(top, bass.AP(top.h, 0, [[256, 128], [32, 8], [1, 16], [0, 16]]))
        in1 = V(top, bass.AP(top.h, 16, [[256, 128], [32, 8], [0, 16], [1, 16]]))
        P.tt("dve", V(cand, cand.h[:].rearrange("p h (i j) -> p h i j", i=16)), in0, in1, ALU.add)
        yield
        for h in range(8):
            c2v = lambda h=h: scs2.h[:, 2 * h:2 * h + 2, :].rearrange("p a b -> p (a b)")
            P.op("dve", lambda e, h=h: e.max(out=best.h[:, h, 0:8], in_=cand.h[:, h, :]), [cand], [best])
            P.op("dve", lambda e, h=h: e.match_replace(out=c2v(h), in_to_replace=best.h[:, h, 0:8],
                                                       in_values=cand.h[:, h, :], imm_value=NEG), [cand, best], [scs2])
            yield
            P.op("dve", lambda e, h=h: e.max(out=best.h[:, h, 8:16], in_=c2v(h)), [scs2], [best])
            P.op("dve", lambda e, h=h: e.max_index(out=pos.h[:, h, 0:8], in_max=best.h[:, h, 0:8], in_values=cand.h[:, h, :]), [cand, best], [pos])
            yield
            P.op("dve", lambda e, h=h: e.max_index(out=pos.h[:, h, 8:16], in_max=best.h[:, h, 8:16], in_values=cand.h[:, h, :]), [cand, best], [pos])
            yield
        P.op("dve", lambda e: e.tensor_single_scalar(out=pi_.h[:], in_=pos.h[:], scalar=4, op=ALU.logical_shift_right), [pos], [pi_])
        P.op("dve", lambda e: e.tensor_single_scalar(out=pj_.h[:], in_=pos.h[:], scalar=15, op=ALU.bitwise_and), [pos], [pj_])
        yield
        P.cp("dve", pif[:], pi_[:])
        P.cp("dve", pjf[:], pj_[:])
        yield
        io = V(iota, bass.AP(iota.h, 0, [[16, 128], [0, 8], [0, 16], [1, 16]]))
        for (pf, which, dst) in ((pif, 0, i1), (pjf, 1, i2)):
            pfb = V(pf, bass.AP(pf.h, 0, [[128, 128], [16, 8], [1, 16], [0, 16]]))
            P.tt("dve", oh[:], pfb, io, ALU.is_equal)
            yield
            ixb = V(idxf, bass.AP(idxf.h, 16 * which, [[256, 128], [32, 8], [0, 16], [1, 16]]))
            P.tt("dve", oh[:], oh[:], ixb, ALU.mult)
            yield
            P.op("dve", lambda e, dst=dst: e.tensor_reduce(out=dst.h[:], in_=oh.h[:], axis=AX.X, op=ALU.add), [oh], [dst])
            yield
        P.stt("dve", eidf[:], V(i1, i1.h[:].rearrange("p a b -> p (a b)")), 128.0, V(i2, i2.h[:].rearrange("p a b -> p (a b)")), ALU.mult, ALU.add)
        P.cp("dve", eid[:], eidf[:])
        yield
        b0 = V(best, bass.AP(best.h, 0, [[128, 128], [16, 8], [0, 16]]))
        P.tt("dve", gate[:], best[:], b0, ALU.subtract)
        P.act(gate[:], gate[:], AF.Exp)
        yield
        P.op("dve", lambda e: e.tensor_reduce(out=gsum.h[:], in_=gate.h[:], axis=AX.X, op=ALU.add), [gate], [gsum])
        P.recip(gsum[:], gsum[:])
        gs = V(gsum, bass.AP(gsum.h, 0, [[8, 128], [1, 8], [0, 16]]))
        P.tt("dve", gate[:], gate[:], gs, ALU.mult)
        yield
        if ti == 0:
            tap("eidf", eidf, [128, 128]); tap("gate", gate, [128, 8, 16])

    def adv(g, n):
        if g is None:
            return None
        for _ in range(n):
            try:
                next(g)
            except StopIteration:
                return None
        return g

    def gather(ti, fg):
        S = ti % 2
        x1, h2, eid, gate = x1s[S], h2s[S], eids[S], gates[S]
        r0 = ti * 128
        g2p = RB1[3]
        for s in range(128):
            u = nextbuf()
            P.dma("pool", u[:], V(pu, pu.h[:, :]),
                  fn=lambda e, u=u, s=s: e.indirect_dma_start(out=u.h[:], out_offset=None, in_=pu.h[:, :],
                                                             in_offset=bass.IndirectOffsetOnAxis(ap=eid.h[:, s:s + 1], axis=0)),
                  extra=[eid])
            P.op("dve", lambda e, u=u, s=s: e.scalar_tensor_tensor(out=ot.h[:], in0=u.h[:], scalar=1.0, in1=h2.h[:],
                                                                 op0=ALU.mult, op1=ALU.mult, accum_out=z.h[:, s:s + 1]), [u, h2], [ot, z])
            fg = adv(fg, 1)
        P.act(a_[:], z[:], AF.Gelu)
        P.tt("dve", a_[:], a_[:], V(gate, gate.h[:].rearrange("p a b -> p (a b)")), ALU.mult)
        for s in range(128):
            v = nextbuf()
            P.dma("pool", v[:], V(pv, pv.h[:, :]),
                  fn=lambda e, v=v, s=s: e.indirect_dma_start(out=v.h[:], out_offset=None, in_=pv.h[:, :],
                                                             in_offset=bass.IndirectOffsetOnAxis(ap=eid.h[:, s:s + 1], axis=0)),
                  extra=[eid])
            if s % 3 != 0:
                if s == 1:
                    P.ts("dve", acc2[:], v[:], a_[:, s:s + 1], ALU.mult)
                else:
                    P.stt("dve", acc2[:], v[:], a_[:, s:s + 1], acc2[:], ALU.mult, ALU.add)
            else:
                dg = dgs[s % 4]
                P.act(dg[:], ident[:, :], AF.Copy, scale=a_[:, s:s + 1])
                for hf in range(2):
                    P.mm(PS[6 + hf][:, :], dg[:], v[:, hf * 512:(hf + 1) * 512], start=(s == 0), stop=(s == 126))
            fg = adv(fg, 2)
        fg = adv(fg, 100000)
        P.tt("dve", acc[:, 0:512], PS[6][:, :], acc2[:, 0:512], ALU.add)
        P.tt("dve", acc[:, 512:1024], PS[7][:, :], acc2[:, 512:1024], ALU.add)
        if ti == 0:
            tap("ffn", acc, [128, D]); tap("z", z, [128, 128])
        P.tt("dve", vtg[:], acc[:], g2p[:], ALU.mult)
        P.stt("dve", vtg[:], x1[:], ALPHA, vtg[:], ALU.mult, ALU.add)
        for _ in layer_norm_tile(P, vtg, ot, statsg, mvg, scg, lnw[2], lnw[3]):
            pass
        P.dma("sp", out_d[r0:r0 + 128, :], ot[:], src_sem=True)

    adv(front(0), 100000)
    for ti in range(NT):
        nxt = ti + 1
        if nxt < NT and nxt % (SEQ // 128) != 0:
            gather(ti, front(nxt))
        else:
            gather(ti, None)
            if nxt < NT:
                adv(front(nxt), 100000)


_CACHE = {}


def kernel(**inputs):
    shared = _prep_shared(inputs)
    x = np.ascontiguousarray(inputs["x"], dtype=np.float32)
    c = np.ascontiguousarray(inputs["c"], dtype=np.float32)
    in_maps = []
    for core in range(NCORES):
        m = dict(shared)
        m["x"] = x[core * NB:(core + 1) * NB].reshape(TOK, D)
        m["cT"] = np.ascontiguousarray(c[core * NB:(core + 1) * NB].T.reshape(8, 128, NB).transpose(1, 0, 2))
        in_maps.append(m)
    nc = build()
    res = run_bass_kernel_spmd(nc, in_maps, core_ids=list(range(NCORES)))
    out = np.concatenate([r["out"].reshape(NB, SEQ, D) for r in res.results], axis=0)
    return out.astype(np.float32)
```

```python
import math
from contextlib import ExitStack, contextmanager
import numpy as np
import concourse.bass as bass
import concourse.mybir as mybir
from concourse.bass_utils import run_bass_kernel_spmd

F32 = mybir.dt.float32
I32 = mybir.dt.int32
U32 = mybir.dt.uint32
AF = mybir.ActivationFunctionType
ALU = mybir.AluOpType
AX = mybir.AxisListType

NCORES = 8
D = 1024
SEQ = 2048
NB = 2
TOK = NB * SEQ
ALPHA = 2.0 ** 0.25
LN_EPS = 1e-5
RMS_EPS = 1e-6
MID = 31
NEG = -1.0e30


class V:
    __slots__ = ("t", "ap")

    def __init__(self, t, ap):
        self.t = t
        self.ap = ap


class T:
    def __init__(self, h, name):
        self.h = h
        self.name = name
        self.w = None
        self.wd = {}
        self.r = {}
        self.dsem = None
        self.dcnt = 0

    def __getitem__(self, idx):
        return V(self, self.h[idx])

    def cust(self, offset, dims):
        return V(self, bass.AP(self.h, offset, [list(d) for d in dims]))


class Prog:
    def __init__(self, nc):
        self.nc = nc
        self.root = ExitStack()
        self.stacks = [self.root]
        self.E = {"pe": nc.tensor, "act": nc.scalar, "dve": nc.vector, "pool": nc.gpsimd, "sp": nc.sync}
        self.sem = {k: self.root.enter_context(nc.semaphore("s_" + k)) for k in ("pe", "act", "dve", "pool")}
        self.cnt = {k: 0 for k in self.sem}
        self.waited = {k: {} for k in self.E}
        self.dma_tiles = []
        self.nname = 0

    def sb(self, name, shape, dt=F32):
        self.nname += 1
        h = self.stacks[-1].enter_context(self.nc.sbuf_tensor("%s_%d" % (name, self.nname), list(shape), dt))
        return T(h, name)

    def ps(self, name):
        self.nname += 1
        h = self.stacks[-1].enter_context(self.nc.psum_tensor("%s_%d" % (name, self.nname), [128, 512], F32))
        return T(h, name)

    def dram(self, name, shape, dt=F32, kind="Internal"):
        h = self.nc.dram_tensor(name, list(shape), dt, kind=kind)
        return T(h, name)

    @contextmanager
    def scope(self):
        st = ExitStack()
        self.stacks.append(st)
        try:
            yield
        finally:
            self.barrier()
            self.stacks.pop()
            st.close()

    def _wait(self, eng, evs):
        need = {}
        for ev in evs:
            if ev is None:
                continue
            s, v = ev
            k = id(s)
            if k not in need or need[k][1] < v:
                need[k] = (s, v)
        for k, (s, v) in need.items():
            if eng == "pe" and s is self.sem["pe"]:
                continue
            if self.waited[eng].get(k, 0) >= v:
                continue
            self.E[eng].wait_ge(s, v)
            self.waited[eng][k] = v

    def barrier(self):
        evs = [(self.sem[k], self.cnt[k]) for k in self.sem if self.cnt[k] > 0]
        evs += [(t.dsem, t.dcnt) for t in self.dma_tiles if t.dcnt > 0]
        for eng in self.E:
            self._wait(eng, evs)

    def op(self, eng, fn, reads=(), writes=()):
        evs = []
        for t in reads:
            evs.append(t.w)
            evs.extend(t.wd.values())
        for t in writes:
            evs.append(t.w)
            evs.extend(t.wd.values())
            evs.extend(t.r.values())
        self._wait(eng, evs)
        inst = fn(self.E[eng])
        self.cnt[eng] += 1
        inst.then_inc(self.sem[eng], 1)
        ev = (self.sem[eng], self.cnt[eng])
        for t in writes:
            t.w = ev
            t.wd = {}
            t.r = {}
        for t in reads:
            if t not in writes:
                t.r[eng] = ev
        return inst

    def dma(self, q, o, i, fn=None, extra=(), src_sem=False):
        ot, it = o.t, i.t
        st = it if src_sem else ot
        evs = [it.w] + list(it.wd.values()) + [t.w for t in extra]
        if ot.w is not None and not (st.dsem is not None and ot.w[0] is st.dsem):
            evs.append(ot.w)
        for k, ev in ot.wd.items():
            if not (st.dsem is not None and ev[0] is st.dsem):
                evs.append(ev)
        evs.extend(ot.r.values())
        self._wait(q, evs)
        if st.dsem is None:
            self.nname += 1
            st.dsem = self.root.enter_context(self.nc.semaphore("d%d" % self.nname))
            self.dma_tiles.append(st)
        if fn is None:
            inst = self.E[q].dma_start(out=o.ap, in_=i.ap)
        else:
            inst = fn(self.E[q])
        st.dcnt += 16
        inst.then_inc(st.dsem, 16)
        ev = (st.dsem, st.dcnt)
        if src_sem:
            ot.wd[id(st.dsem)] = ev
        else:
            ot.w = ev
            ot.wd = {}
        ot.r = {}
        it.r["d%d" % id(st)] = ev
        for t in extra:
            t.r["d%d" % id(st)] = ev
        return inst

    @staticmethod
    def _sv(x, reads):
        if isinstance(x, V):
            reads.append(x.t)
            return x.ap
        return x

    def tt(self, eng, o, a, b, op):
        return self.op(eng, lambda e: e.tensor_tensor(out=o.ap, in0=a.ap, in1=b.ap, op=op), [a.t, b.t], [o.t])

    def ts(self, eng, o, a, s1, op0, s2=None, op1=None):
        reads = [a.t]
        s1a = self._sv(s1, reads)
        s2a = self._sv(s2, reads)
        if op1 is None:
            return self.op(eng, lambda e: e.tensor_scalar(out=o.ap, in0=a.ap, scalar1=s1a, scalar2=None, op0=op0), reads, [o.t])
        return self.op(eng, lambda e: e.tensor_scalar(out=o.ap, in0=a.ap, scalar1=s1a, scalar2=s2a, op0=op0, op1=op1), reads, [o.t])

    def stt(self, eng, o, a, s, b, op0, op1):
        reads = [a.t, b.t]
        sa = self._sv(s, reads)
        return self.op(eng, lambda e: e.scalar_tensor_tensor(out=o.ap, in0=a.ap, scalar=sa, in1=b.ap, op0=op0, op1=op1), reads, [o.t])

    def cp(self, eng, o, a):
        if eng == "act":
            return self.op(eng, lambda e: e.copy(out=o.ap, in_=a.ap), [a.t], [o.t])
        return self.op(eng, lambda e: e.tensor_copy(out=o.ap, in_=a.ap), [a.t], [o.t])

    def act(self, o, a, func, bias=None, scale=None, accum=None):
        reads = [a.t]
        kw = {}
        if bias is not None:
            kw["bias"] = self._sv(bias, reads)
        if scale is not None:
            kw["scale"] = self._sv(scale, reads)
        writes = [o.t]
        if accum is not None:
            kw["accum_out"] = accum.ap
            writes.append(accum.t)
        return self.op("act", lambda e: e.activation(out=o.ap, in_=a.ap, func=func, **kw), reads, writes)

    def mm(self, o, l, r, start=True, stop=True, tp=None):
        if tp is None:
            return self.op("pe", lambda e: e.matmul(o.ap, l.ap, r.ap, start=start, stop=stop), [l.t, r.t], [o.t])
        return self.op("pe", lambda e: e.matmul(o.ap, l.ap, r.ap, start=start, stop=stop, tile_position=tp), [l.t, r.t], [o.t])

    def tr(self, o, a, ident):
        return self.op("pe", lambda e: e.transpose(o.ap, a.ap, ident.ap), [a.t, ident.t], [o.t])

    def recip(self, o, a):
        return self.op("dve", lambda e: e.reciprocal(out=o.ap, in_=a.ap), [a.t], [o.t])


def _consts():
    s = np.arange(64)[:, None]
    t = np.arange(64)[None, :]
    le = (s <= t).astype(np.float32)
    lm = (s <= MID).astype(np.float32) * np.ones((1, 64), np.float32)
    A1 = le - lm
    A2 = lm - le
    A3 = (s > t).astype(np.float32)
    z = np.zeros((64, 64), np.float32)
    bd = lambda a: np.block([[a, z], [z, a]]).astype(np.float32)
    RB = np.zeros((128, 132), np.float32)
    RB[0:64, 0:64] = A1
    RB[64:128, 64:128] = A1
    RB[0:64, 128] = 1.0
    RB[0:64, 129] = lm[:, 0]
    RB[64:128, 130] = 1.0
    RB[64:128, 131] = lm[:, 0]
    sel = np.zeros((2, 2, 128), np.float32)
    sel[0, 0, :] = 1.0
    sel[1, 1, :] = 1.0
    return {
        "k_ident": np.eye(128, dtype=np.float32),
        "k_a2": bd(A2),
        "k_a3": bd(A3),
        "k_rb": RB,
        "k_mask": bd(le),
        "k_iota": np.broadcast_to(np.arange(16, dtype=np.float32), (128, 16)).copy(),
        "k_ones": np.ones((128, 128), np.float32),
        "k_sel": sel,
        "k_selj": _selj(),
        "k_cmask": np.kron((np.arange(4)[:, None] <= np.arange(4)[None, :]).astype(np.float32), np.ones((32, 32), np.float32)),
    }


def _selj():
    a = np.zeros((128, 16, 128), np.float32)
    for j in range(4):
        for sl in range(4):
            for c in range(32):
                a[32 * j + c, j * 4 + sl, 32 * sl + c] = 1.0
    return a


def _prep_shared(inp):
    f = lambda a: np.ascontiguousarray(a, dtype=np.float32)
    o = {}
    o["ada_w"] = f(inp["ada_w"][0])
    o["ada_b2"] = f(np.broadcast_to(inp["ada_b"][0][None, :], (2, 6 * D)))
    o["w_in"] = f(inp["w_in"][0])
    o["hb"] = f(inp["hg_lower_bound"])
    col = lambda v, n: f(np.asarray(v).reshape(n, 128).T)
    o["hg_nw"] = col(inp["hg_norm_w"][0], 4)
    o["s_ar"] = col(inp["ssm_a_re"][0], 16)
    o["s_ai"] = col(inp["ssm_a_im"][0], 16)
    o["s_ldt"] = col(np.repeat(inp["ssm_log_dt"][0], 64), 16)
    bre, bim = inp["ssm_b_re"][0], inp["ssm_b_im"][0]
    cre, cim = inp["ssm_c_re"][0], inp["ssm_c_im"][0]
    BTr = np.zeros((512, 128), np.float32)
    BTi = np.zeros((512, 128), np.float32)
    CTr = np.zeros((16, 128, 128), np.float32)
    CTi = np.zeros((16, 128, 128), np.float32)
    for g in range(32):
        gl = g % 2
        st = g // 2
        j = st % 4
        BTr[g * 16:(g + 1) * 16, gl * 64:gl * 64 + 64] = bre[g].T
        BTi[g * 16:(g + 1) * 16, gl * 64:gl * 64 + 64] = bim[g].T
        CTr[st, gl * 64:gl * 64 + 64, 32 * j + gl * 16:32 * j + gl * 16 + 16] = cre[g].T
        CTi[st, gl * 64:gl * 64 + 64, 32 * j + gl * 16:32 * j + gl * 16 + 16] = cim[g].T
    o["s_btr"] = f(BTr.reshape(4, 128, 128).transpose(1, 0, 2))
    o["s_bti"] = f(BTi.reshape(4, 128, 128).transpose(1, 0, 2))
    o["s_ctr"] = f(CTr.transpose(1, 0, 2))
    o["s_cti"] = f(CTi.transpose(1, 0, 2))
    o["s_d"] = col(inp["ssm_d"][0].reshape(512), 4)
    BM_r = np.zeros((16, 128, 32), np.float32); BM_i = np.zeros((16, 128, 32), np.float32)
    CM_r = np.zeros((16, 128, 32), np.float32); CM_i = np.zeros((16, 128, 32), np.float32)
    for g in range(32):
        gl = g % 2
        st = g // 2
        BM_r[st, gl * 64:gl * 64 + 64, gl * 16:gl * 16 + 16] = bre[g]
        BM_i[st, gl * 64:gl * 64 + 64, gl * 16:gl * 16 + 16] = bim[g]
        CM_r[st, gl * 64:gl * 64 + 64, gl * 16:gl * 16 + 16] = cre[g].T
        CM_i[st, gl * 64:gl * 64 + 64, gl * 16:gl * 16 + 16] = cim[g].T
    o["s_bmr"] = f(BM_r.transpose(1, 0, 2)); o["s_bmi"] = f(BM_i.transpose(1, 0, 2))
    o["s_cmr"] = f(CM_r.transpose(1, 0, 2)); o["s_cmi"] = f(CM_i.transpose(1, 0, 2))
    dd = inp["ssm_d"][0].reshape(16, 32)
    o["s_drep"] = f(np.tile(dd, (1, 4)).T)
    o["glu_w"] = f(inp["ssm_glu_w"][0])
    o["glu_b"] = col(inp["ssm_glu_b"][0], 4)
    o["s_nw"] = col(inp["ssm_norm_w"][0], 4)
    o["w_out"] = f(inp["w_out"][0])
    o["ln1w"] = f(inp["ln1_w"])
    o["ln1b"] = f(inp["ln1_b"])
    o["ln2w"] = f(inp["ln2_w"])
    o["ln2b"] = f(inp["ln2_b"])
    o["w_q"] = f(inp["peer_w_q"][0])
    o["keysT"] = f(inp["peer_sub_keys"][0].transpose(3, 0, 1, 2).reshape(128, 16, 128))
    o["pu"] = f(inp["peer_u"][0])
    o["pv"] = f(inp["peer_v"][0])
    o.update(_consts())
    return o


SHAPES = {
    "x": [TOK, D], "cT": [128, 8, 2], "ada_w": [D, 6 * D], "ada_b2": [2, 6 * D], "w_in": [D, 2560], "hb": [2, 512],
    "hg_nw": [128, 4], "s_ar": [128, 16], "s_ai": [128, 16], "s_ldt": [128, 16], "s_btr": [128, 4, 128],
    "s_bti": [128, 4, 128], "s_ctr": [128, 16, 128], "s_cti": [128, 16, 128], "s_d": [128, 4], "glu_w": [512, 512],
    "glu_b": [128, 4], "s_nw": [128, 4], "w_out": [D, D], "ln1w": [1, D], "ln1b": [1, D], "ln2w": [1, D],
    "ln2b": [1, D], "w_q": [D, 2048], "keysT": [128, 16, 128], "pu": [16384, D], "pv": [16384, D],
    "k_ident": [128, 128], "k_a2": [128, 128], "k_a3": [128, 128], "k_rb": [128, 132], "k_mask": [128, 128],
    "k_iota": [128, 16], "k_ones": [128, 128], "k_sel": [2, 2, 128], "k_selj": [128, 16, 128], "k_cmask": [128, 128],
    "s_bmr": [128, 16, 32], "s_bmi": [128, 16, 32], "s_cmr": [128, 16, 32], "s_cmi": [128, 16, 32], "s_drep": [128, 16],
}


def build(upto=99, taps=()):
    nc = bass.Bass("TRN2", target_bir_lowering=False)
    P = Prog(nc)
    I = {k: P.dram(k, v, F32, kind="ExternalInput") for k, v in SHAPES.items()}
    out_d = P.dram("out", [TOK, D], F32, kind="ExternalOutput")
    mixed_d = P.dram("mixed_scr", [TOK, D], F32, kind="Internal")
    tapd = {}

    def tap(name, src_t, shape):
        if name in taps:
            td = P.dram("tap_" + name, shape, F32, kind="ExternalOutput")
            tapd[name] = td
            P.dma("sp", td[tuple(slice(None) for _ in shape)], src_t[tuple(slice(None) for _ in shape)])

    def ld(dst, src, q="sp"):
        P.dma(q, dst, src)

    def bc_row(tname, rows=128):
        t = I[tname]
        n = SHAPES[tname][1]
        return V(t, bass.AP(t.h, 0, [[0, rows], [1, n]]))

    ident = P.sb("ident", [128, 128]); ld(ident[:], I["k_ident"][:, :])
    ones = P.sb("ones", [128, 128]); ld(ones[:], I["k_ones"][:, :])
    PS = [P.ps("ps%d" % i) for i in range(8)]
    sh1T = P.sb("sh1T", [128, 8, 2])
    sc1T = P.sb("sc1T", [128, 8, 2])
    mod_d = P.dram("mod_scr", [2, 6 * D], F32, kind="Internal")

    with P.scope():
        cT = P.sb("cT", [128, 8, 2]); ld(cT[:], I["cT"][:, :, :])
        condT = P.sb("condT", [128, 8, 2])
        P.act(condT[:], cT[:], AF.Silu)
        adab = P.sb("adab", [2, 6 * D]); ld(adab[:], I["ada_b2"][:, :])
        mod = P.sb("mod", [2, 6 * D])
        sel = P.sb("sel", [2, 2, 128]); ld(sel[:], I["k_sel"][:, :, :])
        wab = [P.sb("wab%d" % i, [128, 8, 512]) for i in range(2)]
        aw = I["ada_w"]
        for cb in range(12):
            wb = wab[cb % 2]
            src = V(aw, aw.h[:, cb * 512:(cb + 1) * 512].rearrange("(kt p) n -> p kt n", p=128))
            ld(wb[:], src)
            for kt in range(8):
                P.mm(PS[cb % 2][0:2, :], condT[:, kt, :], wb[:, kt, :], start=(kt == 0), stop=(kt == 7))
            P.tt("dve", mod[:, cb * 512:(cb + 1) * 512], PS[cb % 2][0:2, :], adab[:, cb * 512:(cb + 1) * 512], ALU.add)
        tap("mod", mod, [2, 6 * D])
        for j in range(16):
            P.tr(PS[2][:, 2 * j:2 * j + 2], mod[:, j * 128:(j + 1) * 128], ident[0:2, 0:2])
        P.cp("dve", V(sh1T, sh1T.h[:].rearrange("p a b -> p (a b)")), PS[2][:, 0:16])
        P.ts("dve", V(sc1T, sc1T.h[:].rearrange("p a b -> p (a b)")), PS[2][:, 16:32], 1.0, ALU.add)
        P.dma("sp", mod_d[:, :], mod[:])
    if upto <= 0:
        return finish(nc, P, out_d)

    with P.scope():
        a2 = P.sb("a2", [128, 128]); ld(a2[:], I["k_a2"][:, :])
        a3 = P.sb("a3", [128, 128]); ld(a3[:], I["k_a3"][:, :])
        rb = P.sb("rbm", [128, 132]); ld(rb[:], I["k_rb"][:, :])
        mask = P.sb("mask", [128, 128]); ld(mask[:], I["k_mask"][:, :])
        hgnw = P.sb("hgnw", [128, 4]); ld(hgnw[:], I["hg_nw"][:, :])
        lbb = P.sb("lbb", [128, 512])
        omlb = P.sb("omlb", [128, 512])
        with P.scope():
            h0 = P.sb("h0", [128, 512]); h1 = P.sb("h1", [128, 512])
            hbt = I["hb"]
            ld(h0[:], V(hbt, bass.AP(hbt.h, 0, [[0, 128], [1, 512]])))
            ld(h1[:], V(hbt, bass.AP(hbt.h, 512, [[0, 128], [1, 512]])))
            P.tt("dve", h0[:], h0[:], h1[:], ALU.subtract)
            P.act(lbb[:], h0[:], AF.Sigmoid)
            P.ts("dve", omlb[:], lbb[:], -1.0, ALU.mult, 1.0, ALU.add)
        s_zre = P.sb("s_zre", [128, 16]); s_zim = P.sb("s_zim", [128, 16]); s_zimn = P.sb("s_zimn", [128, 16])
        Lre = P.sb("Lre", [128, 11, 16]); Lim = P.sb("Lim", [128, 11, 16]); Limn = P.sb("Limn", [128, 11, 16])
        PWr = P.sb("PWr", [128, 9, 16]); PWi = P.sb("PWi", [128, 9, 16])
        PWDr = P.sb("PWDr", [128, 8, 16]); PWDi = P.sb("PWDi", [128, 8, 16])
        IPr = P.sb("IPr", [128, 8, 16]); IPi = P.sb("IPi", [128, 8, 16])
        LCr = P.sb("LCr", [128, 8, 16]); LCi = P.sb("LCi", [128, 8, 16]); LCin = P.sb("LCin", [128, 8, 16])
        S5T = dict(PWr=PWr, PWi=PWi, PWDr=PWDr, PWDi=PWDi, IPr=IPr, IPi=IPi, LCr=LCr, LCi=LCi, LCin=LCin)
        sdk = P.sb("sdk", [128, 4]); ld(sdk[:], I["s_d"][:, :])
        glub = P.sb("glub", [128, 4]); ld(glub[:], I["glu_b"][:, :])
        snw = P.sb("snw", [128, 4]); ld(snw[:], I["s_nw"][:, :])
        with P.scope():
            ar = P.sb("ar", [128, 16]); ld(ar[:], I["s_ar"][:, :])
            ai = P.sb("ai", [128, 16]); ld(ai[:], I["s_ai"][:, :])
            dt = P.sb("dt", [128, 16]); ld(dt[:], I["s_ldt"][:, :])
            t1 = P.sb("t1", [128, 16]); t2 = P.sb("t2", [128, 16]); t3 = P.sb("t3", [128, 16])
            cc = P.sb("cc", [128, 16]); ss = P.sb("ss", [128, 16]); mag = P.sb("mag", [128, 16])
            P.act(dt[:], dt[:], AF.Exp)
            P.tt("dve", t1[:], ar[:], dt[:], ALU.mult)
            P.act(mag[:], t1[:], AF.Exp)
            P.tt("dve", t2[:], ai[:], dt[:], ALU.mult)
            P.ts("dve", t2[:], t2[:], 1.0 / 16.0, ALU.mult)
            P.act(ss[:], t2[:], AF.Sin)
            P.ts("dve", t3[:], t2[:], -1.0, ALU.mult, math.pi / 2.0, ALU.add)
            P.act(cc[:], t3[:], AF.Sin)
            for _ in range(4):
                P.tt("dve", t1[:], cc[:], cc[:], ALU.mult)
                P.tt("dve", t3[:], ss[:], ss[:], ALU.mult)
                P.tt("dve", t2[:], ss[:], cc[:], ALU.mult)
                P.tt("dve", cc[:], t1[:], t3[:], ALU.subtract)
                P.ts("dve", ss[:], t2[:], 2.0, ALU.mult)
            P.tt("dve", Lre[:, 0, :], mag[:], cc[:], ALU.mult)
            P.tt("dve", Lim[:, 0, :], mag[:], ss[:], ALU.mult)
            den = P.sb("den", [128, 16]); nr = P.sb("nr", [128, 16])
            P.tt("dve", t1[:], ar[:], ar[:], ALU.mult)
            P.tt("dve", t2[:], ai[:], ai[:], ALU.mult)
            P.tt("dve", den[:], t1[:], t2[:], ALU.add)
            P.recip(den[:], den[:])
            P.ts("dve", nr[:], Lre[:, 0, :], -1.0, ALU.add)
            P.tt("dve", t1[:], nr[:], ar[:], ALU.mult)
            P.tt("dve", t2[:], Lim[:, 0, :], ai[:], ALU.mult)
            P.tt("dve", t1[:], t1[:], t2[:], ALU.add)
            P.tt("dve", s_zre[:], t1[:], den[:], ALU.mult)
            P.tt("dve", t1[:], Lim[:, 0, :], ar[:], ALU.mult)
            P.tt("dve", t2[:], nr[:], ai[:], ALU.mult)
            P.tt("dve", t1[:], t1[:], t2[:], ALU.subtract)
            P.tt("dve", s_zim[:], t1[:], den[:], ALU.mult)
            P.ts("dve", s_zimn[:], s_zim[:], -1.0, ALU.mult)
            for k in range(1, 11):
                P.tt("dve", t1[:], Lre[:, k - 1, :], Lre[:, k - 1, :], ALU.mult)
                P.tt("dve", t2[:], Lim[:, k - 1, :], Lim[:, k - 1, :], ALU.mult)
                P.tt("dve", t3[:], Lre[:, k - 1, :], Lim[:, k - 1, :], ALU.mult)
                P.tt("dve", Lre[:, k, :], t1[:], t2[:], ALU.subtract)
                P.ts("dve", Lim[:, k, :], t3[:], 2.0, ALU.mult)
            P.ts("dve", Limn[:], Lim[:], -1.0, ALU.mult)
            P.op("dve", lambda e: e.memset(PWr.h[:, 0, :], 1.0), [], [PWr])
            P.op("dve", lambda e: e.memset(PWi.h[:, 0, :], 0.0), [], [PWi])
            for k in range(1, 9):
                cmul_s(P, PWr[:, k, :], PWi[:, k, :], PWr[:, k - 1, :], PWi[:, k - 1, :], Lre[:, 0, :], Lim[:, 0, :], t1, t2)
            for sx in range(8):
                P.cp("dve", PWDr[:, sx, :], PWr[:, 7 - sx, :])
                P.cp("dve", PWDi[:, sx, :], PWi[:, 7 - sx, :])
                P.tt("dve", t1[:], PWr[:, sx, :], PWr[:, sx, :], ALU.mult)
                P.tt("dve", t2[:], PWi[:, sx, :], PWi[:, sx, :], ALU.mult)
                P.tt("dve", t1[:], t1[:], t2[:], ALU.add)
                P.recip(t1[:], t1[:])
                P.tt("dve", IPr[:, sx, :], PWr[:, sx, :], t1[:], ALU.mult)
                P.tt("dve", t2[:], PWi[:, sx, :], t1[:], ALU.mult)
                P.ts("dve", IPi[:, sx, :], t2[:], -1.0, ALU.mult)
            P.cp("dve", LCr[:, 0, :], PWr[:, 8, :])
            P.cp("dve", LCi[:, 0, :], PWi[:, 8, :])
            for k in range(1, 8):
                cmul_s(P, LCr[:, k, :], LCi[:, k, :], LCr[:, k - 1, :], LCi[:, k - 1, :], LCr[:, k - 1, :], LCi[:, k - 1, :], t1, t2)
            P.ts("dve", LCin[:], LCi[:], -1.0, ALU.mult)
        tap("Lre", Lre, [128, 11, 16]); tap("Lim", Lim, [128, 11, 16]); tap("zre", s_zre, [128, 16]); tap("zim", s_zim, [128, 16])

        for b in range(NB):
            with P.scope():
                ohg = P.sb("ohg", [128, 4, SEQ])
                uT = P.sb("uT", [128, 4, SEQ])
                with P.scope():
                    stage1(P, I, PS, b, ident, ones, sh1T, sc1T, a2, a3, rb, mask, hgnw, lbb, omlb, ohg, uT, tap)
                if upto <= 1:
                    continue
                yT = P.sb("yT", [128, 4, SEQ])
                if True:
                    with P.scope():
                        stage2c(P, I, PS, ident, uT, yT, s_zre, s_zim, s_zimn, S5T, tap, b)
                if upto <= 2:
                    continue
                with P.scope():
                    stage3a(P, I, PS, b, ones, ohg, yT, glub, snw, mixed_d, tap)
    if upto <= 3:
        if "mixed" in taps:
            td = P.dram("tap_mixed", [TOK, D], F32, kind="ExternalOutput")
            with P.scope():
                tmp = P.sb("tmpm", [128, D])
                for i in range(TOK // 128):
                    P.dma("sp", tmp[:], mixed_d[i * 128:(i + 1) * 128, :])
                    P.dma("sp", td[i * 128:(i + 1) * 128, :], tmp[:])
        return finish(nc, P, out_d)

    with P.scope():
        stage3b(P, I, PS, ident, mod_d, mixed_d, out_d, bc_row, tap, upto)
    return finish(nc, P, out_d)


def finish(nc, P, out_d):
    P.barrier()
    P.root.close()
    return nc


def stage1(P, I, PS, b, ident, ones, sh1T, sc1T, a2, a3, rb, mask, hgnw, lbb, omlb, ohg, uT, tap):
    xin = I["x"]
    win = I["w_in"]
    hT = P.sb("hT", [128, 8, 512])
    xt = [P.sb("xt%d" % i, [128, D]) for i in range(2)]
    wbuf = [P.sb("wbuf%d" % i, [128, 8, 512]) for i in range(2)]
    qsT = P.sb("qsT", [128, 4, 512])
    gsT = P.sb("gsT", [128, 4, 512])
    lf = P.sb("lf", [128, 4, 512])
    omf = P.sb("omf", [128, 4, 512])
    vv = P.sb("vv", [128, 4, 512])
    tmpa = P.sb("tmpa", [128, 512])
    tmpb = P.sb("tmpb", [128, 512])
    S = P.sb("S", [128, 4, 128])
    Smid = P.sb("Smid", [128, 4, 128])
    Stmp = P.sb("Stmp", [128, 4, 128])
    Esb = P.sb("Esb", [128, 4, 132])
    EK = P.sb("EK", [128, 512])
    EH = P.sb("EH", [128, 512])
    Kt = P.sb("Kt", [128, 512])
    Kh = P.sb("Kh", [128, 512])
    QT = P.sb("QT", [128, 4, 128])
    KTT = P.sb("KTT", [128, 4, 128])
    ST = P.sb("ST", [128, 4, 128])
    P.op("dve", lambda e: e.memset(S.h[:], 0.0), [], [S])
    wi = 0
    for tb in range(4):
        t0 = b * SEQ + tb * 512
        for tt_ in range(4):
            xb = xt[tt_ % 2]
            P.dma("sp", xb[:], xin[t0 + tt_ * 128: t0 + (tt_ + 1) * 128, :])
            for half in range(2):
                pt = PS[half]
                for j in range(4):
                    kt = half * 4 + j
                    P.tr(pt[:, j * 128:(j + 1) * 128], xb[:, kt * 128:(kt + 1) * 128], ident[:, :])
                for j in range(4):
                    kt = half * 4 + j
                    P.act(hT[:, kt, tt_ * 128:(tt_ + 1) * 128], pt[:, j * 128:(j + 1) * 128], AF.Identity,
                          bias=sh1T[:, kt, b:b + 1], scale=sc1T[:, kt, b:b + 1])
        if tb == 0 and b == 0:
            tap("hT", hT, [128, 8, 512])
        for grp in (0, 3, 4, 1, 2):
            wb = wbuf[wi % 2]
            wi += 1
            src = V(win, win.h[:, grp * 512:(grp + 1) * 512].rearrange("(kt p) n -> p kt n", p=128))
            P.dma("sp", wb[:], src)
            if grp in (0, 3, 4):
                for ct in range(4):
                    pt = PS[2 + ct % 2]
                    for kt in range(8):
                        P.mm(pt[:, :], wb[:, kt, ct * 128:(ct + 1) * 128], hT[:, kt, :], start=(kt == 0), stop=(kt == 7))
                    if grp == 0:
                        P.act(qsT[:, ct, :], pt[:, :], AF.Silu)
                    elif grp == 3:
                        P.act(gsT[:, ct, :], pt[:, :], AF.Silu)
                    else:
                        P.cp("dve", uT[:, ct, tb * 512:(tb + 1) * 512], pt[:, :])
            else:
                for tt_ in range(4):
                    pt = PS[2 + tt_ % 2]
                    for kt in range(8):
                        P.mm(pt[:, :], hT[:, kt, tt_ * 128:(tt_ + 1) * 128], wb[:, kt, :], start=(kt == 0), stop=(kt == 7))
                    if grp == 1:
                        P.act(tmpa[:], pt[:, :], AF.Sigmoid)
                        P.tt("dve", tmpa[:], tmpa[:], omlb[:], ALU.mult)
                        P.tt("dve", tmpa[:], tmpa[:], lbb[:], ALU.add)
                        P.act(lf[:, tt_, :], tmpa[:], AF.Ln)
                        P.ts("dve", omf[:, tt_, :], tmpa[:], -1.0, ALU.mult, 1.0, ALU.add)
                    else:
                        P.cp("dve", vv[:, tt_, :], pt[:, :])
        if tb == 0 and b == 0:
            tap("qsT", qsT, [128, 4, 512]); tap("lf", lf, [128, 4, 512]); tap("vv", vv, [128, 4, 512])
        pK, pH, pB0, pB1, pT, pS, pO, pD = PS[0], PS[1], PS[2], PS[3], PS[4], PS[5], PS[6], PS[7]
        for tt_ in range(4):
            P.mm(pK[:, :], a2[:, :], lf[:, tt_, :])
            P.mm(pH[:, :], a3[:, :], lf[:, tt_, :])
            P.act(EK[:], pK[:, :], AF.Exp)
            P.act(EH[:], pH[:, :], AF.Exp)
            P.tt("dve", Kt[:], omf[:, tt_, :], EK[:], ALU.mult)
            P.tt("dve", Kh[:], omf[:, tt_, :], EH[:], ALU.mult)
            for h in range(4):
                pb = pB0 if h < 2 else pB1
                P.mm(pb[:, (h % 2) * 132:(h % 2) * 132 + 132], lf[:, tt_, h * 128:(h + 1) * 128], rb[:, :])
            P.act(Esb[:, 0:2, :], V(pB0, pB0.h[:, 0:264].rearrange("p (a b) -> p a b", a=2)), AF.Exp)
            P.act(Esb[:, 2:4, :], V(pB1, pB1.h[:, 0:264].rearrange("p (a b) -> p a b", a=2)), AF.Exp)
            P.tt("dve", QT[:], qsT[:, :, tt_ * 128:(tt_ + 1) * 128], Esb[:, :, 0:128], ALU.mult)
            for h in range(4):
                P.tr(pT[:, h * 128:(h + 1) * 128], Kt[:, h * 128:(h + 1) * 128], ident[:, :])
            P.cp("act", V(KTT, KTT.h[:].rearrange("p a b -> p (a b)")), pT[:, :])
            for h in range(4):
                P.mm(pS[:, h * 128:(h + 1) * 128], KTT[:, h, :], QT[:, h, :])
            mk = V(mask, bass.AP(mask.h, 0, [[128, 128], [0, 4], [1, 128]]))
            P.tt("dve", ST[:], V(pS, pS.h[:, :].rearrange("p (a b) -> p a b", a=4)), mk, ALU.mult)
            if tb == 0 and b == 0 and tt_ == 0:
                tap("Esb", Esb, [128, 4, 132]); tap("ST", ST, [128, 4, 128]); tap("QT", QT, [128, 4, 128]); tap("KTT", KTT, [128, 4, 128])
            for j in range(2):
                em = V(Esb, bass.AP(Esb.h, 129 + 2 * j, [[528, 128], [132, 4], [0, 128]]))
                P.tt("dve", Smid[:], S[:], em, ALU.mult)
                for h in range(4):
                    oc = pO[:, h * 128 + j * 64: h * 128 + j * 64 + 64]
                    P.mm(oc, vv[:, tt_, h * 128:(h + 1) * 128], ST[:, h, j * 64:(j + 1) * 64], start=True, stop=False)
                    P.mm(oc, Smid[:, h, :], QT[:, h, j * 64:(j + 1) * 64], start=False, stop=True)
                for h in range(4):
                    P.mm(pD[:, h * 128:(h + 1) * 128], Kh[j * 64:(j + 1) * 64, h * 128:(h + 1) * 128],
                         vv[j * 64:(j + 1) * 64, tt_, h * 128:(h + 1) * 128])
                dc = V(Esb, bass.AP(Esb.h, 128 + 2 * j, [[528, 128], [132, 4], [0, 128]]))
                P.tt("dve", Stmp[:], S[:], dc, ALU.mult)
                P.tt("dve", S[:], Stmp[:], V(pD, pD.h[:, :].rearrange("p (a b) -> p a b", a=4)), ALU.add)
            tc0 = tb * 512 + tt_ * 128
            P.cp("act", ohg[:, :, tc0:tc0 + 128], V(pO, pO.h[:, :].rearrange("p (a b) -> p a b", a=4)))
        if tb == 0 and b == 0:
            tap("oraw", ohg, [128, 4, SEQ])
        cs = slice(tb * 512, (tb + 1) * 512)
        for h in range(4):
            pn = PS[h % 2]
            P.act(tmpa[:], ohg[:, h, cs], AF.Square)
            P.mm(pn[:, :], ones[:, :], tmpa[:])
            P.act(tmpb[:], pn[:, :], AF.Sqrt, bias=RMS_EPS, scale=1.0 / 128.0)
            P.recip(tmpb[:], tmpb[:])
            P.stt("dve", tmpa[:], ohg[:, h, cs], hgnw[:, h:h + 1], tmpb[:], ALU.mult, ALU.mult)
            P.tt("dve", ohg[:, h, cs], tmpa[:], gsT[:, h, :], ALU.mult)
    if b == 0:
        tap("ohg", ohg, [128, 4, SEQ]); tap("uT", uT, [128, 4, SEQ])


def cmul_s(P, outr, outi, ar, ai, br, bi, t1, t2):
    P.tt("dve", t1[:], ar, br, ALU.mult)
    P.tt("dve", t2[:], ai, bi, ALU.mult)
    P.tt("dve", outr, t1[:], t2[:], ALU.subtract)
    P.tt("dve", t1[:], ar, bi, ALU.mult)
    P.tt("dve", t2[:], ai, br, ALU.mult)
    P.tt("dve", outi, t1[:], t2[:], ALU.add)


def stage2c(P, I, PS, ident, uT, yT, zre, zim, zimn, TB, tap, b):
    NCH = SEQ // 8
    bmr = P.sb("bmr", [128, 16, 32]); P.dma("sp", bmr[:], I["s_bmr"][:, :, :])
    bmi = P.sb("bmi", [128, 16, 32]); P.dma("sp", bmi[:], I["s_bmi"][:, :, :])
    cmr = P.sb("cmr", [128, 16, 32]); P.dma("sp", cmr[:], I["s_cmr"][:, :, :])
    cmi = P.sb("cmi", [128, 16, 32]); P.dma("sp", cmi[:], I["s_cmi"][:, :, :])
    drep = P.sb("drep", [128, 16]); P.dma("sp", drep[:], I["s_drep"][:, :])
    selj = P.sb("selj", [128, 16, 128]); P.dma("sp", selj[:], I["k_selj"][:, :, :])
    cmask = P.sb("cmask", [128, 128]); P.dma("sp", cmask[:], I["k_cmask"][:, :])
    sets = []
    for i in range(2):
        d = {}
        d["Bbr"] = P.sb("Bbr", [128, 32]); d["Bbi"] = P.sb("Bbi", [128, 32])
        for nm in ("ETr", "ETi", "G1r", "G1in", "G2r", "G2i", "Fr", "Fin", "ta", "tb"):
            d[nm] = P.sb(nm, [128, 8, 32])
        d["Esb"] = P.sb("Esb2", [128, 2, 2, 128])
        d["Msb"] = P.sb("Msb", [128, 3, 128])
        d["Usb"] = P.sb("Usb", [128, 2, NCH])
        d["XA"] = P.sb("XA", [128, 2, NCH + 1]); d["XB"] = P.sb("XB", [128, 2, NCH + 1])
        P.op("dve", lambda e, t=d["XA"]: e.memset(t.h[:], 0.0), [], [d["XA"]])
        P.op("dve", lambda e, t=d["XB"]: e.memset(t.h[:], 0.0), [], [d["XB"]])
        sets.append(d)
    pY = PS[0:4]
    pU, pW, pE, pM = PS[4], PS[5], PS[6], PS[7]

    def bc8(t, off, pstride, kstride):
        return V(t, bass.AP(t.h, off, [[pstride, 128], [kstride, 8], [0, 32]]))

    def bcx(t, off, pstride):
        return V(t, bass.AP(t.h, off, [[pstride, 128], [0, 8], [1, 32]]))

    def cm(d, outr, outi, ar, ai, br, bi, neg_im=False):
        ta, tb_ = d["ta"], d["tb"]
        P.tt("dve", ta[:], ar, br, ALU.mult)
        P.tt("dve", tb_[:], ai, bi, ALU.mult)
        P.tt("dve", outr[:], ta[:], tb_[:], ALU.subtract)
        P.tt("dve", ta[:], ar, bi, ALU.mult)
        P.tt("dve", tb_[:], ai, br, ALU.mult)
        if neg_im:
            P.stt("dve", outi[:], ta[:], -1.0, tb_[:], ALU.mult, ALU.subtract)
        else:
            P.tt("dve", outi[:], ta[:], tb_[:], ALU.add)

    Xf = {}

    def phaseA(st):
        ct, j = st // 4, st % 4
        st = ct * 4 + j
        d = sets[st % 2]
        Bbr, Bbi = d["Bbr"], d["Bbi"]
        P.ts("dve", Bbr[:], bmi[:, st, :], zimn[:, st:st + 1], ALU.mult)
        P.stt("dve", Bbr[:], bmr[:, st, :], zre[:, st:st + 1], Bbr[:], ALU.mult, ALU.add)
        P.ts("dve", Bbi[:], bmr[:, st, :], zim[:, st:st + 1], ALU.mult)
        P.stt("dve", Bbi[:], bmi[:, st, :], zre[:, st:st + 1], Bbi[:], ALU.mult, ALU.add)
        bbr_b, bbi_b = bcx(Bbr, 0, 32), bcx(Bbi, 0, 32)
        cr_b, ci_b = bcx(cmr, st * 32, 512), bcx(cmi, st * 32, 512)
        cm(d, d["ETr"], d["ETi"], bbr_b, bbi_b, bc8(TB["PWDr"], st, 128, 16), bc8(TB["PWDi"], st, 128, 16))
        cm(d, d["G1r"], d["G1in"], bbr_b, bbi_b, bc8(TB["IPr"], st, 128, 16), bc8(TB["IPi"], st, 128, 16), neg_im=True)
        cm(d, d["G2r"], d["G2i"], cr_b, ci_b, bc8(TB["PWr"], st, 144, 16), bc8(TB["PWi"], st, 144, 16))
        cm(d, d["Fr"], d["Fin"], cr_b, ci_b, bc8(TB["PWr"], st + 16, 144, 16), bc8(TB["PWi"], st + 16, 144, 16), neg_im=True)
        fl = lambda t: t.h[:].rearrange("p a b -> p (a b)")
        for ri, nm in enumerate(("ETr", "ETi")):
            for k in range(2):
                P.tr(pE[:, (ri * 2 + k) * 128:(ri * 2 + k + 1) * 128], V(d[nm], fl(d[nm])[:, k * 128:(k + 1) * 128]), ident[:, :])
        P.cp("act", V(d["Esb"], d["Esb"].h[:].rearrange("p a b c -> p (a b c)")), pE[:, :])
        for bi_, (k, mt) in enumerate(((0, 0), (0, 1), (1, 1))):
            oc = pM[:, bi_ * 128:(bi_ + 1) * 128]
            P.mm(oc, V(d["G1r"], fl(d["G1r"])[:, k * 128:(k + 1) * 128]), V(d["G2r"], fl(d["G2r"])[:, mt * 128:(mt + 1) * 128]), start=True, stop=False)
            P.mm(oc, V(d["G1in"], fl(d["G1in"])[:, k * 128:(k + 1) * 128]), V(d["G2i"], fl(d["G2i"])[:, mt * 128:(mt + 1) * 128]), start=False, stop=True)
        Msb = d["Msb"]
        P.tt("dve", Msb[:, 0, :], pM[:, 0:128], cmask[:, :], ALU.mult)
        P.cp("dve", Msb[:, 1, :], pM[:, 128:256])
        P.tt("dve", Msb[:, 2, :], pM[:, 256:384], cmask[:, :], ALU.mult)
        P.stt("dve", Msb[:, 0, :], ident[:, :], drep[:, st:st + 1], Msb[:, 0, :], ALU.mult, ALU.add)
        P.stt("dve", Msb[:, 2, :], ident[:, :], drep[:, st:st + 1], Msb[:, 2, :], ALU.mult, ALU.add)
        Usb = d["Usb"]
        for k in range(2):
            for sl in range(4):
                sx = 4 * k + sl
                rhs = V(uT, bass.AP(uT.h, ct * SEQ + sx, [[4 * SEQ, 128], [8, NCH]]))
                P.mm(pU[:, k * NCH:(k + 1) * NCH], selj[:, j * 4 + sl, :], rhs, start=(sl == 0), stop=(sl == 3))
        P.cp("act", V(Usb, Usb.h[:].rearrange("p a b -> p (a b)")), pU[:, :])
        Esb = d["Esb"]
        for ri in range(2):
            for k in range(2):
                P.mm(pW[:, ri * NCH:(ri + 1) * NCH], Esb[:, ri, k, :], Usb[:, k, :], start=(k == 0), stop=(k == 1))
        XA, XB = d["XA"], d["XB"]
        P.cp("dve", XA[:, :, 1:NCH + 1], V(pW, pW.h[:, :].rearrange("p (a b) -> p a b", a=2)))
        src, dst = XA, XB
        for k in range(8):
            dd = 1 << k
            lr = TB["LCr"][:, k, st:st + 1]; li = TB["LCi"][:, k, st:st + 1]; lin = TB["LCin"][:, k, st:st + 1]
            P.cp("act", dst[:, :, 1:1 + dd], src[:, :, 1:1 + dd])
            lo = slice(1, NCH + 1 - dd)
            hi = slice(1 + dd, NCH + 1)
            P.stt("dve", dst[:, 0, hi], src[:, 0, lo], lr, src[:, 0, hi], ALU.mult, ALU.add)
            P.stt("dve", dst[:, 0, hi], src[:, 1, lo], lin, dst[:, 0, hi], ALU.mult, ALU.add)
            P.stt("dve", dst[:, 1, hi], src[:, 1, lo], lr, src[:, 1, hi], ALU.mult, ALU.add)
            P.stt("dve", dst[:, 1, hi], src[:, 0, lo], li, dst[:, 1, hi], ALU.mult, ALU.add)
            src, dst = dst, src
        Xf[st] = src

    def phaseB(st):
        ct, j = st // 4, st % 4
        d = sets[st % 2]
        Msb, Usb, X = d["Msb"], d["Usb"], Xf[st]
        pr = slice(32 * j, 32 * j + 32)
        for t in range(8):
            mt, tl = t // 4, t % 4
            oc = pY[t // 2][pr, (t % 2) * NCH:(t % 2 + 1) * NCH]
            cs_ = slice(tl * 32, (tl + 1) * 32)
            tp = (0, 32 * j)
            P.mm(oc, Msb[:, mt, cs_], Usb[:, 0, :], start=True, stop=False, tp=tp)
            if mt == 1:
                P.mm(oc, Msb[:, 2, cs_], Usb[:, 1, :], start=False, stop=False, tp=tp)
            P.mm(oc, d["Fr"][:, t, :], X[:, 0, 0:NCH], start=False, stop=False, tp=tp)
            P.mm(oc, d["Fin"][:, t, :], X[:, 1, 0:NCH], start=False, stop=True, tp=tp)

    def gelu_out(ct):
        for t in range(8):
            dst_ = V(yT, bass.AP(yT.h, ct * SEQ + t, [[4 * SEQ, 128], [8, NCH]]))
            P.act(dst_, pY[t // 2][:, (t % 2) * NCH:(t % 2 + 1) * NCH], AF.Gelu)

    phaseA(0)
    for st in range(16):
        if st + 1 < 16:
            phaseA(st + 1)
        phaseB(st)
        if st % 4 == 3:
            gelu_out(st // 4)
    if b == 0:
        tap("ygelu", yT, [128, 4, SEQ])


def stage2_old(P, I, PS, uT, yT, s_zre, s_zim, s_zimn, Lre, Lim, Limn, sdk, tap, b):
    N = SEQ
    btr = P.sb("btr", [128, 4, 128]); P.dma("sp", btr[:], I["s_btr"][:, :, :])
    bti = P.sb("bti", [128, 4, 128]); P.dma("sp", bti[:], I["s_bti"][:, :, :])
    ctr = P.sb("ctr", [128, 16, 128]); P.dma("sp", ctr[:], I["s_ctr"][:, :, :])
    ctin = P.sb("ctin", [128, 16, 128]); P.dma("sp", ctin[:], I["s_cti"][:, :, :])
    P.ts("dve", ctin[:], ctin[:], -1.0, ALU.mult)
    X = [[P.sb("x%d%d" % (i, j), [128, N]) for j in range(2)] for i in range(2)]
    t1 = P.sb("s2t1", [128, 512])
    t2 = P.sb("s2t2", [128, 512])
    pY = PS[0:4]
    pR, pI_ = PS[4], PS[5]
    for ct in range(4):
        for j in range(4):
            st = ct * 4 + j
            pr = slice(32 * j, 32 * j + 32)
            for tb in range(4):
                cs = slice(tb * 512, (tb + 1) * 512)
                P.mm(pR[:, :], btr[pr, ct, :], uT[pr, ct, cs], tp=(32 * j, 0))
                P.mm(pI_[:, :], bti[pr, ct, :], uT[pr, ct, cs], tp=(32 * j, 0))
                P.ts("dve", t1[:], pI_[:, :], s_zimn[:, st:st + 1], ALU.mult)
                P.stt("dve", X[0][0][:, cs], pR[:, :], s_zre[:, st:st + 1], t1[:], ALU.mult, ALU.add)
                P.ts("dve", t2[:], pR[:, :], s_zim[:, st:st + 1], ALU.mult)
                P.stt("dve", X[0][1][:, cs], pI_[:, :], s_zre[:, st:st + 1], t2[:], ALU.mult, ALU.add)
            if st == 0 and b == 0:
                tap("bure", X[0][0], [128, N])
            cur = 0
            for k in range(11):
                d = 1 << k
                sr, si = X[cur]
                dr, di = X[1 - cur]
                lr = Lre[:, k, st:st + 1]
                li = Lim[:, k, st:st + 1]
                lin = Limn[:, k, st:st + 1]
                P.cp("act", dr[:, 0:d], sr[:, 0:d])
                P.cp("act", di[:, 0:d], si[:, 0:d])
                P.stt("dve", dr[:, d:N], sr[:, 0:N - d], lr, sr[:, d:N], ALU.mult, ALU.add)
                P.stt("dve", dr[:, d:N], si[:, 0:N - d], lin, dr[:, d:N], ALU.mult, ALU.add)
                P.stt("dve", di[:, d:N], si[:, 0:N - d], lr, si[:, d:N], ALU.mult, ALU.add)
                P.stt("dve", di[:, d:N], sr[:, 0:N - d], li, di[:, d:N], ALU.mult, ALU.add)
                cur = 1 - cur
            xr, xi = X[cur]
            if st == 0 and b == 0:
                tap("xre", xr, [128, N])
            for tb in range(4):
                cs = slice(tb * 512, (tb + 1) * 512)
                P.mm(pY[tb][:, :], ctr[:, st, :], xr[:, cs], start=(j == 0), stop=False)
                P.mm(pY[tb][:, :], ctin[:, st, :], xi[:, cs], start=False, stop=(j == 3))
        for tb in range(4):
            cs = slice(tb * 512, (tb + 1) * 512)
            P.stt("dve", t1[:], uT[:, ct, cs], sdk[:, ct:ct + 1], pY[tb][:, :], ALU.mult, ALU.add)
            P.act(yT[:, ct, cs], t1[:], AF.Gelu)
    if b == 0:
        tap("ygelu", yT, [128, 4, SEQ])


def stage3a(P, I, PS, b, ones, ohg, yT, glub, snw, mixed_d, tap):
    gluw = P.sb("gluw", [128, 4, 512])
    gw = I["glu_w"]
    P.dma("sp", gluw[:], V(gw, gw.h[:, :].rearrange("(kt p) n -> p kt n", p=128)))
    wout = P.sb("wout", [128, 8, D])
    wo = I["w_out"]
    P.dma("sp", wout[:], V(wo, wo.h[:, :].rearrange("(kt p) n -> p kt n", p=128)))
    y2 = P.sb("y2", [128, 4, 512])
    sq = P.sb("sq", [128, 512])
    rstd = P.sb("rstd", [128, 512])
    sg = P.sb("sg", [128, 512])
    mt = [P.sb("mt%d" % i, [128, D]) for i in range(2)]
    for tb in range(4):
        cs = slice(tb * 512, (tb + 1) * 512)
        pn = PS[2]
        for c2 in range(4):
            pg = PS[c2 % 2]
            for kt in range(4):
                P.mm(pg[:, :], gluw[:, kt, c2 * 128:(c2 + 1) * 128], yT[:, kt, cs], start=(kt == 0), stop=(kt == 3))
            P.act(sg[:], pg[:, :], AF.Sigmoid, bias=glub[:, c2:c2 + 1])
            P.tt("dve", y2[:, c2, :], yT[:, c2, cs], sg[:], ALU.mult)
            P.act(sq[:], y2[:, c2, :], AF.Square)
            P.mm(pn[:, :], ones[:, :], sq[:], start=(c2 == 0), stop=(c2 == 3))
        P.act(rstd[:], pn[:, :], AF.Sqrt, bias=RMS_EPS, scale=1.0 / 512.0)
        P.recip(rstd[:], rstd[:])
        for c2 in range(4):
            P.stt("dve", yT[:, c2, cs], y2[:, c2, :], snw[:, c2:c2 + 1], rstd[:], ALU.mult, ALU.mult)
        for tt_ in range(4):
            tcs = slice(tb * 512 + tt_ * 128, tb * 512 + (tt_ + 1) * 128)
            m = mt[tt_ % 2]
            for nb in range(2):
                pm = PS[3 + nb]
                for ft in range(8):
                    l = ohg[:, ft, tcs] if ft < 4 else yT[:, ft - 4, tcs]
                    P.mm(pm[:, :], l, wout[:, ft, nb * 512:(nb + 1) * 512], start=(ft == 0), stop=(ft == 7))
                P.cp("act" if nb == 0 else "dve", m[:, nb * 512:(nb + 1) * 512], pm[:, :])
            r0 = b * SEQ + tb * 512 + tt_ * 128
            P.dma("sp", mixed_d[r0:r0 + 128, :], m[:], src_sem=True)
    if b == 0:
        tap("ossm", yT, [128, 4, SEQ])


def layer_norm_tile(P, v, outt, stats, mv, sc, w_bc, b_bc):
    for c in range(2):
        P.op("dve", lambda e, c=c: e.bn_stats(out=stats.h[:, c, :], in_=v.h[:, c * 512:(c + 1) * 512]), [v], [stats])
    P.op("dve", lambda e: e.bn_aggr(out=mv.h[:, :], in_=stats.h[:].rearrange("p a b -> p (a b)")), [stats], [mv])
    P.ts("dve", sc[:, 0:1], mv[:, 1:2], LN_EPS, ALU.add)
    P.act(sc[:, 0:1], sc[:, 0:1], AF.Sqrt)
    P.recip(sc[:, 0:1], sc[:, 0:1])
    P.stt("dve", sc[:, 1:2], mv[:, 0:1], -1.0, sc[:, 0:1], ALU.mult, ALU.mult)
    yield
    P.act(v[:], v[:], AF.Identity, bias=sc[:, 1:2], scale=sc[:, 0:1])
    P.tt("dve", v[:], v[:], w_bc[:], ALU.mult)
    yield
    P.tt("dve", outt[:], v[:], b_bc[:], ALU.add)
    yield


def stage3b(P, I, PS, ident, mod_d, mixed_d, out_d, bc_row, tap, upto):
    xin = I["x"]
    wq_d = I["w_q"]
    pu, pv = I["pu"], I["pv"]
    NT = TOK // 128
    RB1 = [P.sb("rb_%d" % q, [128, D]) for q in range(4)]
    sel = P.sb("sel3", [2, 2, 128]); P.dma("sp", sel[:], I["k_sel"][:, :, :])
    modq = P.sb("modq", [2, 1024])
    lnw = []
    for nm in ("ln1w", "ln1b", "ln2w", "ln2b"):
        t = P.sb(nm, [128, D])
        P.dma("sp", t[:], bc_row(nm))
        lnw.append(t)
    keysT = P.sb("keysT", [128, 16, 128]); P.dma("sp", keysT[:], I["keysT"][:, :, :])
    iota = P.sb("iota", [128, 16]); P.dma("sp", iota[:], I["k_iota"][:, :])
    xt = P.sb("xt3", [128, D]); mt = P.sb("mt3", [128, D]); vtf = P.sb("vtf", [128, D])
    statsf = P.sb("statsf", [128, 2, 6]); mvf = P.sb("mvf", [128, 2]); scf = P.sb("scf", [128, 2])
    h2T = P.sb("h2T", [128, 8, 128])
    wqb = [P.sb("wqb%d" % i, [128, 8, 128]) for i in range(2)]
    qT = P.sb("qT", [128, 16, 128])
    scs = P.sb("scs", [128, 16, 128]); scs2 = P.sb("scs2", [128, 16, 128])
    top = P.sb("top", [128, 16, 16])
    idx = P.sb("idx", [128, 16, 16], U32)
    idxf = P.sb("idxf", [128, 16, 16])
    cand = P.sb("cand", [128, 8, 256])
    best = P.sb("best", [128, 8, 16])
    pos = P.sb("pos", [128, 8, 16], U32)
    pi_ = P.sb("pi", [128, 8, 16], U32); pj_ = P.sb("pj", [128, 8, 16], U32)
    pif = P.sb("pif", [128, 8, 16]); pjf = P.sb("pjf", [128, 8, 16])
    oh = P.sb("oh", [128, 8, 16, 16])
    i1 = P.sb("i1", [128, 8, 16]); i2 = P.sb("i2", [128, 8, 16])
    eidf = P.sb("eidf", [128, 128]); gsum = P.sb("gsum", [128, 8])
    x1s = [P.sb("x1_%d" % i, [128, D]) for i in range(2)]
    h2s = [P.sb("h2_%d" % i, [128, D]) for i in range(2)]
    eids = [P.sb("eid_%d" % i, [128, 128], I32) for i in range(2)]
    gates = [P.sb("gate_%d" % i, [128, 8, 16]) for i in range(2)]
    vtg = P.sb("vtg", [128, D]); acc = P.sb("acc", [128, D]); ot = P.sb("ot", [128, D])
    acc2 = P.sb("acc2", [128, D])
    statsg = P.sb("statsg", [128, 2, 6]); mvg = P.sb("mvg", [128, 2]); scg = P.sb("scg", [128, 2])
    z = P.sb("z", [128, 128]); a_ = P.sb("a", [128, 128])
    NBUF = 6
    dgs = [P.sb("dg%d" % i, [128, 128]) for i in range(4)]
    gb = [P.sb("gb%d" % i, [128, D]) for i in range(2 * NBUF)]
    gi = [0]

    def nextbuf():
        t = gb[gi[0] % (2 * NBUF)]
        gi[0] += 1
        return t
    wi = [0]

    def front(ti):
        S = ti % 2
        x1, h2, eid, gate = x1s[S], h2s[S], eids[S], gates[S]
        b = ti // (SEQ // 128)
        r0 = ti * 128
        if ti % (SEQ // 128) == 0:
            for q, (off, plus1) in enumerate([(2048, True), (3072, False), (4096, True), (5120, True)]):
                P.dma("sp", modq[:], mod_d[:, off:off + 1024])
                for hb_ in range(2):
                    pt = PS[hb_]
                    P.mm(pt[:, :], sel[:, b, :], modq[:, hb_ * 512:(hb_ + 1) * 512])
                    dst = RB1[q][:, hb_ * 512:(hb_ + 1) * 512]
                    if plus1:
                        P.ts("dve", dst, pt[:, :], 1.0, ALU.add)
                    else:
                        P.cp("dve", dst, pt[:, :])
                    yield
        g1p, sh2, sc2p, g2p = RB1
        P.dma("sp", xt[:], xin[r0:r0 + 128, :])
        P.dma("sp", mt[:], mixed_d[r0:r0 + 128, :])
        P.tt("dve", vtf[:], mt[:], g1p[:], ALU.mult)
        yield
        P.stt("dve", vtf[:], xt[:], ALPHA, vtf[:], ALU.mult, ALU.add)
        yield
        for _ in layer_norm_tile(P, vtf, x1, statsf, mvf, scf, lnw[0], lnw[1]):
            yield
        P.tt("dve", h2[:], x1[:], sc2p[:], ALU.mult)
        yield
        P.tt("dve", h2[:], h2[:], sh2[:], ALU.add)
        yield
        if ti == 0:
            tap("x1", x1, [128, D]); tap("h2", h2, [128, D])
        for half in range(2):
            pt = PS[half]
            for j in range(4):
                kt = half * 4 + j
                P.tr(pt[:, j * 128:(j + 1) * 128], h2[:, kt * 128:(kt + 1) * 128], ident[:, :])
            P.cp("act", V(h2T, h2T.h[:, half * 4:(half + 1) * 4, :].rearrange("p a b -> p (a b)")), pt[:, :])
            yield
        for cj in range(16):
            wb = wqb[wi[0] % 2]
            wi[0] += 1
            P.dma("sp", wb[:], V(wq_d, wq_d.h[:, cj * 128:(cj + 1) * 128].rearrange("(kt p) n -> p kt n", p=128)))
            pq = PS[2 + (cj // 4) % 2]
            for kt in range(8):
                P.mm(pq[:, (cj % 4) * 128:(cj % 4 + 1) * 128], wb[:, kt, :], h2T[:, kt, :], start=(kt == 0), stop=(kt == 7))
                if kt % 2 == 1:
                    yield
            if cj % 4 == 3:
                g4 = cj // 4
                P.cp("act", V(qT, qT.h[:, g4 * 4:(g4 + 1) * 4, :].rearrange("p a b -> p (a b)")), pq[:, :])
        for g4 in range(4):
            psc = PS[4 + g4 % 2]
            for jj in range(4):
                cj = g4 * 4 + jj
                P.mm(psc[:, jj * 128:(jj + 1) * 128], qT[:, cj, :], keysT[:, cj, :])
            P.cp("act" if g4 % 2 else "dve", V(scs, scs.h[:, g4 * 4:(g4 + 1) * 4, :].rearrange("p a b -> p (a b)")), psc[:, :])
            yield
        if ti == 0:
            tap("scs", scs, [128, 16, 128])
        for cj in range(16):
            P.op("dve", lambda e, cj=cj: e.max(out=top.h[:, cj, 0:8], in_=scs.h[:, cj, :]), [scs], [top])
            P.op("dve", lambda e, cj=cj: e.match_replace(out=scs2.h[:, cj, :], in_to_replace=top.h[:, cj, 0:8],
                                                         in_values=scs.h[:, cj, :], imm_value=NEG), [scs, top], [scs2])
            yield
            P.op("dve", lambda e, cj=cj: e.max(out=top.h[:, cj, 8:16], in_=scs2.h[:, cj, :]), [scs2], [top])
            P.op("dve", lambda e, cj=cj: e.max_index(out=idx.h[:, cj, 0:8], in_max=top.h[:, cj, 0:8], in_values=scs.h[:, cj, :]), [scs, top], [idx])
            yield
            P.op("dve", lambda e, cj=cj: e.max_index(out=idx.h[:, cj, 8:16], in_max=top.h[:, cj, 8:16], in_values=scs.h[:, cj, :]), [scs, top], [idx])
            yield
        P.cp("dve", idxf[:], idx[:])
        in0 = V(top, bass.AP(top.h, 0, [[256, 128], [32, 8], [1, 16], [0, 16]]))
        in1 = V(top, bass.AP(top.h, 16, [[256, 128], [32, 8], [0, 16], [1, 16]]))
        P.tt("dve", V(cand, cand.h[:].rearrange("p h (i j) -> p h i j", i=16)), in0, in1, ALU.add)
        yield
        for h in range(8):
            c2v = lambda h=h: scs2.h[:, 2 * h:2 * h + 2, :].rearrange("p a b -> p (a b)")
            P.op("dve", lambda e, h=h: e.max(out=best.h[:, h, 0:8], in_=cand.h[:, h, :]), [cand], [best])
            P.op("dve", lambda e, h=h: e.match_replace(out=c2v(h), in_to_replace=best.h[:, h, 0:8],
                                                       in_values=cand.h[:, h, :], imm_value=NEG), [cand, best], [scs2])
            yield
            P.op("dve", lambda e, h=h: e.max(out=best.h[:, h, 8:16], in_=c2v(h)), [scs2], [best])
            P.op("dve", lambda e, h=h: e.max_index(out=pos.h[:, h, 0:8], in_max=best.h[:, h, 0:8], in_values=cand.h[:, h, :]), [cand, best], [pos])
            yield
            P.op("dve", lambda e, h=h: e.max_index(out=pos.h[:, h, 8:16], in_max=best.h[:, h, 8:16], in_values=cand.h[:, h, :]), [cand, best], [pos])
            yield
        P.op("dve", lambda e: e.tensor_single_scalar(out=pi_.h[:], in_=pos.h[:], scalar=4, op=ALU.logical_shift_right), [pos], [pi_])
        P.op("dve", lambda e: e.tensor_single_scalar(out=pj_.h[:], in_=pos.h[:], scalar=15, op=ALU.bitwise_and), [pos], [pj_])
        yield
        P.cp("dve", pif[:], pi_[:])
        P.cp("dve", pjf[:], pj_[:])
        yield
        io = V(iota, bass.AP(iota.h, 0, [[16, 128], [0, 8], [0, 16], [1, 16]]))
        for (pf, which, dst) in ((pif, 0, i1), (pjf, 1, i2)):
            pfb = V(pf, bass.AP(pf.h, 0, [[128, 128], [16, 8], [1, 16], [0, 16]]))
            P.tt("dve", oh[:], pfb, io, ALU.is_equal)
            yield
            ixb = V(idxf, bass.AP(idxf.h, 16 * which, [[256, 128], [32, 8], [0, 16], [1, 16]]))
            P.tt("dve", oh[:], oh[:], ixb, ALU.mult)
            yield
            P.op("dve", lambda e, dst=dst: e.tensor_reduce(out=dst.h[:], in_=oh.h[:], axis=AX.X, op=ALU.add), [oh], [dst])
            yield
        P.stt("dve", eidf[:], V(i1, i1.h[:].rearrange("p a b -> p (a b)")), 128.0, V(i2, i2.h[:].rearrange("p a b -> p (a b)")), ALU.mult, ALU.add)
        P.cp("dve", eid[:], eidf[:])
        yield
        b0 = V(best, bass.AP(best.h, 0, [[128, 128], [16, 8], [0, 16]]))
        P.tt("dve", gate[:], best[:], b0, ALU.subtract)
        P.act(gate[:], gate[:], AF.Exp)
        yield
        P.op("dve", lambda e: e.tensor_reduce(out=gsum.h[:], in_=gate.h[:], axis=AX.X, op=ALU.add), [gate], [gsum])
        P.recip(gsum[:], gsum[:])
        gs = V(gsum, bass.AP(gsum.h, 0, [[8, 128], [1, 8], [0, 16]]))
        P.tt("dve", gate[:], gate[:], gs, ALU.mult)
        yield
        if ti == 0:
            tap("eidf", eidf, [128, 128]); tap("gate", gate, [128, 8, 16])

    def adv(g, n):
        if g is None:
            return None
        for _ in range(n):
            try:
                next(g)
            except StopIteration:
                return None
        return g

    def gather(ti, fg):
        S = ti % 2
        x1, h2, eid, gate = x1s[S], h2s[S], eids[S], gates[S]
        r0 = ti * 128
        g2p = RB1[3]
        for s in range(128):
            u = nextbuf()
            P.dma("pool", u[:], V(pu, pu.h[:, :]),
                  fn=lambda e, u=u, s=s: e.indirect_dma_start(out=u.h[:], out_offset=None, in_=pu.h[:, :],
                                                             in_offset=bass.IndirectOffsetOnAxis(ap=eid.h[:, s:s + 1], axis=0)),
                  extra=[eid])
            P.op("dve", lambda e, u=u, s=s: e.scalar_tensor_tensor(out=ot.h[:], in0=u.h[:], scalar=1.0, in1=h2.h[:],
                                                                 op0=ALU.mult, op1=ALU.mult, accum_out=z.h[:, s:s + 1]), [u, h2], [ot, z])
            fg = adv(fg, 1)
        P.act(a_[:], z[:], AF.Gelu)
        P.tt("dve", a_[:], a_[:], V(gate, gate.h[:].rearrange("p a b -> p (a b)")), ALU.mult)
        for s in range(128):
            v = nextbuf()
            P.dma("pool", v[:], V(pv, pv.h[:, :]),
                  fn=lambda e, v=v, s=s: e.indirect_dma_start(out=v.h[:], out_offset=None, in_=pv.h[:, :],
                                                             in_offset=bass.IndirectOffsetOnAxis(ap=eid.h[:, s:s + 1], axis=0)),
                  extra=[eid])
            if s % 2 == 1:
                if s == 1:
                    P.ts("dve", acc2[:], v[:], a_[:, s:s + 1], ALU.mult)
                else:
                    P.stt("dve", acc2[:], v[:], a_[:, s:s + 1], acc2[:], ALU.mult, ALU.add)
            else:
                dg = dgs[s % 4]
                P.act(dg[:], ident[:, :], AF.Copy, scale=a_[:, s:s + 1])
                for hf in range(2):
                    P.mm(PS[6 + hf][:, :], dg[:], v[:, hf * 512:(hf + 1) * 512], start=(s == 0), stop=(s == 126))
            fg = adv(fg, 2)
        fg = adv(fg, 100000)
        P.tt("dve", acc[:, 0:512], PS[6][:, :], acc2[:, 0:512], ALU.add)
        P.tt("dve", acc[:, 512:1024], PS[7][:, :], acc2[:, 512:1024], ALU.add)
        if ti == 0:
            tap("ffn", acc, [128, D]); tap("z", z, [128, 128])
        P.tt("dve", vtg[:], acc[:], g2p[:], ALU.mult)
        P.stt("dve", vtg[:], x1[:], ALPHA, vtg[:], ALU.mult, ALU.add)
        for _ in layer_norm_tile(P, vtg, ot, statsg, mvg, scg, lnw[2], lnw[3]):
            pass
        P.dma("sp", out_d[r0:r0 + 128, :], ot[:], src_sem=True)

    adv(front(0), 100000)
    for ti in range(NT):
        nxt = ti + 1
        if nxt < NT and nxt % (SEQ // 128) != 0:
            gather(ti, front(nxt))
        else:
            gather(ti, None)
            if nxt < NT:
                adv(front(nxt), 100000)


_CACHE = {}


def kernel(**inputs):
    shared = _prep_shared(inputs)
    x = np.ascontiguousarray(inputs["x"], dtype=np.float32)
    c = np.ascontiguousarray(inputs["c"], dtype=np.float32)
    in_maps = []
    for core in range(NCORES):
        m = dict(shared)
        m["x"] = x[core * NB:(core + 1) * NB].reshape(TOK, D)
        m["cT"] = np.ascontiguousarray(c[core * NB:(core + 1) * NB].T.reshape(8, 128, NB).transpose(1, 0, 2))
        in_maps.append(m)
    nc = build()
    res = run_bass_kernel_spmd(nc, in_maps, core_ids=list(range(NCORES)))
    out = np.concatenate([r["out"].reshape(NB, SEQ, D) for r in res.results], axis=0)
    return out.astype(np.float32)
```
